# Optimizing a Trainium2 kernel written in Bass

```python
import math
import numpy as np
import jax, jax.numpy as jnp
from jax import lax

D_MODEL = 1024
BATCH = 4
SEQ = 4096
DEPTH = 1

D_MIX = D_MODEL
D_PLE = 256
DA = D_MIX // 2
HA = 4
DA_V = DA // HA
DA_QK = DA_V // 2
DB = D_MIX - DA
HB = 16
DB_H = DB // HB
D_IN = 3 * DA + 3 * DB
GRID_W = 64
WIN_R = 8
WIN_C = 16
Q_COLS = 16
K_COLS = 32
ROPE_THETA = 10000.0
EPS = 1e-6
Q_BLOCK = 128
NEG = -1e30
N_GROUPS = 4
EXPERTS_PER_GROUP = 8
N_EXPERTS = N_GROUPS * EXPERTS_PER_GROUP
TOP_K = 2
D_EXPERT = 512
MOE_BLOCK = 128

kernel_name = 'hybrid_diffattn_natten_hmoe_encoder'


def rms_norm(x, g):
    xf = x.astype(jnp.float32)
    y = xf * lax.rsqrt(jnp.mean(xf * xf, axis=-1, keepdims=True) + EPS)
    return (y * g.astype(jnp.float32)).astype(x.dtype)


def rope_tables(seq, dim):
    inv = ROPE_THETA ** (-jnp.arange(0, dim, 2, dtype=jnp.float32) / dim)
    ang = jnp.arange(seq, dtype=jnp.float32)[:, None] * inv[None, :]
    return jnp.cos(ang), jnp.sin(ang)


def apply_rope(x, cos, sin):
    s = x.shape[1]
    bshape = (s,) + (1,) * (x.ndim - 3) + (-1,)
    c = cos.reshape(bshape)
    sn = sin.reshape(bshape)
    x1, x2 = jnp.split(x.astype(jnp.float32), 2, axis=-1)
    return jnp.concatenate([x1 * c - x2 * sn, x2 * c + x1 * sn], axis=-1).astype(x.dtype)


def diff_attention(q, k, v, g_q, g_k, lam_q1, lam_k1, lam_q2, lam_k2, g_sub, lam_init):
    b, s = q.shape[:2]
    cos, sin = rope_tables(s, DA_QK)
    q = apply_rope(rms_norm(q, g_q), cos, sin)
    k = apply_rope(rms_norm(k, g_k), cos, sin)
    lam = (jnp.exp(jnp.sum(lam_q1.astype(jnp.float32) * lam_k1.astype(jnp.float32)))
           - jnp.exp(jnp.sum(lam_q2.astype(jnp.float32) * lam_k2.astype(jnp.float32)))
           + lam_init)
    scale = DA_QK ** -0.5
    qt = q.transpose(0, 2, 1, 3, 4)
    kt = k.transpose(0, 2, 1, 3, 4)
    vt = v.transpose(0, 2, 1, 3)
    k1, k2 = kt[..., 0, :], kt[..., 1, :]
    nqb = s // Q_BLOCK
    qb = qt.reshape(b, HA, nqb, Q_BLOCK, 2, DA_QK).transpose(2, 0, 1, 3, 4, 5)

    def block(qblk):
        s1 = jnp.einsum('bhqd,bhkd->bhqk', qblk[..., 0, :], k1).astype(jnp.float32) * scale
        s2 = jnp.einsum('bhqd,bhkd->bhqk', qblk[..., 1, :], k2).astype(jnp.float32) * scale
        w = (jax.nn.softmax(s1, axis=-1) - lam * jax.nn.softmax(s2, axis=-1)).astype(vt.dtype)
        return jnp.einsum('bhqk,bhkd->bhqd', w, vt)

    o = lax.map(block, qb)
    o = o.transpose(1, 0, 3, 2, 4).reshape(b, s, HA, DA_V)
    o = rms_norm(o, g_sub) * (1.0 - lam_init)
    return o.reshape(b, s, HA * DA_V)


def neighbourhood_attention(q, k, v, g_q, g_k, rpb):
    b, s, h, d = q.shape
    rows = s // GRID_W
    kr = min(WIN_R, rows)
    ncb = GRID_W // Q_COLS
    q = rms_norm(q, g_q)
    k = rms_norm(k, g_k)
    r = np.arange(rows)
    rs = np.clip(r - kr // 2, 0, rows - kr)
    key_rows = rs[:, None] + np.arange(kr)[None, :]
    row_off = (key_rows - r[:, None] + (WIN_R - 1)).astype(np.int32)
    c = np.arange(GRID_W).reshape(ncb, Q_COLS)
    cs = np.clip(c - WIN_C // 2, 0, GRID_W - WIN_C)
    kb = np.minimum(cs[:, 0], GRID_W - K_COLS)
    key_cols = kb[:, None] + np.arange(K_COLS)[None, :]
    col_valid = ((key_cols[:, None, :] >= cs[:, :, None])
                 & (key_cols[:, None, :] < cs[:, :, None] + WIN_C))
    col_off = np.clip(key_cols[:, None, :] - c[:, :, None] + (WIN_C - 1), 0, 2 * WIN_C - 2)
    key_idx = (key_rows[:, None, :, None] * GRID_W + key_cols[None, :, None, :])
    key_idx = key_idx.reshape(rows, ncb * kr * K_COLS).astype(np.int32)
    bias_cols = rpb.astype(jnp.float32)[:, :, col_off]
    bias_cols = jnp.where(col_valid, bias_cols, NEG)
    scale = d ** -0.5
    q_rows = q.reshape(b, rows, GRID_W, h, d).transpose(1, 0, 2, 3, 4)

    def row_step(args):
        q_row, idx, roff = args
        qb = q_row.reshape(b, ncb, Q_COLS, h, d)
        kg = k[:, idx].reshape(b, ncb, kr * K_COLS, h, d)
        vg = v[:, idx].reshape(b, ncb, kr * K_COLS, h, d)
        bias = bias_cols[:, roff]
        bias = bias.transpose(0, 2, 3, 1, 4).reshape(h, ncb, Q_COLS, kr * K_COLS)
        sc = jnp.einsum('bjqhd,bjkhd->bhjqk', qb, kg).astype(jnp.float32) * scale + bias
        pr = jax.nn.softmax(sc, axis=-1).astype(vg.dtype)
        o = jnp.einsum('bhjqk,bjkhd->bjqhd', pr, vg)
        return o.reshape(b, GRID_W, h, d)

    o = lax.map(row_step, (q_rows, jnp.asarray(key_idx), jnp.asarray(row_off)))
    return o.transpose(1, 0, 2, 3, 4).reshape(b, s, h * d)


def hier_moe(x, w_rg, w_re, w1, w3, w2):
    b, s, dm = x.shape
    t = b * s
    xf = x.reshape(t, dm)
    g_prob = jax.nn.softmax((xf @ w_rg).astype(jnp.float32), axis=-1)
    g_gate, g_idx = lax.top_k(g_prob, 1)
    e_logits = (xf @ w_re).astype(jnp.float32).reshape(t, N_GROUPS, EXPERTS_PER_GROUP)
    e_logits = jnp.take_along_axis(e_logits, g_idx[:, :, None], axis=1)[:, 0]
    e_w, e_i = lax.top_k(jax.nn.softmax(e_logits, axis=-1), TOP_K)
    weights = g_gate * (e_w / jnp.sum(e_w, axis=-1, keepdims=True))
    experts = (g_idx * EXPERTS_PER_GROUP + e_i).astype(jnp.int32)
    n = t * TOP_K
    slot_e = experts.reshape(n)
    slot_tok = jnp.repeat(jnp.arange(t, dtype=jnp.int32), TOP_K)
    slot_w = weights.reshape(n)
    order = jnp.argsort(slot_e)
    se = slot_e[order]
    counts = jnp.zeros((N_EXPERTS,), jnp.int32).at[slot_e].add(1)
    starts = jnp.cumsum(counts) - counts
    padded = (counts + MOE_BLOCK - 1) // MOE_BLOCK * MOE_BLOCK
    pad_end = jnp.cumsum(padded)
    pad_start = pad_end - padded
    dest = pad_start[se] + jnp.arange(n, dtype=jnp.int32) - starts[se]
    n_blocks = n // MOE_BLOCK + N_EXPERTS
    p_len = n_blocks * MOE_BLOCK
    pad_tok = jnp.full((p_len,), t, jnp.int32).at[dest].set(slot_tok[order])
    pad_w = jnp.zeros((p_len,), jnp.float32).at[dest].set(slot_w[order])
    block_e = jnp.minimum(jnp.searchsorted(pad_end, jnp.arange(n_blocks, dtype=jnp.int32) * MOE_BLOCK,
                                           side='right'), N_EXPERTS - 1).astype(jnp.int32)
    xf_pad = jnp.concatenate([xf, jnp.zeros((1, dm), xf.dtype)], axis=0)
    xs = xf_pad[pad_tok].reshape(n_blocks, MOE_BLOCK, dm)

    def expert_block(args):
        xb, e = args
        hdn = jax.nn.silu(xb @ w1[e]) * (xb @ w3[e])
        return hdn @ w2[e]

    ys = lax.map(expert_block, (xs, block_e)).reshape(p_len, dm)
    out = jnp.zeros((t + 1, dm), ys.dtype).at[pad_tok].add(ys * pad_w[:, None].astype(ys.dtype))
    return out[:t].reshape(b, s, dm).astype(x.dtype)


def setup_inputs(seed: int = 0) -> dict:
    key = jax.random.key(seed)
    ks = jax.random.split(key, 25)
    f = jnp.float32

    def nrm(k, shape, scale):
        return jax.random.normal(k, shape, f) * scale

    return {
        'x': nrm(ks[0], (BATCH, SEQ, D_MODEL), 1.0),
        'p': nrm(ks[1], (DEPTH, BATCH, SEQ, D_PLE), 1.0),
        'g_mix': 1.0 + nrm(ks[2], (DEPTH, D_MODEL), 0.02),
        'w_in': nrm(ks[3], (DEPTH, D_MODEL, D_IN), D_MODEL ** -0.5),
        'g_qa': 1.0 + nrm(ks[4], (DEPTH, DA_QK), 0.02),
        'g_ka': 1.0 + nrm(ks[5], (DEPTH, DA_QK), 0.02),
        'lam_q1': nrm(ks[6], (DEPTH, DA_QK), 0.1),
        'lam_k1': nrm(ks[7], (DEPTH, DA_QK), 0.1),
        'lam_q2': nrm(ks[8], (DEPTH, DA_QK), 0.1),
        'lam_k2': nrm(ks[9], (DEPTH, DA_QK), 0.1),
        'g_sub': 1.0 + nrm(ks[10], (DEPTH, DA_V), 0.02),
        'g_qb': 1.0 + nrm(ks[11], (DEPTH, DB_H), 0.02),
        'g_kb': 1.0 + nrm(ks[12], (DEPTH, DB_H), 0.02),
        'rpb': nrm(ks[13], (DEPTH, HB, 2 * WIN_R - 1, 2 * WIN_C - 1), 0.1),
        'w_out': nrm(ks[14], (DEPTH, D_MIX, D_MODEL), D_MIX ** -0.5),
        'g_ffn': 1.0 + nrm(ks[15], (DEPTH, D_MODEL), 0.02),
        'w_rg': nrm(ks[16], (DEPTH, D_MODEL, N_GROUPS), D_MODEL ** -0.5),
        'w_re': nrm(ks[17], (DEPTH, D_MODEL, N_EXPERTS), D_MODEL ** -0.5),
        'w1': nrm(ks[18], (DEPTH, N_EXPERTS, D_MODEL, D_EXPERT), D_MODEL ** -0.5),
        'w3': nrm(ks[19], (DEPTH, N_EXPERTS, D_MODEL, D_EXPERT), D_MODEL ** -0.5),
        'w2': nrm(ks[20], (DEPTH, N_EXPERTS, D_EXPERT, D_MODEL), D_EXPERT ** -0.5),
        'g_plg': 1.0 + nrm(ks[21], (DEPTH, D_MODEL), 0.02),
        'w_plg': nrm(ks[22], (DEPTH, D_MODEL, D_MODEL), D_MODEL ** -0.5),
        'w_ple': nrm(ks[23], (DEPTH, D_PLE, D_MODEL), D_PLE ** -0.5),
        'g_ple': 1.0 + nrm(ks[24], (DEPTH, D_MODEL), 0.02),
    }


def reference(x, p, g_mix, w_in, g_qa, g_ka, lam_q1, lam_k1, lam_q2, lam_k2, g_sub, g_qb, g_kb,
              rpb, w_out, g_ffn, w_rg, w_re, w1, w3, w2, g_plg, w_plg, w_ple, g_ple):
    b, s, _ = x.shape
    h = x
    for i in range(DEPTH):
        lam_init = 0.8 - 0.6 * math.exp(-0.3 * i)
        a = rms_norm(h, g_mix[i])
        proj = a @ w_in[i]
        qa, ka, va, qb, kb, vb = jnp.split(proj, np.cumsum([DA, DA, DA, DB, DB])[:5].tolist(), axis=-1)
        oa = diff_attention(qa.reshape(b, s, HA, 2, DA_QK), ka.reshape(b, s, HA, 2, DA_QK),
                            va.reshape(b, s, HA, DA_V), g_qa[i], g_ka[i],
                            lam_q1[i], lam_k1[i], lam_q2[i], lam_k2[i], g_sub[i], lam_init)
        ob = neighbourhood_attention(qb.reshape(b, s, HB, DB_H), kb.reshape(b, s, HB, DB_H),
                                     vb.reshape(b, s, HB, DB_H), g_qb[i], g_kb[i], rpb[i])
        h = h + jnp.concatenate([oa, ob], axis=-1) @ w_out[i]
        h = h + hier_moe(rms_norm(h, g_ffn[i]), w_rg[i], w_re[i], w1[i], w3[i], w2[i])
        h = h + rms_norm(p[i] @ w_ple[i], g_ple[i]) * jax.nn.sigmoid(rms_norm(h, g_plg[i]) @ w_plg[i])
    return h
```

```python
import math
import numpy as np
import concourse.bass as bass
import concourse.mybir as mybir
from contextlib import ExitStack
from concourse.bass_utils import run_bass_kernel_spmd

F32 = mybir.dt.float32
BF16 = mybir.dt.bfloat16
AF = mybir.ActivationFunctionType
ALU = mybir.AluOpType
AX = mybir.AxisListType

COMPUTE = ("pe", "act", "dve", "pool")
ALL_ENG = ("pe", "act", "dve", "pool", "sp")
N_DMA_SEMS = 24
EPS = 1e-6
NEG = -1e30
NT_OWN = 16
NT_ALL = 32


class Tok:
    __slots__ = ("eng", "idx", "needed", "sem", "val")

    def __init__(self, eng, idx):
        self.eng = eng
        self.idx = idx
        self.needed = False
        self.sem = None
        self.val = None


class Buf:
    __slots__ = ("name", "w", "r")

    def __init__(self, name=""):
        self.name = name
        self.w = None
        self.r = {}


class Op:
    __slots__ = ("fn", "waits", "tok")

    def __init__(self, fn, waits, tok):
        self.fn = fn
        self.waits = waits
        self.tok = tok


class Sched:
    def __init__(self, nc, es):
        self.nc = nc
        self.ops = {e: [] for e in ALL_ENG}
        self.waited = {e: {} for e in ALL_ENG}
        self.eng_sem = {e: es.enter_context(nc.semaphore("s_" + e)) for e in COMPUTE}
        self.dma_sems = [es.enter_context(nc.semaphore("s_dma%d" % i)) for i in range(N_DMA_SEMS)]
        self.dma_cnt = [0] * N_DMA_SEMS
        self.dma_last = [None] * N_DMA_SEMS
        self.dma_rr = 0
        self.dma_rr_sw = 0
        self.dma_rr_sw = 0
        self.n_dma = 0
        self.last_tok = {e: None for e in COMPUTE}

    def _need(self, eng, t, out):
        wd = self.waited[eng]
        if t.eng in COMPUTE:
            key = t.eng
            if wd.get(key, -1) >= t.idx:
                return
            wd[key] = t.idx
        else:
            key = t.sem
            if wd.get(key, -1) >= t.val:
                return
            wd[key] = t.val
        t.needed = True
        out.append(t)

    def _collect(self, eng, reads, writes, is_dma):
        out = []
        for b in reads:
            t = b.w
            if t is not None:
                if t.eng == eng and not is_dma and eng == "pe":
                    continue
                self._need(eng, t, out)
        for b in writes:
            t = b.w
            if t is not None:
                if not (t.eng == eng and not is_dma and eng == "pe"):
                    self._need(eng, t, out)
            for t in b.r.values():
                if t.eng == eng and not is_dma and eng == "pe":
                    continue
                self._need(eng, t, out)
        return out

    def op(self, eng, fn, reads=(), writes=()):
        waits = self._collect(eng, reads, writes, False)
        tok = Tok(eng, len(self.ops[eng]))
        self.ops[eng].append(Op(fn, waits, tok))
        self.last_tok[eng] = tok
        for b in reads:
            b.r[eng] = tok
        for b in writes:
            b.w = tok
            b.r = {}
        return tok

    def dma(self, queue, fn, reads=(), writes=()):
        waits = self._collect(queue, reads, writes, True)
        if queue == "pool":
            i = 16 + self.dma_rr_sw
            self.dma_rr_sw = (self.dma_rr_sw + 1) % (N_DMA_SEMS - 16)
        else:
            i = self.dma_rr
            self.dma_rr = (self.dma_rr + 1) % 16
        prev = self.dma_last[i]
        if prev is not None:
            self._need(queue, prev, waits)
        self.dma_cnt[i] += 1
        tok = Tok("dma", self.n_dma)
        self.n_dma += 1
        tok.sem = self.dma_sems[i]
        tok.val = 16 * self.dma_cnt[i]
        tok.needed = True
        self.dma_last[i] = tok
        self.ops[queue].append(Op(fn, waits, tok))
        for b in reads:
            b.r[("dma", tok.idx)] = tok
        for b in writes:
            b.w = tok
            b.r = {}
        return tok

    def barrier(self):
        toks = [self.last_tok[e] for e in COMPUTE if self.last_tok[e] is not None]
        toks += [t for t in self.dma_last if t is not None]
        for e in ALL_ENG:
            waits = []
            for t in toks:
                if t.eng == e:
                    continue
                self._need(e, t, waits)
            if waits:
                self.ops[e].append(Op(None, waits, None))

    def emit(self):
        nc = self.nc
        for e in COMPUTE:
            c = 0
            for o in self.ops[e]:
                t = o.tok
                if t is not None and t.eng == e and t.needed:
                    c += 1
                    t.sem = self.eng_sem[e]
                    t.val = c

        def run(e, eng):
            for o in self.ops[e]:
                for t in o.waits:
                    eng.wait_ge(t.sem, t.val)
                if o.fn is None:
                    continue
                ins = o.fn(eng)
                t = o.tok
                if t.eng == "dma":
                    ins.then_inc(t.sem, 16)
                elif t.needed:
                    ins.then_inc(t.sem, 1)

        with nc.Block() as block:
            @block.tensor
            def _(eng):
                run("pe", eng)

            @block.scalar
            def _(eng):
                run("act", eng)

            @block.vector
            def _(eng):
                run("dve", eng)

            @block.gpsimd
            def _(eng):
                run("pool", eng)

            @block.sync
            def _(eng):
                run("sp", eng)


class K:
    def __init__(self, S):
        self.S = S

    def mm(self, out, lhsT, rhs, start, stop, R, W, tp=None):
        if tp is None:
            f = lambda e: e.matmul(out, lhsT=lhsT, rhs=rhs, start=start, stop=stop)
        else:
            f = lambda e: e.matmul(out, lhsT=lhsT, rhs=rhs, start=start, stop=stop, tile_position=tp)
        return self.S.op("pe", f, R, W)

    def tr(self, out, in_, ident, R, W):
        return self.S.op("pe", lambda e: e.transpose(out=out, in_=in_, identity=ident), R, W)

    def act(self, out, in_, func, R, W, scale=1.0, bias=0.0, accum=None):
        if accum is None:
            f = lambda e: e.activation(out=out, in_=in_, func=func, bias=bias, scale=scale)
        else:
            f = lambda e: e.activation(out=out, in_=in_, func=func, bias=bias, scale=scale, accum_out=accum)
        return self.S.op("act", f, R, W)

    def tt(self, eng, out, in0, in1, op, R, W):
        return self.S.op(eng, lambda e: e.tensor_tensor(out=out, in0=in0, in1=in1, op=op), R, W)

    def ts(self, eng, out, in0, s1, op0, R, W, s2=None, op1=None):
        if op1 is None:
            f = lambda e: e.tensor_scalar(out=out, in0=in0, scalar1=s1, scalar2=None, op0=op0)
        else:
            f = lambda e: e.tensor_scalar(out=out, in0=in0, scalar1=s1, scalar2=s2, op0=op0, op1=op1)
        return self.S.op(eng, f, R, W)

    def stt(self, eng, out, in0, scalar, in1, op0, op1, R, W):
        return self.S.op(eng, lambda e: e.scalar_tensor_tensor(out=out, in0=in0, scalar=scalar, in1=in1, op0=op0, op1=op1), R, W)

    def rsum(self, out, in_, R, W):
        return self.S.op("dve", lambda e: e.reduce_sum(out=out, in_=in_, axis=AX.X), R, W)

    def rmax(self, out, in_, R, W):
        return self.S.op("dve", lambda e: e.reduce_max(out=out, in_=in_, axis=AX.X), R, W)

    def recip(self, out, in_, R, W):
        return self.S.op("dve", lambda e: e.reciprocal(out=out, in_=in_), R, W)

    def copy(self, eng, out, in_, R, W):
        if eng == "act":
            return self.S.op("act", lambda e: e.activation(out=out, in_=in_, func=AF.Copy), R, W)
        return self.S.op(eng, lambda e: e.tensor_copy(out=out, in_=in_), R, W)

    def memset(self, eng, out, val, W):
        return self.S.op(eng, lambda e: e.memset(out, val), (), W)

    def dma(self, queue, out, in_, R, W):
        return self.S.dma(queue, lambda e: e.dma_start(out=out, in_=in_), R, W)


def bc(ap, shape):
    return ap.broadcast_to(shape)


NA_OFFS = {0: [-2, -1, 0, 1, 2, 3], 1: [-2, -1, 0, 1, 2], 14: [-2, -1, 0, 1, 2], 15: [-3, -2, -1, 0, 1, 2]}
NA_GEN = [-2, -1, 0, 1, 2]
NA_SPECIAL = [0, 1, 14, 15]


PHASES = ["A1", "B1", "A2", "B2", "C", "D1", "D2", "E"]


def build_program(stop=None):
    nc = bass.Bass("TRN2", target_bir_lowering=False)
    dr = {}
    last = len(PHASES) - 1 if stop is None else PHASES.index(stop)

    def on(ph):
        return PHASES.index(ph) <= last

    def din(name, shape):
        dr[name] = nc.dram_tensor(name, list(shape), F32, kind="ExternalInput")
        return dr[name]

    din("x", [4096, 1024])
    din("p", [2048, 256])
    din("cs", [128, 32 * 64])
    din("sn", [128, 32 * 64])
    din("w_in", [1024, 3072])
    din("w_out", [1024, 1024])
    din("w_plg", [1024, 1024])
    din("w_ple", [256, 1024])
    din("w_r", [1024, 36])
    din("w1", [32, 1024, 512])
    din("w3", [32, 1024, 512])
    din("w2", [32, 512, 1024])
    for n in ("g_mix", "g_ffn", "g_plg", "g_ple"):
        din(n, [1, 1024])
    for n in ("g_qa", "g_ka", "lam_q1", "lam_k1", "lam_q2", "lam_k2"):
        din(n, [1, 64])
    din("g_sub", [1, 128])
    din("g_qb", [1, 32])
    din("g_kb", [1, 32])
    din("ident", [128, 128])
    din("biasG", [4, 128, 4 * 5 * 128])
    din("biasS", [4, 4, 128, 4 * 6 * 128])
    y = nc.dram_tensor("y", [2048, 1024], F32, kind="ExternalOutput")

    x_ap = dr["x"].ap()
    p_ap = dr["p"].ap()
    y_ap = y.ap()

    def bvec(name, n):
        return bass.AP(dr[name], 0, [[0, 128], [1, n]])

    with ExitStack() as es:
        S = Sched(nc, es)
        k = K(S)

        uid = [0]

        def sb(stack, name, shape, dt):
            uid[0] += 1
            return stack.enter_context(nc.sbuf_tensor("%s_%d" % (name, uid[0]), shape, dt))

        def ps(stack, name, shape, dt):
            uid[0] += 1
            return stack.enter_context(nc.psum_tensor("%s_%d" % (name, uid[0]), shape, dt))

        dbg_toks = []

        def dump(ph, name, tens, shape, dt):
            if stop != ph:
                return
            S.barrier()
            d = nc.dram_tensor("dbg_" + name, list(shape), dt, kind="ExternalOutput")
            dbg_toks.append(k.dma("sp", d.ap(), tens, [], ()))

        idf = sb(es, "idf", [128, 128], F32)
        idb = sb(es, "idb", [128, 128], BF16)
        o_cat = sb(es, "o_cat", [128, NT_OWN, 1024], BF16)
        B_id = Buf("id")
        B_ocat = [[Buf("ocatA%d" % t), Buf("ocatB%d" % t)] for t in range(NT_OWN)]
        k.dma("sp", idf[:], dr["ident"].ap(), (), [B_id])
        k.copy("dve", idb[:], idf[:], [B_id], [B_id])

        def pipeline(n, stages, offs):
            for T in range(n + max(offs)):
                for s_ in reversed(range(len(stages))):
                    i = T - offs[s_]
                    if 0 <= i < n:
                        stages[s_](i)

        def head_norm(raw, ngrp, gd, gtile, wk, BBw, B_raw, eng2="pool"):
            n = ngrp * gd
            v3 = lambda ap: ap[:, 0:n].rearrange("p (a b) -> p a b", b=gd)
            return [
                lambda: k.tt(eng2, wk["sq"][:, 0:n], raw[:, 0:n], raw[:, 0:n], ALU.mult, [B_raw], [BBw["sq"]]),
                lambda: k.rsum(wk["ss"][:, 0:ngrp], v3(wk["sq"]), [BBw["sq"]], [BBw["ss"]]),
                lambda: k.act(wk["ss"][:, 16:16 + ngrp], wk["ss"][:, 0:ngrp], AF.Sqrt, [BBw["ss"]], [BBw["ss"]],
                              scale=1.0 / gd, bias=EPS),
                lambda: k.recip(wk["ss"][:, 32:32 + ngrp], wk["ss"][:, 16:16 + ngrp], [BBw["ss"]], [BBw["ss"]]),
                lambda: k.tt("dve", v3(wk["kn"]), v3(raw), bc(wk["ss"][:, 32:32 + ngrp].unsqueeze(2), [128, ngrp, gd]),
                             ALU.mult, [B_raw, BBw["ss"]], [BBw["kn"]]),
                lambda: k.tt(eng2, v3(wk["kn"]), v3(wk["kn"]), bc(gtile[:].unsqueeze(1), [128, ngrp, gd]), ALU.mult,
                             [BBw["kn"], BBw["gv"]], [BBw["kn"]]),
            ]

        def proj_phase(s1, kind, tiles, col0, QT, KT, Vst, B_QT, B_KT, B_V):
            da = kind == "da"
            ngrp, gd = (8, 64) if da else (16, 32)
            w_sb = sb(s1, "w_in_" + kind, [128, 8, 1536], BF16)
            xt = [sb(s1, "xt%d" % i, [128, 1024], F32) for i in range(2)]
            a_bf = [sb(s1, "abf%d" % i, [128, 1024], BF16) for i in range(2)]
            junk = sb(s1, "junk", [128, 1024], BF16)
            stat = [sb(s1, "stat%d" % i, [128, 4], F32) for i in range(2)]
            aT = [sb(s1, "aT%d" % i, [128, 8, 128], BF16) for i in range(2)]
            gmix = sb(s1, "gmix", [128, 1024], F32)
            gq = sb(s1, "gq", [128, gd], F32)
            gk = sb(s1, "gk", [128, gd], F32)
            raw = [[sb(s1, "raw%d_%d" % (i, c), [128, 512], F32) for c in range(2)] for i in range(2)]
            knb = [[sb(s1, "knb%d_%d" % (i, c), [128, 512], BF16) for c in range(2)] for i in range(2)]
            names = ("sq", "kn", "A", "Bt") if da else ("sq", "kn")
            wks = []
            for c in range(2):
                wks.append({n: sb(s1, "wk_%s%d" % (n, c), [128, 512], F32) for n in names})
                wks[c]["ss"] = sb(s1, "wk_ss%d" % c, [128, 48], F32)
            if da:
                cs_sb = sb(s1, "cs_sb", [128, 32, 64], F32)
                sn_sb = sb(s1, "sn_sb", [128, 32, 64], F32)
            pT = [ps(s1, "pT%d" % i, [128, 1024], BF16) for i in range(2)]
            pP = [ps(s1, "pP%d" % i, [128, 512], F32) for i in range(3)]
            pT2 = [ps(s1, "pT2%d" % i, [128, 1024], BF16) for i in range(2)]
            B_xt, B_st, B_a, B_pT, B_aT = ([Buf(), Buf()] for _ in range(5))
            B_g, B_tab, B_gq, B_gk = Buf(), Buf(), Buf(), Buf()
            B_w = [Buf() for _ in range(3)]
            B_pP = [Buf() for _ in range(3)]
            B_pT2 = [Buf(), Buf()]
            B_raw = [[Buf(), Buf()], [Buf(), Buf()]]
            B_knb = [[Buf(), Buf()], [Buf(), Buf()]]
            BW = [{n: Buf() for n in ("sq", "ss", "kn", "A", "Bt")} for _ in range(2)]
            BW[0]["gv"], BW[1]["gv"] = B_gq, B_gk
            w_in_v = dr["w_in"].ap().rearrange("(kc p) n -> p kc n", p=128)
            order = [1, 2, 0]
            for cg in order:
                k.dma("pool", w_sb[:, :, cg * 512:(cg + 1) * 512], w_in_v[:, :, col0 + cg * 512:col0 + (cg + 1) * 512], (), [B_w[cg]])
            k.dma("sp", gmix[:], bvec("g_mix", 1024), (), [B_g])
            k.dma("sp", gq[:], bvec("g_qa" if da else "g_qb", gd), (), [B_gq])
            k.dma("sp", gk[:], bvec("g_ka" if da else "g_kb", gd), (), [B_gk])
            if da:
                k.dma("sp", cs_sb[:].rearrange("p a b -> p (a b)"), dr["cs"].ap(), (), [B_tab])
                k.dma("sp", sn_sb[:].rearrange("p a b -> p (a b)"), dr["sn"].ap(), (), [B_tab])
            cnt = {"pp": 0, "p2": 0}

            def groups(i):
                return ([0] if tiles[i] < NT_OWN else []) + [1, 2]

            def s0(i):
                t, par = tiles[i], i % 2
                k.dma("sp", xt[par][:], x_ap[t * 128:(t + 1) * 128, :], (), [B_xt[par]])
                k.act(junk[:], xt[par][:], AF.Square, [B_xt[par]], [B_st[par]], accum=stat[par][:, 0:1])
                k.act(stat[par][:, 1:2], stat[par][:, 0:1], AF.Sqrt, [B_st[par]], [B_st[par]], scale=1.0 / 1024, bias=EPS)
                k.recip(stat[par][:, 2:3], stat[par][:, 1:2], [B_st[par]], [B_st[par]])
                k.stt("dve", a_bf[par][:], xt[par][:], stat[par][:, 2:3], gmix[:], ALU.mult, ALU.mult,
                      [B_xt[par], B_st[par], B_g], [B_a[par]])

            def s1_(i):
                par = i % 2
                for kc in range(8):
                    k.tr(pT[par][:, kc * 128:(kc + 1) * 128], a_bf[par][:, kc * 128:(kc + 1) * 128], idb[:],
                         [B_a[par], B_id], [B_pT[par]])
                k.copy("act", aT[par][:].rearrange("p a b -> p (a b)"), pT[par][:], [B_pT[par]], [B_aT[par]])

            def s2_(i):
                t, par = tiles[i], i % 2
                for cg in groups(i):
                    pp = cnt["pp"] % 3
                    cnt["pp"] += 1
                    for kc in range(8):
                        k.mm(pP[pp][:], aT[par][:, kc, :], w_sb[:, kc, cg * 512:(cg + 1) * 512], kc == 0, kc == 7,
                             [B_aT[par], B_w[cg]], [B_pP[pp]])
                    if cg == 2:
                        if da:
                            k.copy("act", Vst[:, t, :, 0:128], pP[pp][:].rearrange("p (a b) -> p a b", b=128), [B_pP[pp]], [B_V[t]])
                        else:
                            k.copy("act", Vst[:, i, :, 0:32], pP[pp][:].rearrange("p (a b) -> p a b", b=32), [B_pP[pp]], [B_V[i]])
                    else:
                        k.copy("dve", raw[par][cg][:], pP[pp][:], [B_pP[pp]], [B_raw[par][cg]])

            def s3_chain(i, cg):
                t, par = tiles[i], i % 2
                wk, bw = wks[cg], BW[cg]
                ch = head_norm(raw[par][cg], ngrp, gd, gq if cg == 0 else gk, wk, bw, B_raw[par][cg])
                if da:
                    kn3 = wk["kn"][:].rearrange("p (a b) -> p a b", b=64)
                    A3 = wk["A"][:].rearrange("p (a b) -> p a b", b=64)
                    Bt3 = wk["Bt"][:].rearrange("p (a b) -> p a b", b=64)
                    ch += [
                        lambda: k.tt("dve", A3, kn3, bc(cs_sb[:, t, :].unsqueeze(1), [128, 8, 64]), ALU.mult,
                                     [bw["kn"], B_tab], [bw["A"]]),
                        lambda: k.tt("pool", Bt3[:, :, 0:32], kn3[:, :, 32:64], bc(sn_sb[:, t, 0:32].unsqueeze(1), [128, 8, 32]),
                                     ALU.mult, [bw["kn"], B_tab], [bw["Bt"]]),
                        lambda: k.tt("pool", Bt3[:, :, 32:64], kn3[:, :, 0:32], bc(sn_sb[:, t, 32:64].unsqueeze(1), [128, 8, 32]),
                                     ALU.mult, [bw["kn"], B_tab], [bw["Bt"]]),
                        lambda: k.tt("dve", knb[par][cg][:], wk["A"][:], wk["Bt"][:], ALU.add, [bw["A"], bw["Bt"]],
                                     [B_knb[par][cg]]),
                    ]
                else:
                    ch.append(lambda: k.copy("dve", knb[par][cg][:], wk["kn"][:], [bw["kn"]], [B_knb[par][cg]]))
                return ch

            def s3_(i):
                chains = [s3_chain(i, cg) for cg in groups(i) if cg != 2]
                for j in range(max(len(c) for c in chains)):
                    for c in chains:
                        if j < len(c):
                            c[j]()

            def s4_(i):
                t, par = tiles[i], i % 2
                for cg in groups(i):
                    if cg == 2:
                        continue
                    p2 = cnt["p2"] % 2
                    cnt["p2"] += 1
                    for hh in range(4):
                        k.tr(pT2[p2][:, hh * 128:(hh + 1) * 128], knb[par][cg][:, hh * 128:(hh + 1) * 128], idb[:],
                             [B_knb[par][cg], B_id], [B_pT2[p2]])
                    src = pT2[p2][:, 0:512].rearrange("p (a b) -> p a b", b=128)
                    if cg == 0:
                        k.copy("act", QT[:, :, t * 128:(t + 1) * 128], src, [B_pT2[p2]], [B_QT[t]])
                    else:
                        kt_ = t if da else i
                        k.copy("act", KT[:, :, kt_ * 128:(kt_ + 1) * 128], src, [B_pT2[p2]], [B_KT[kt_]])

            pipeline(len(tiles), [s0, s1_, s2_, s3_, s4_], [0, 1, 2, 3, 4])

        with ExitStack() as sa:
            QaT = sb(sa, "QaT", [128, 4, 2048], BF16)
            KaT = sb(sa, "KaT", [128, 4, 4096], BF16)
            Va = sb(sa, "Va", [128, NT_ALL, 4, 129], BF16)
            B_QaT = [Buf("QaT%d" % t) for t in range(NT_OWN)]
            B_KaT = [Buf("KaT%d" % t) for t in range(NT_ALL)]
            B_Va = [Buf("Va%d" % t) for t in range(NT_ALL)]
            k.memset("pool", Va[:], 1.0, B_Va)

            for _once in ([0] if on("A1") else []):
                with ExitStack() as s1:
                    proj_phase(s1, "da", list(range(NT_ALL)), 0, QaT, KaT, Va, B_QaT, B_KaT, B_Va)
                    dump("A1", "QaT", QaT[:], [128, 4, 2048], BF16)
                    dump("A1", "KaT", KaT[:], [128, 4, 4096], BF16)
                    dump("A1", "Va", Va[:], [128, NT_ALL, 4, 129], BF16)
                    S.barrier()

            for _once in ([0] if on("B1") else []):
                with ExitStack() as s2:
                    NB = 4
                    NBP = 8
                    Pt = [sb(s2, "Pt%d" % i, [128, 512], BF16) for i in range(NBP)]
                    Osb = [sb(s2, "Osb%d" % i, [128, 132], F32) for i in range(4)]
                    t1 = sb(s2, "t1", [128, 4, 128], F32)
                    ob = sb(s2, "ob", [128, 4, 128], F32)
                    sqb = sb(s2, "sqb", [128, 128], F32)
                    sm = sb(s2, "sm", [128, 4, 8], F32)
                    ssq = sb(s2, "ssq", [128, 16], F32)
                    lamb = sb(s2, "lamb", [128, 4, 64], F32)
                    lamw = sb(s2, "lamw", [128, 2, 64], F32)
                    lams = sb(s2, "lams", [128, 8], F32)
                    gsub = sb(s2, "gsub", [128, 128], F32)
                    pS = [ps(s2, "pS%d" % i, [128, 512], F32) for i in range(NB)]
                    pO = [ps(s2, "pO%d" % i, [128, 512], F32) for i in range(4)]
                    B_Pt = [Buf() for _ in range(NBP)]
                    B_pS = [Buf() for _ in range(NB)]
                    B_pO = [Buf() for _ in range(4)]
                    B_Osb = [Buf() for _ in range(4)]
                    B_t1 = [Buf() for _ in range(4)]
                    B_ob = [Buf() for _ in range(4)]
                    B_sm = [Buf() for _ in range(4)]
                    B_lam, B_gs, B_ssq, B_sqb = Buf(), Buf(), Buf(), Buf()
                    for i, n in enumerate(("lam_q1", "lam_k1", "lam_q2", "lam_k2")):
                        k.dma("sp", lamb[:, i, :], bvec(n, 64), (), [B_lam])
                    k.dma("sp", gsub[:], bvec("g_sub", 128), (), [B_gs])
                    k.ts("dve", gsub[:], gsub[:], 0.8, ALU.mult, [B_gs], [B_gs])
                    k.tt("dve", lamw[:, 0, :], lamb[:, 0, :], lamb[:, 1, :], ALU.mult, [B_lam], [B_lam])
                    k.tt("dve", lamw[:, 1, :], lamb[:, 2, :], lamb[:, 3, :], ALU.mult, [B_lam], [B_lam])
                    k.rsum(lams[:, 0:2], lamw[:], [B_lam], [B_lam])
                    k.act(lams[:, 2:4], lams[:, 0:2], AF.Exp, [B_lam], [B_lam])
                    k.tt("dve", lams[:, 4:5], lams[:, 3:4], lams[:, 2:3], ALU.subtract, [B_lam], [B_lam])
                    k.ts("dve", lams[:, 5:6], lams[:, 4:5], -0.2, ALU.add, [B_lam], [B_lam])
                    items = [(h, qg, comp, kt) for h in range(4) for qg in range(4) for comp in range(2) for kt in range(NT_ALL)]
                    QaZ = [sb(s2, "QaZ%d" % c, [128, 4, 2048], BF16) for c in range(2)]
                    B_QaZ = [[Buf() for _ in range(4)] for _ in range(2)]
                    for c in range(2):
                        zr = slice(64, 128) if c == 0 else slice(0, 64)
                        cr = slice(0, 64) if c == 0 else slice(64, 128)
                        k.memset("pool", QaZ[c][zr], 0.0, B_QaZ[c])
                        for qg_ in range(4):
                            k.copy("dve" if c == 0 else "pool", QaZ[c][cr, :, qg_ * 512:(qg_ + 1) * 512],
                                   QaT[cr, :, qg_ * 512:(qg_ + 1) * 512], B_QaT[qg_ * 4:qg_ * 4 + 4], [B_QaZ[c][qg_]])

                    def b1_s0(i):
                        h, qg, comp, kt = items[i]
                        s = i % NB
                        k.mm(pS[s][:], KaT[:, h, kt * 128:(kt + 1) * 128], QaZ[comp][:, h, qg * 512:(qg + 1) * 512],
                             True, True, [B_KaT[kt], B_QaZ[comp][qg]], [B_pS[s]])
                        k.act(Pt[i % NBP][:], pS[s][:], AF.Exp, [B_pS[s]], [B_Pt[i % NBP]], scale=0.125)

                    def b1_s1(i):
                        h, qg, comp, kt = items[i]
                        s = i % NBP
                        for qs in range(4):
                            k.mm(pO[qs][:, 0:129], Pt[s][:, qs * 128:(qs + 1) * 128], Va[:, kt, h, :],
                                 kt == 0, kt == NT_ALL - 1, [B_Pt[s], B_Va[kt]], [B_pO[qs]])
                        if kt != NT_ALL - 1:
                            return
                        for qs in range(4):
                            k.copy("dve", Osb[qs][:, 0:129], pO[qs][:, 0:129], [B_pO[qs]], [B_Osb[qs]])
                        for qs in range(4):
                            O = Osb[qs]
                            bo = B_Osb[qs]
                            if comp == 0:
                                k.recip(sm[:, qs, 0:1], O[:, 128:129], [bo], [B_sm[qs]])
                                k.ts("dve", t1[:, qs, :], O[:, 0:128], sm[:, qs, 0:1], ALU.mult, [bo, B_sm[qs]], [B_t1[qs]])
                            else:
                                k.recip(sm[:, qs, 1:2], O[:, 128:129], [bo], [B_sm[qs]])
                                k.tt("dve", sm[:, qs, 2:3], sm[:, qs, 1:2], lams[:, 5:6], ALU.mult, [B_sm[qs], B_lam], [B_sm[qs]])
                                k.stt("dve", ob[:, qs, :], O[:, 0:128], sm[:, qs, 2:3], t1[:, qs, :], ALU.mult, ALU.add,
                                      [bo, B_sm[qs], B_t1[qs]], [B_ob[qs]])
                                k.tt("dve", sqb[:], ob[:, qs, :], ob[:, qs, :], ALU.mult, [B_ob[qs]], [B_sqb])
                                k.rsum(ssq[:, qs:qs + 1], sqb[:], [B_sqb], [B_ssq])
                        if comp == 1:
                            k.act(ssq[:, 4:8], ssq[:, 0:4], AF.Sqrt, [B_ssq], [B_ssq], scale=1.0 / 128, bias=EPS)
                            k.recip(ssq[:, 8:12], ssq[:, 4:8], [B_ssq], [B_ssq])
                            for qs in range(4):
                                tq = qg * 4 + qs
                                k.stt("dve", o_cat[:, tq, h * 128:(h + 1) * 128], ob[:, qs, :], ssq[:, 8 + qs:9 + qs], gsub[:],
                                      ALU.mult, ALU.mult, [B_ob[qs], B_ssq, B_gs], [B_ocat[tq][0]])

                    pipeline(len(items), [b1_s0, b1_s1], [0, 6])
                    dump("B1", "ocat", o_cat[:], [128, NT_OWN, 1024], BF16)
                    S.barrier()

        with ExitStack() as sa:
            QbT = sb(sa, "QbT", [128, 4, 2048], BF16)
            KbT = sb(sa, "KbT", [128, 4, 20 * 128], BF16)
            Vb = sb(sa, "Vb", [128, 20, 16, 33], BF16)
            B_QbT = [Buf() for _ in range(NT_OWN)]
            B_KbT = [Buf() for _ in range(20)]
            B_Vb = [Buf() for _ in range(20)]
            k.memset("pool", Vb[:], 1.0, B_Vb)
            for _once in ([0] if on("A2") else []):
                with ExitStack() as s1:
                    s2t = [30, 31] + list(range(16)) + [16, 17]
                    proj_phase(s1, "na", s2t, 1536, QbT, KbT, Vb, B_QbT, B_KbT, B_Vb)
                    dump("A2", "QbT", QbT[:], [128, 4, 2048], BF16)
                    dump("A2", "KbT", KbT[:], [128, 4, 2560], BF16)
                    dump("A2", "Vb", Vb[:], [128, 20, 16, 33], BF16)
                    S.barrier()

            for _once in ([0] if on("B2") else []):
                with ExitStack() as s2:
                    NB = 4
                    biasG = sb(s2, "biasG", [128, 4, 5, 128], F32)
                    biasS = [sb(s2, "biasS%d" % i, [128, 4, 6, 128], F32) for i in range(2)]
                    Sb = [sb(s2, "Sb%d" % i, [128, 768], F32) for i in range(2)]
                    Pb = [sb(s2, "Pb%d" % i, [128, 768], BF16) for i in range(NB)]
                    rl = [sb(s2, "rl%d" % i, [128, 4], F32) for i in range(2)]
                    pS = [ps(s2, "pSb%d" % i, [128, 1024], F32) for i in range(2)]
                    pO = [ps(s2, "pOb%d" % i, [128, 4, 128], F32) for i in range(2)]
                    B_bG = [Buf() for _ in range(4)]
                    B_bS = [Buf(), Buf()]
                    B_Sb = [Buf(), Buf()]
                    B_Pb = [Buf() for _ in range(NB)]
                    B_rl = [Buf(), Buf()]
                    B_pS = [Buf(), Buf()]
                    B_pO = [Buf(), Buf()]
                    scale_b = 32 ** -0.5
                    items = [(g, j, hh) for g in range(4) for j in range(NT_OWN) for hh in range(4)]
                    spec_idx = {}
                    c_ = 0
                    for g in range(4):
                        for j in range(NT_OWN):
                            if j in NA_OFFS:
                                spec_idx[(g, j)] = c_ % 2
                                c_ += 1

                    def b2_s0(i):
                        g, j, hh = items[i]
                        if j == 0 and hh == 0:
                            k.dma("sp", biasG[:].rearrange("p a b c -> p (a b c)"), dr["biasG"].ap()[g], (), [B_bG[0]])
                        if j in NA_OFFS:
                            offs = NA_OFFS[j]
                            sp_i = spec_idx[(g, j)]
                            if hh == 0:
                                k.dma("sp", biasS[sp_i][:].rearrange("p a b c -> p (a b c)"),
                                      dr["biasS"].ap()[NA_SPECIAL.index(j), g], (), [B_bS[sp_i]])
                            btile, bbuf = biasS[sp_i], B_bS[sp_i]
                        else:
                            offs = NA_GEN
                            btile, bbuf = biasG, B_bG[0]
                        nof = len(offs)
                        b2 = i % 2
                        pb = i % NB
                        rows = slice(hh * 32, (hh + 1) * 32)
                        for ci, off in enumerate(offs):
                            s_idx = j + off + 2
                            k.mm(pS[b2][:, ci * 128:(ci + 1) * 128], KbT[rows, g, s_idx * 128:(s_idx + 1) * 128],
                                 QbT[rows, g, j * 128:(j + 1) * 128], True, True,
                                 [B_KbT[s_idx], B_QbT[j]], [B_pS[b2]], tp=(hh * 32, 0))
                        k.stt("dve", Sb[b2][:, 0:nof * 128], pS[b2][:, 0:nof * 128], scale_b,
                              btile[:, hh, 0:nof, :].rearrange("p a b -> p (a b)"), ALU.mult, ALU.add,
                              [B_pS[b2], bbuf], [B_Sb[b2]])
                        k.act(Pb[pb][:, 0:nof * 128], Sb[b2][:, 0:nof * 128], AF.Exp, [B_Sb[b2]], [B_Pb[pb]])

                    def b2_s1(i):
                        g, j, hh = items[i]
                        offs = NA_OFFS.get(j, NA_GEN)
                        nof = len(offs)
                        h = g * 4 + hh
                        pb = i % NB
                        oi = (g * NT_OWN + j) % 2
                        for ci, off in enumerate(offs):
                            s_idx = j + off + 2
                            k.mm(pO[oi][:, hh, 0:33], Pb[pb][:, ci * 128:(ci + 1) * 128], Vb[:, s_idx, h, :],
                                 ci == 0, ci == nof - 1, [B_Pb[pb], B_Vb[s_idx]], [B_pO[oi]])
                        if hh == 3:
                            k.recip(rl[oi][:].unsqueeze(2), pO[oi][:, :, 32:33], [B_pO[oi]], [B_rl[oi]])
                            k.tt("dve", o_cat[:, j, 512 + g * 128:512 + (g + 1) * 128].rearrange("p (a b) -> p a b", b=32),
                                 pO[oi][:, :, 0:32], bc(rl[oi][:].unsqueeze(2), [128, 4, 32]), ALU.mult,
                                 [B_pO[oi], B_rl[oi]], [B_ocat[j][1]])

                    pipeline(len(items), [b2_s0, b2_s1], [0, 3])
                    dump("B2", "ocat", o_cat[:], [128, NT_OWN, 1024], BF16)
                    S.barrier()

        hres = sb(es, "hres", [128, NT_OWN, 1024], F32)
        B_h = [[Buf(), Buf()] for _ in range(NT_OWN)]
        for _once in ([0] if on("C") else []):
            with ExitStack() as s1:
                w_sb = sb(s1, "w_outb", [128, 8, 1024], BF16)
                xt = [sb(s1, "xtc%d" % i, [128, 1024], F32) for i in range(2)]
                oT = [sb(s1, "oT%d" % i, [128, 8, 128], BF16) for i in range(2)]
                pT = [ps(s1, "pTc%d" % i, [128, 1024], BF16) for i in range(2)]
                pP = [ps(s1, "pPc%d" % i, [128, 512], F32) for i in range(3)]
                B_w = Buf()
                B_xt = [Buf(), Buf()]
                B_oT = [Buf(), Buf()]
                B_pT = [Buf(), Buf()]
                B_pP = [Buf() for _ in range(3)]
                k.dma("pool", w_sb[:], dr["w_out"].ap().rearrange("(kc p) n -> p kc n", p=128), (), [B_w])
                cntc = {"pp": 0}

                def c_s0(t):
                    par = t % 2
                    k.dma("sp", xt[par][:], x_ap[t * 128:(t + 1) * 128, :], (), [B_xt[par]])
                    for kc in range(8):
                        k.tr(pT[par][:, kc * 128:(kc + 1) * 128], o_cat[:, t, kc * 128:(kc + 1) * 128], idb[:],
                             [B_ocat[t][0], B_ocat[t][1], B_id], [B_pT[par]])
                    k.copy("act", oT[par][:].rearrange("p a b -> p (a b)"), pT[par][:], [B_pT[par]], [B_oT[par]])

                def c_s1(t):
                    par = t % 2
                    for cg in range(2):
                        pp = cntc["pp"] % 3
                        cntc["pp"] += 1
                        for kc in range(8):
                            k.mm(pP[pp][:], oT[par][:, kc, :], w_sb[:, kc, cg * 512:(cg + 1) * 512], kc == 0, kc == 7,
                                 [B_oT[par], B_w], [B_pP[pp]])
                        k.tt("dve", hres[:, t, cg * 512:(cg + 1) * 512], pP[pp][:], xt[par][:, cg * 512:(cg + 1) * 512], ALU.add,
                             [B_pP[pp], B_xt[par]], [B_h[t][cg]])

                pipeline(NT_OWN, [c_s0, c_s1], [0, 1])
                dump("C", "hres", hres[:], [128, NT_OWN, 1024], F32)
                S.barrier()

        with ExitStack() as sd:
            xnT = sb(sd, "xnT", [128, 8, 2048], BF16)
            gates = sb(sd, "gates", [128, NT_OWN, 32], F32)
            B_xnT = [Buf() for _ in range(NT_OWN)]
            B_gates = [Buf() for _ in range(NT_OWN)]
            for _once in ([0] if on("D1") else []):
                with ExitStack() as s1:
                    gffn = sb(s1, "gffn", [128, 1024], F32)
                    w_r = sb(s1, "w_r", [128, 8, 36], F32)
                    xn = [sb(s1, "xn%d" % i, [128, 1024], F32) for i in range(2)]
                    junk = sb(s1, "junkd", [128, 1024], BF16)
                    stat = [sb(s1, "statd%d" % i, [128, 4], F32) for i in range(2)]
                    xT32 = [sb(s1, "xT32%d" % i, [128, 8, 128], F32) for i in range(2)]
                    rt = [sb(s1, "rt%d" % i, [128, 128], F32) for i in range(2)]
                    pT = [ps(s1, "pTd%d" % i, [128, 1024], F32) for i in range(2)]
                    pL = [ps(s1, "pL%d" % i, [128, 512], F32) for i in range(2)]
                    B_g, B_wr = Buf(), Buf()
                    B_xn = [Buf(), Buf()]
                    B_st = [Buf(), Buf()]
                    B_xT = [Buf(), Buf()]
                    B_rt = [Buf(), Buf()]
                    B_pT = [Buf(), Buf()]
                    B_pL = [Buf(), Buf()]
                    k.dma("sp", gffn[:], bvec("g_ffn", 1024), (), [B_g])
                    k.dma("sp", w_r[:], dr["w_r"].ap().rearrange("(kc p) n -> p kc n", p=128), (), [B_wr])

                    def d1_s0(t):
                        par = t % 2
                        hb = B_h[t]
                        k.act(junk[:], hres[:, t, :], AF.Square, hb, [B_st[par]], accum=stat[par][:, 0:1])
                        k.act(stat[par][:, 1:2], stat[par][:, 0:1], AF.Sqrt, [B_st[par]], [B_st[par]], scale=1.0 / 1024, bias=EPS)
                        k.recip(stat[par][:, 2:3], stat[par][:, 1:2], [B_st[par]], [B_st[par]])
                        k.stt("dve", xn[par][:], hres[:, t, :], stat[par][:, 2:3], gffn[:], ALU.mult, ALU.mult,
                              hb + [B_st[par], B_g], [B_xn[par]])

                    def d1_s1(t):
                        par = t % 2
                        for kc in range(8):
                            k.tr(pT[par][:, kc * 128:(kc + 1) * 128], xn[par][:, kc * 128:(kc + 1) * 128], idf[:],
                                 [B_xn[par], B_id], [B_pT[par]])
                        k.copy("act", xT32[par][:].rearrange("p a b -> p (a b)"), pT[par][:], [B_pT[par]], [B_xT[par], B_pT[par]])
                        k.copy("dve", xnT[:, :, t * 128:(t + 1) * 128], pT[par][:].rearrange("p (a b) -> p a b", b=128),
                               [B_pT[par]], [B_xnT[t]])

                    def d1_s2(t):
                        par = t % 2
                        for kc in range(8):
                            k.mm(pL[par][:, 0:36], xT32[par][:, kc, :], w_r[:, kc, :], kc == 0, kc == 7, [B_xT[par], B_wr], [B_pL[par]])
                        r = rt[par]
                        br = [B_rt[par]]
                        k.copy("dve", r[:, 0:36], pL[par][:, 0:36], [B_pL[par]], br)
                        k.rmax(r[:, 40:41], r[:, 0:4], br, br)
                        k.ts("dve", r[:, 44:48], r[:, 0:4], r[:, 40:41], ALU.is_ge, br, br)
                        k.ts("dve", r[:, 48:52], r[:, 0:4], r[:, 40:41], ALU.subtract, br, br)
                        k.act(r[:, 52:56], r[:, 48:52], AF.Exp, br, br)
                        k.rsum(r[:, 41:42], r[:, 52:56], br, br)
                        k.recip(r[:, 42:43], r[:, 41:42], br, br)
                        k.tt("dve", r[:, 64:96].rearrange("p (e g) -> p e g", g=4),
                             r[:, 4:36].rearrange("p (g e) -> p e g", e=8),
                             bc(r[:, 44:48].unsqueeze(1), [128, 8, 4]), ALU.mult, br, br)
                        k.rsum(r[:, 56:64], r[:, 64:96].rearrange("p (e g) -> p e g", g=4), br, br)
                        k.rmax(r[:, 96:97], r[:, 56:64], br, br)
                        k.ts("dve", r[:, 100:108], r[:, 56:64], r[:, 96:97], ALU.is_ge, br, br)
                        k.stt("dve", r[:, 108:116], r[:, 100:108], -1e30, r[:, 56:64], ALU.mult, ALU.add, br, br)
                        k.rmax(r[:, 97:98], r[:, 108:116], br, br)
                        k.ts("dve", r[:, 116:124], r[:, 56:64], r[:, 97:98], ALU.is_ge, br, br)
                        k.ts("dve", r[:, 100:108], r[:, 56:64], r[:, 96:97], ALU.subtract, br, br)
                        k.act(r[:, 100:108], r[:, 100:108], AF.Exp, br, br)
                        k.tt("dve", r[:, 100:108], r[:, 100:108], r[:, 116:124], ALU.mult, br, br)
                        k.rsum(r[:, 98:99], r[:, 100:108], br, br)
                        k.recip(r[:, 99:100], r[:, 98:99], br, br)
                        k.tt("dve", r[:, 99:100], r[:, 99:100], r[:, 42:43], ALU.mult, br, br)
                        k.ts("dve", r[:, 100:108], r[:, 100:108], r[:, 99:100], ALU.mult, br, br)
                        k.tt("dve", gates[:, t, :].rearrange("p (g e) -> p g e", e=8),
                             bc(r[:, 44:48].unsqueeze(2), [128, 4, 8]), bc(r[:, 100:108].unsqueeze(1), [128, 4, 8]),
                             ALU.mult, br, [B_gates[t]])

                    pipeline(NT_OWN, [d1_s0, d1_s1, d1_s2], [0, 1, 2])
                    dump("D1", "gates", gates[:], [128, NT_OWN, 32], F32)
                    dump("D1", "xnT", xnT[:], [128, 8, 2048], BF16)
                    S.barrier()

            for _once in ([0] if on("D2") else []):
                with ExitStack() as s2:
                    w1s = [sb(s2, "w1s%d" % i, [128, 8, 512], BF16) for i in range(2)]
                    w3s = [sb(s2, "w3s%d" % i, [128, 8, 512], BF16) for i in range(2)]
                    w2s = [sb(s2, "w2s%d" % i, [128, 4, 1024], BF16) for i in range(2)]
                    hdn = [sb(s2, "hdn%d" % i, [128, 4, 512], BF16) for i in range(2)]
                    sil = [sb(s2, "sil%d" % i, [128, 512], F32) for i in range(2)]
                    p1 = [ps(s2, "p1_%d" % i, [128, 512], F32) for i in range(2)]
                    p3 = [ps(s2, "p3_%d" % i, [128, 512], F32) for i in range(2)]
                    pY = [ps(s2, "pY%d" % i, [128, 512], F32) for i in range(3)]
                    B_w1 = [Buf(), Buf()]
                    B_w3 = [Buf(), Buf()]
                    B_w2 = [Buf(), Buf()]
                    B_hdn = [[Buf() for _ in range(4)] for _ in range(2)]
                    B_sil = [Buf(), Buf()]
                    B_p1 = [Buf(), Buf()]
                    B_p3 = [Buf(), Buf()]
                    B_pY = [Buf() for _ in range(3)]
                    w1v = dr["w1"].ap()
                    w3v = dr["w3"].ap()
                    w2v = dr["w2"].ap()
                    cntd = {"p": 0, "y": 0}

                    def load_w(e):
                        wb = e % 2
                        k.dma("pool", w1s[wb][:], w1v[e].rearrange("(kc p) n -> p kc n", p=128), (), [B_w1[wb]])
                        k.dma("pool", w3s[wb][:], w3v[e].rearrange("(kc p) n -> p kc n", p=128), (), [B_w3[wb]])
                        k.dma("pool", w2s[wb][:], w2v[e].rearrange("(kc p) n -> p kc n", p=128), (), [B_w2[wb]])

                    load_w(0)

                    def d2_s0(i):
                        e, tg = i // 4, i % 4
                        wb = e % 2
                        hb = i % 2
                        if tg == 0 and e + 1 < 32:
                            load_w(e + 1)
                        for hc in range(4):
                            pb = cntd["p"] % 2
                            cntd["p"] += 1
                            for kc in range(8):
                                k.mm(p1[pb][:], w1s[wb][:, kc, hc * 128:(hc + 1) * 128], xnT[:, kc, tg * 512:(tg + 1) * 512],
                                     kc == 0, kc == 7, [B_w1[wb]] + B_xnT[tg * 4:tg * 4 + 4], [B_p1[pb]])
                            for kc in range(8):
                                k.mm(p3[pb][:], w3s[wb][:, kc, hc * 128:(hc + 1) * 128], xnT[:, kc, tg * 512:(tg + 1) * 512],
                                     kc == 0, kc == 7, [B_w3[wb]] + B_xnT[tg * 4:tg * 4 + 4], [B_p3[pb]])
                            k.act(sil[pb][:], p1[pb][:], AF.Silu, [B_p1[pb]], [B_sil[pb]])
                            k.tt("dve", hdn[hb][:, hc, :], sil[pb][:], p3[pb][:], ALU.mult, [B_sil[pb], B_p3[pb]], [B_hdn[hb][hc]])

                    def d2_s1(i):
                        e, tg = i // 4, i % 4
                        wb = e % 2
                        hb = i % 2
                        for tt_ in range(4):
                            t = tg * 4 + tt_
                            for cg in range(2):
                                yb = cntd["y"] % 3
                                cntd["y"] += 1
                                for hc in range(4):
                                    k.mm(pY[yb][:], hdn[hb][:, hc, tt_ * 128:(tt_ + 1) * 128], w2s[wb][:, hc, cg * 512:(cg + 1) * 512],
                                         hc == 0, hc == 3, [B_hdn[hb][hc], B_w2[wb]], [B_pY[yb]])
                                k.stt("dve", hres[:, t, cg * 512:(cg + 1) * 512], pY[yb][:], gates[:, t, e:e + 1],
                                      hres[:, t, cg * 512:(cg + 1) * 512], ALU.mult, ALU.add,
                                      [B_pY[yb], B_gates[t], B_h[t][cg]], [B_h[t][cg]])

                    pipeline(128, [d2_s0, d2_s1], [0, 1])
                    dump("D2", "hres", hres[:], [128, NT_OWN, 1024], F32)
                    S.barrier()

        out_toks = []
        for _once in ([0] if on("E") else []):
            with ExitStack() as s1:
                wple = sb(s1, "wple", [128, 2, 1024], BF16)
                wplg = sb(s1, "wplg", [128, 8, 1024], BF16)
                gplg = sb(s1, "gplg", [128, 1024], F32)
                gple = sb(s1, "gple", [128, 1024], F32)
                pt = [sb(s1, "pt%d" % i, [128, 256], F32) for i in range(2)]
                ptb = [sb(s1, "ptb%d" % i, [128, 256], BF16) for i in range(2)]
                pTs = [sb(s1, "pTs%d" % i, [128, 2, 128], BF16) for i in range(2)]
                hn = [sb(s1, "hn%d" % i, [128, 1024], BF16) for i in range(2)]
                hT = [sb(s1, "hT%d" % i, [128, 8, 128], BF16) for i in range(2)]
                junk = sb(s1, "junke", [128, 1024], BF16)
                statA = [sb(s1, "stateA%d" % i, [128, 4], F32) for i in range(2)]
                statB = [sb(s1, "stateB%d" % i, [128, 4], F32) for i in range(2)]
                pe_s = [sb(s1, "pe_s%d" % i, [128, 1024], F32) for i in range(2)]
                sg = [sb(s1, "sg%d" % i, [128, 1024], F32) for i in range(2)]
                yo = [sb(s1, "yo%d" % i, [128, 1024], F32) for i in range(2)]
                pTp = ps(s1, "pTp", [128, 1024], BF16)
                pTh = ps(s1, "pTh", [128, 1024], BF16)
                pE = ps(s1, "pE", [128, 1024], F32)
                pG = ps(s1, "pG", [128, 1024], F32)
                B = {n: [Buf(), Buf()] for n in ("pt", "ptb", "pTs", "hn", "hT", "stA", "stB", "pe_s", "sg", "yo")}
                B_pTp, B_pTh, B_pE, B_pG = Buf(), Buf(), [Buf(), Buf()], [Buf(), Buf()]
                B_wple, B_wplg, B_g1, B_g2 = Buf(), Buf(), Buf(), Buf()
                k.dma("pool", wple[:], dr["w_ple"].ap().rearrange("(kc p) n -> p kc n", p=128), (), [B_wple])
                k.dma("pool", wplg[:], dr["w_plg"].ap().rearrange("(kc p) n -> p kc n", p=128), (), [B_wplg])
                k.dma("sp", gplg[:], bvec("g_plg", 1024), (), [B_g1])
                k.dma("sp", gple[:], bvec("g_ple", 1024), (), [B_g2])

                def e_s0(t):
                    par = t % 2
                    hb = B_h[t]
                    st_ = statA[par]
                    bs = [B["stA"][par]]
                    k.dma("sp", pt[par][:], p_ap[t * 128:(t + 1) * 128, :], (), [B["pt"][par]])
                    k.copy("dve", ptb[par][:], pt[par][:], [B["pt"][par]], [B["ptb"][par]])
                    k.act(junk[:], hres[:, t, :], AF.Square, hb, bs, accum=st_[:, 0:1])
                    k.act(st_[:, 1:2], st_[:, 0:1], AF.Sqrt, bs, bs, scale=1.0 / 1024, bias=EPS)
                    k.recip(st_[:, 2:3], st_[:, 1:2], bs, bs)
                    k.stt("dve", hn[par][:], hres[:, t, :], st_[:, 2:3], gplg[:], ALU.mult, ALU.mult, hb + bs + [B_g1], [B["hn"][par]])

                def e_s1(t):
                    par = t % 2
                    for kc in range(2):
                        k.tr(pTp[:, kc * 128:(kc + 1) * 128], ptb[par][:, kc * 128:(kc + 1) * 128], idb[:], [B["ptb"][par], B_id], [B_pTp])
                    k.copy("act", pTs[par][:].rearrange("p a b -> p (a b)"), pTp[:, 0:256], [B_pTp], [B["pTs"][par]])
                    for kc in range(8):
                        k.tr(pTh[:, kc * 128:(kc + 1) * 128], hn[par][:, kc * 128:(kc + 1) * 128], idb[:], [B["hn"][par], B_id], [B_pTh])
                    k.copy("act", hT[par][:].rearrange("p a b -> p (a b)"), pTh[:], [B_pTh], [B["hT"][par]])

                def e_s2(t):
                    par = t % 2
                    for cg in range(2):
                        for kc in range(2):
                            k.mm(pE[:, cg * 512:(cg + 1) * 512], pTs[par][:, kc, :], wple[:, kc, cg * 512:(cg + 1) * 512], kc == 0, kc == 1,
                                 [B["pTs"][par], B_wple], [B_pE[cg]])
                    k.copy("act", pe_s[par][:], pE[:], B_pE, [B["pe_s"][par]])
                    for cg in range(2):
                        for kc in range(8):
                            k.mm(pG[:, cg * 512:(cg + 1) * 512], hT[par][:, kc, :], wplg[:, kc, cg * 512:(cg + 1) * 512], kc == 0, kc == 7,
                                 [B["hT"][par], B_wplg], [B_pG[cg]])
                    k.act(sg[par][:], pG[:], AF.Sigmoid, B_pG, [B["sg"][par]])

                def e_s3(t):
                    par = t % 2
                    hb = B_h[t]
                    st_ = statB[par]
                    bs = [B["stB"][par]]
                    k.act(junk[:], pe_s[par][:], AF.Square, [B["pe_s"][par]], bs, accum=st_[:, 0:1])
                    k.act(st_[:, 1:2], st_[:, 0:1], AF.Sqrt, bs, bs, scale=1.0 / 1024, bias=EPS)
                    k.recip(st_[:, 2:3], st_[:, 1:2], bs, bs)
                    k.stt("dve", pe_s[par][:], pe_s[par][:], st_[:, 2:3], gple[:], ALU.mult, ALU.mult,
                          [B["pe_s"][par], B_g2] + bs, [B["pe_s"][par]])
                    k.tt("dve", sg[par][:], sg[par][:], pe_s[par][:], ALU.mult, [B["sg"][par], B["pe_s"][par]], [B["sg"][par]])
                    k.tt("dve", yo[par][:], sg[par][:], hres[:, t, :], ALU.add, [B["sg"][par]] + hb, [B["yo"][par]])
                    out_toks.append(k.dma("sp", y_ap[t * 128:(t + 1) * 128, :], yo[par][:], [B["yo"][par]], ()))

                pipeline(NT_OWN, [e_s0, e_s1, e_s2, e_s3], [0, 1, 2, 3])
                S.barrier()
        S.ops["sp"].append(Op(None, list(out_toks) + dbg_toks, None))
        S.emit()
    return nc


def _rope_tables():
    inv = (np.float32(10000.0) ** (-np.arange(0, 64, 2, dtype=np.float32) / np.float32(64))).astype(np.float32)
    ang = np.arange(4096, dtype=np.float32)[:, None] * inv[None, :]
    return np.cos(ang).astype(np.float32), np.sin(ang).astype(np.float32)


def _bias_tables(rpb, hf):
    kp = np.arange(128)
    kpar, kcol = kp // 64, kp % 64
    qpar, qcol = kp // 64, kp % 64
    cs = np.clip(qcol - 8, 0, 48)
    col_valid = (kcol[:, None] >= cs[None, :]) & (kcol[:, None] < cs[None, :] + 16)
    col_off = np.clip(kcol[:, None] - qcol[None, :] + 15, 0, 30)

    def tile(j, off, hf_):
        r = hf_ * 32 + 2 * j + qpar
        kr = hf_ * 32 + 2 * (j + off) + kpar
        rs = np.clip(r - 4, 0, 56)
        row_valid = (kr[:, None] >= rs[None, :]) & (kr[:, None] <= rs[None, :] + 7) & (kr[:, None] >= 0) & (kr[:, None] <= 63)
        row_off = np.clip(kr[:, None] - r[None, :] + 7, 0, 14)
        valid = row_valid & col_valid
        vals = rpb[:, row_off, col_off]
        return np.where(valid[None], vals, np.float32(NEG)).astype(np.float32)

    G = np.zeros((4, 128, 4, 5, 128), np.float32)
    for oi, off in enumerate(NA_GEN):
        tl = tile(8, off, 0)
        for g in range(4):
            G[g, :, :, oi, :] = tl[g * 4:(g + 1) * 4].transpose(1, 0, 2)
    Sp = np.full((4, 4, 128, 4, 6, 128), np.float32(NEG), np.float32)
    for ji, j in enumerate(NA_SPECIAL):
        for oi, off in enumerate(NA_OFFS[j]):
            tl = tile(j, off, hf)
            for g in range(4):
                Sp[ji, g, :, :, oi, :] = tl[g * 4:(g + 1) * 4].transpose(1, 0, 2)
    return G.reshape(4, 128, 4 * 5 * 128), Sp.reshape(4, 4, 128, 4 * 6 * 128)


_NC_CACHE = {}


def prep_inputs(x, p, g_mix, w_in, g_qa, g_ka, lam_q1, lam_k1, lam_q2, lam_k2, g_sub, g_qb, g_kb,
           rpb, w_out, g_ffn, w_rg, w_re, w1, w3, w2, g_plg, w_plg, w_ple, g_ple):
    f = lambda a: np.ascontiguousarray(np.asarray(a, dtype=np.float32))
    x = f(x); p = f(p)
    cos, sin = _rope_tables()
    common = {
        "w_in": f(w_in)[0], "w_out": f(w_out)[0], "w_plg": f(w_plg)[0], "w_ple": f(w_ple)[0],
        "w_r": np.ascontiguousarray(np.concatenate([f(w_rg)[0], f(w_re)[0]], axis=1)),
        "w1": f(w1)[0], "w3": f(w3)[0], "w2": f(w2)[0],
        "g_mix": f(g_mix), "g_ffn": f(g_ffn), "g_plg": f(g_plg), "g_ple": f(g_ple),
        "g_qa": f(g_qa), "g_ka": f(g_ka), "lam_q1": f(lam_q1), "lam_k1": f(lam_k1),
        "lam_q2": f(lam_q2), "lam_k2": f(lam_k2), "g_sub": f(g_sub), "g_qb": f(g_qb), "g_kb": f(g_kb),
        "ident": np.eye(128, dtype=np.float32),
    }
    rpb0 = f(rpb)[0]
    in_maps = []
    for c in range(8):
        b, hf = c // 2, c % 2
        own = slice(hf * 2048, (hf + 1) * 2048)
        oth = slice((1 - hf) * 2048, (2 - hf) * 2048)
        xc = np.ascontiguousarray(np.concatenate([x[b, own], x[b, oth]], axis=0))
        pos = np.concatenate([np.arange(4096)[own], np.arange(4096)[oth]])
        cc = np.concatenate([cos[pos], cos[pos]], axis=1)
        ss = np.concatenate([-sin[pos], sin[pos]], axis=1)
        cs_t = np.ascontiguousarray(cc.reshape(32, 128, 64).transpose(1, 0, 2).reshape(128, 32 * 64))
        sn_t = np.ascontiguousarray(ss.reshape(32, 128, 64).transpose(1, 0, 2).reshape(128, 32 * 64))
        G, Sp = _bias_tables(rpb0, hf)
        m = dict(common)
        m.update({"x": xc, "p": np.ascontiguousarray(p[0, b, own]), "cs": cs_t, "sn": sn_t, "biasG": G, "biasS": Sp})
        in_maps.append(m)
    return in_maps


def kernel(**inputs):
    in_maps = prep_inputs(**inputs)
    if "nc" not in _NC_CACHE:
        _NC_CACHE["nc"] = build_program()
    nc = _NC_CACHE["nc"]
    res = run_bass_kernel_spmd(nc, in_maps, core_ids=list(range(8)))
    out = np.zeros((4, 4096, 1024), np.float32)
    for c in range(8):
        b, hf = c // 2, c % 2
        out[b, hf * 2048:(hf + 1) * 2048] = res.results[c]["y"]
    return out
```

```python
import math
import numpy as np
import concourse.bass as bass
import concourse.mybir as mybir
from contextlib import ExitStack
from concourse.bass_utils import run_bass_kernel_spmd

F32 = mybir.dt.float32
BF16 = mybir.dt.bfloat16
AF = mybir.ActivationFunctionType
ALU = mybir.AluOpType
AX = mybir.AxisListType

COMPUTE = ("pe", "act", "dve", "pool")
ALL_ENG = ("pe", "act", "dve", "pool", "sp")
N_DMA_SEMS = 24
EPS = 1e-6
NEG = -1e30
NT_OWN = 16
NT_ALL = 32


class Tok:
    __slots__ = ("eng", "idx", "needed", "sem", "val")

    def __init__(self, eng, idx):
        self.eng = eng
        self.idx = idx
        self.needed = False
        self.sem = None
        self.val = None


class Buf:
    __slots__ = ("name", "w", "r")

    def __init__(self, name=""):
        self.name = name
        self.w = None
        self.r = {}


class Op:
    __slots__ = ("fn", "waits", "tok")

    def __init__(self, fn, waits, tok):
        self.fn = fn
        self.waits = waits
        self.tok = tok


class Sched:
    def __init__(self, nc, es):
        self.nc = nc
        self.ops = {e: [] for e in ALL_ENG}
        self.waited = {e: {} for e in ALL_ENG}
        self.eng_sem = {e: es.enter_context(nc.semaphore("s_" + e)) for e in COMPUTE}
        self.dma_sems = [es.enter_context(nc.semaphore("s_dma%d" % i)) for i in range(N_DMA_SEMS)]
        self.dma_cnt = [0] * N_DMA_SEMS
        self.dma_last = [None] * N_DMA_SEMS
        self.dma_rr = 0
        self.dma_rr_sw = 0
        self.dma_rr_sw = 0
        self.n_dma = 0
        self.last_tok = {e: None for e in COMPUTE}

    def _need(self, eng, t, out):
        wd = self.waited[eng]
        if t.eng in COMPUTE:
            key = t.eng
            if wd.get(key, -1) >= t.idx:
                return
            wd[key] = t.idx
        else:
            key = t.sem
            if wd.get(key, -1) >= t.val:
                return
            wd[key] = t.val
        t.needed = True
        out.append(t)

    def _collect(self, eng, reads, writes, is_dma):
        out = []
        for b in reads:
            t = b.w
            if t is not None:
                if t.eng == eng and not is_dma and eng == "pe":
                    continue
                self._need(eng, t, out)
        for b in writes:
            t = b.w
            if t is not None:
                if not (t.eng == eng and not is_dma and eng == "pe"):
                    self._need(eng, t, out)
            for t in b.r.values():
                if t.eng == eng and not is_dma and eng == "pe":
                    continue
                self._need(eng, t, out)
        return out

    def op(self, eng, fn, reads=(), writes=()):
        waits = self._collect(eng, reads, writes, False)
        tok = Tok(eng, len(self.ops[eng]))
        self.ops[eng].append(Op(fn, waits, tok))
        self.last_tok[eng] = tok
        for b in reads:
            b.r[eng] = tok
        for b in writes:
            b.w = tok
            b.r = {}
        return tok

    def dma(self, queue, fn, reads=(), writes=()):
        waits = self._collect(queue, reads, writes, True)
        if queue == "pool":
            i = 16 + self.dma_rr_sw
            self.dma_rr_sw = (self.dma_rr_sw + 1) % (N_DMA_SEMS - 16)
        else:
            i = self.dma_rr
            self.dma_rr = (self.dma_rr + 1) % 16
        prev = self.dma_last[i]
        if prev is not None:
            self._need(queue, prev, waits)
        self.dma_cnt[i] += 1
        tok = Tok("dma", self.n_dma)
        self.n_dma += 1
        tok.sem = self.dma_sems[i]
        tok.val = 16 * self.dma_cnt[i]
        tok.needed = True
        self.dma_last[i] = tok
        self.ops[queue].append(Op(fn, waits, tok))
        for b in reads:
            b.r[("dma", tok.idx)] = tok
        for b in writes:
            b.w = tok
            b.r = {}
        return tok

    def barrier(self):
        toks = [self.last_tok[e] for e in COMPUTE if self.last_tok[e] is not None]
        toks += [t for t in self.dma_last if t is not None]
        for e in ALL_ENG:
            waits = []
            for t in toks:
                if t.eng == e:
                    continue
                self._need(e, t, waits)
            if waits:
                self.ops[e].append(Op(None, waits, None))

    def emit(self):
        nc = self.nc
        for e in COMPUTE:
            c = 0
            for o in self.ops[e]:
                t = o.tok
                if t is not None and t.eng == e and t.needed:
                    c += 1
                    t.sem = self.eng_sem[e]
                    t.val = c

        def run(e, eng):
            for o in self.ops[e]:
                for t in o.waits:
                    eng.wait_ge(t.sem, t.val)
                if o.fn is None:
                    continue
                ins = o.fn(eng)
                t = o.tok
                if t.eng == "dma":
                    ins.then_inc(t.sem, 16)
                elif t.needed:
                    ins.then_inc(t.sem, 1)

        with nc.Block() as block:
            @block.tensor
            def _(eng):
                run("pe", eng)

            @block.scalar
            def _(eng):
                run("act", eng)

            @block.vector
            def _(eng):
                run("dve", eng)

            @block.gpsimd
            def _(eng):
                run("pool", eng)

            @block.sync
            def _(eng):
                run("sp", eng)


class K:
    def __init__(self, S):
        self.S = S

    def mm(self, out, lhsT, rhs, start, stop, R, W, tp=None):
        if tp is None:
            f = lambda e: e.matmul(out, lhsT=lhsT, rhs=rhs, start=start, stop=stop)
        else:
            f = lambda e: e.matmul(out, lhsT=lhsT, rhs=rhs, start=start, stop=stop, tile_position=tp)
        return self.S.op("pe", f, R, W)

    def tr(self, out, in_, ident, R, W):
        return self.S.op("pe", lambda e: e.transpose(out=out, in_=in_, identity=ident), R, W)

    def act(self, out, in_, func, R, W, scale=1.0, bias=0.0, accum=None):
        if accum is None:
            f = lambda e: e.activation(out=out, in_=in_, func=func, bias=bias, scale=scale)
        else:
            f = lambda e: e.activation(out=out, in_=in_, func=func, bias=bias, scale=scale, accum_out=accum)
        return self.S.op("act", f, R, W)

    def tt(self, eng, out, in0, in1, op, R, W):
        return self.S.op(eng, lambda e: e.tensor_tensor(out=out, in0=in0, in1=in1, op=op), R, W)

    def ts(self, eng, out, in0, s1, op0, R, W, s2=None, op1=None):
        if op1 is None:
            f = lambda e: e.tensor_scalar(out=out, in0=in0, scalar1=s1, scalar2=None, op0=op0)
        else:
            f = lambda e: e.tensor_scalar(out=out, in0=in0, scalar1=s1, scalar2=s2, op0=op0, op1=op1)
        return self.S.op(eng, f, R, W)

    def stt(self, eng, out, in0, scalar, in1, op0, op1, R, W):
        return self.S.op(eng, lambda e: e.scalar_tensor_tensor(out=out, in0=in0, scalar=scalar, in1=in1, op0=op0, op1=op1), R, W)

    def rsum(self, out, in_, R, W):
        return self.S.op("dve", lambda e: e.reduce_sum(out=out, in_=in_, axis=AX.X), R, W)

    def rmax(self, out, in_, R, W):
        return self.S.op("dve", lambda e: e.reduce_max(out=out, in_=in_, axis=AX.X), R, W)

    def recip(self, out, in_, R, W):
        return self.S.op("dve", lambda e: e.reciprocal(out=out, in_=in_), R, W)

    def copy(self, eng, out, in_, R, W):
        if eng == "act":
            return self.S.op("act", lambda e: e.activation(out=out, in_=in_, func=AF.Copy), R, W)
        return self.S.op(eng, lambda e: e.tensor_copy(out=out, in_=in_), R, W)

    def memset(self, eng, out, val, W):
        return self.S.op(eng, lambda e: e.memset(out, val), (), W)

    def dma(self, queue, out, in_, R, W):
        return self.S.dma(queue, lambda e: e.dma_start(out=out, in_=in_), R, W)


def bc(ap, shape):
    return ap.broadcast_to(shape)


NA_OFFS = {0: [-2, -1, 0, 1, 2, 3], 1: [-2, -1, 0, 1, 2], 14: [-2, -1, 0, 1, 2], 15: [-3, -2, -1, 0, 1, 2]}
NA_GEN = [-2, -1, 0, 1, 2]
NA_SPECIAL = [0, 1, 14, 15]


PHASES = ["A1", "B1", "A2", "B2", "C", "D1", "D2", "E"]


def build_program(stop=None):
    nc = bass.Bass("TRN2", target_bir_lowering=False)
    dr = {}
    last = len(PHASES) - 1 if stop is None else PHASES.index(stop)

    def on(ph):
        return PHASES.index(ph) <= last

    def din(name, shape):
        dr[name] = nc.dram_tensor(name, list(shape), F32, kind="ExternalInput")
        return dr[name]

    din("x", [4096, 1024])
    din("p", [2048, 256])
    din("cs", [128, 32 * 64])
    din("sn", [128, 32 * 64])
    din("w_in", [1024, 3072])
    din("w_out", [1024, 1024])
    din("w_plg", [1024, 1024])
    din("w_ple", [256, 1024])
    din("w_r", [1024, 36])
    din("w1", [32, 1024, 512])
    din("w3", [32, 1024, 512])
    din("w2", [32, 512, 1024])
    for n in ("g_mix", "g_ffn", "g_plg", "g_ple"):
        din(n, [1, 1024])
    for n in ("g_qa", "g_ka", "lam_q1", "lam_k1", "lam_q2", "lam_k2"):
        din(n, [1, 64])
    din("g_sub", [1, 128])
    din("g_qb", [1, 32])
    din("g_kb", [1, 32])
    din("ident", [128, 128])
    din("biasG", [4, 128, 4 * 5 * 128])
    din("biasS", [4, 4, 128, 4 * 6 * 128])
    y = nc.dram_tensor("y", [2048, 1024], F32, kind="ExternalOutput")

    x_ap = dr["x"].ap()
    p_ap = dr["p"].ap()
    y_ap = y.ap()

    def bvec(name, n):
        return bass.AP(dr[name], 0, [[0, 128], [1, n]])

    with ExitStack() as es:
        S = Sched(nc, es)
        k = K(S)

        uid = [0]

        def sb(stack, name, shape, dt):
            uid[0] += 1
            return stack.enter_context(nc.sbuf_tensor("%s_%d" % (name, uid[0]), shape, dt))

        def ps(stack, name, shape, dt):
            uid[0] += 1
            return stack.enter_context(nc.psum_tensor("%s_%d" % (name, uid[0]), shape, dt))

        dbg_toks = []

        def dump(ph, name, tens, shape, dt):
            if stop != ph:
                return
            S.barrier()
            d = nc.dram_tensor("dbg_" + name, list(shape), dt, kind="ExternalOutput")
            dbg_toks.append(k.dma("sp", d.ap(), tens, [], ()))

        idf = sb(es, "idf", [128, 128], F32)
        idb = sb(es, "idb", [128, 128], BF16)
        o_cat = sb(es, "o_cat", [128, NT_OWN, 1024], BF16)
        B_id = Buf("id")
        B_ocat = [[Buf("ocatA%d" % t), Buf("ocatB%d" % t)] for t in range(NT_OWN)]
        k.dma("sp", idf[:], dr["ident"].ap(), (), [B_id])
        k.copy("dve", idb[:], idf[:], [B_id], [B_id])

        def pipeline(n, stages, offs):
            for T in range(n + max(offs)):
                for s_ in reversed(range(len(stages))):
                    i = T - offs[s_]
                    if 0 <= i < n:
                        stages[s_](i)

        def head_norm(raw, ngrp, gd, gtile, wk, BBw, B_raw, eng2="pool"):
            n = ngrp * gd
            v3 = lambda ap: ap[:, 0:n].rearrange("p (a b) -> p a b", b=gd)
            return [
                lambda: k.tt(eng2, wk["sq"][:, 0:n], raw[:, 0:n], raw[:, 0:n], ALU.mult, [B_raw], [BBw["sq"]]),
                lambda: k.rsum(wk["ss"][:, 0:ngrp], v3(wk["sq"]), [BBw["sq"]], [BBw["ss"]]),
                lambda: k.act(wk["ss"][:, 16:16 + ngrp], wk["ss"][:, 0:ngrp], AF.Sqrt, [BBw["ss"]], [BBw["ss"]],
                              scale=1.0 / gd, bias=EPS),
                lambda: k.recip(wk["ss"][:, 32:32 + ngrp], wk["ss"][:, 16:16 + ngrp], [BBw["ss"]], [BBw["ss"]]),
                lambda: k.tt("dve", v3(wk["kn"]), v3(raw), bc(wk["ss"][:, 32:32 + ngrp].unsqueeze(2), [128, ngrp, gd]),
                             ALU.mult, [B_raw, BBw["ss"]], [BBw["kn"]]),
                lambda: k.tt(eng2, v3(wk["kn"]), v3(wk["kn"]), bc(gtile[:].unsqueeze(1), [128, ngrp, gd]), ALU.mult,
                             [BBw["kn"], BBw["gv"]], [BBw["kn"]]),
            ]

        def proj_phase(s1, kind, tiles, col0, QT, KT, Vst, B_QT, B_KT, B_V):
            da = kind == "da"
            ngrp, gd = (8, 64) if da else (16, 32)
            w_sb = sb(s1, "w_in_" + kind, [128, 8, 1536], BF16)
            xt = [sb(s1, "xt%d" % i, [128, 1024], F32) for i in range(2)]
            a_bf = [sb(s1, "abf%d" % i, [128, 1024], BF16) for i in range(2)]
            junk = sb(s1, "junk", [128, 1024], BF16)
            stat = [sb(s1, "stat%d" % i, [128, 4], F32) for i in range(2)]
            aT = [sb(s1, "aT%d" % i, [128, 8, 128], BF16) for i in range(2)]
            gmix = sb(s1, "gmix", [128, 1024], F32)
            gq = sb(s1, "gq", [128, gd], F32)
            gk = sb(s1, "gk", [128, gd], F32)
            raw = [[sb(s1, "raw%d_%d" % (i, c), [128, 512], F32) for c in range(2)] for i in range(2)]
            knb = [[sb(s1, "knb%d_%d" % (i, c), [128, 512], BF16) for c in range(2)] for i in range(2)]
            names = ("sq", "kn", "A", "Bt") if da else ("sq", "kn")
            wks = []
            for c in range(2):
                wks.append({n: sb(s1, "wk_%s%d" % (n, c), [128, 512], F32) for n in names})
                wks[c]["ss"] = sb(s1, "wk_ss%d" % c, [128, 48], F32)
            if da:
                cs_sb = sb(s1, "cs_sb", [128, 32, 64], F32)
                sn_sb = sb(s1, "sn_sb", [128, 32, 64], F32)
            pT = [ps(s1, "pT%d" % i, [128, 1024], BF16) for i in range(2)]
            pP = [ps(s1, "pP%d" % i, [128, 512], F32) for i in range(3)]
            pT2 = [ps(s1, "pT2%d" % i, [128, 1024], BF16) for i in range(2)]
            B_xt, B_st, B_a, B_pT, B_aT = ([Buf(), Buf()] for _ in range(5))
            B_g, B_tab, B_gq, B_gk = Buf(), Buf(), Buf(), Buf()
            B_w = [Buf() for _ in range(3)]
            B_pP = [Buf() for _ in range(3)]
            B_pT2 = [Buf(), Buf()]
            B_raw = [[Buf(), Buf()], [Buf(), Buf()]]
            B_knb = [[Buf(), Buf()], [Buf(), Buf()]]
            BW = [{n: Buf() for n in ("sq", "ss", "kn", "A", "Bt")} for _ in range(2)]
            BW[0]["gv"], BW[1]["gv"] = B_gq, B_gk
            w_in_v = dr["w_in"].ap().rearrange("(kc p) n -> p kc n", p=128)
            order = [1, 2, 0]
            for cg in order:
                k.dma("pool", w_sb[:, :, cg * 512:(cg + 1) * 512], w_in_v[:, :, col0 + cg * 512:col0 + (cg + 1) * 512], (), [B_w[cg]])
            k.dma("sp", gmix[:], bvec("g_mix", 1024), (), [B_g])
            k.dma("sp", gq[:], bvec("g_qa" if da else "g_qb", gd), (), [B_gq])
            k.dma("sp", gk[:], bvec("g_ka" if da else "g_kb", gd), (), [B_gk])
            if da:
                k.dma("sp", cs_sb[:].rearrange("p a b -> p (a b)"), dr["cs"].ap(), (), [B_tab])
                k.dma("sp", sn_sb[:].rearrange("p a b -> p (a b)"), dr["sn"].ap(), (), [B_tab])
            cnt = {"pp": 0, "p2": 0}

            def groups(i):
                return ([0] if tiles[i] < NT_OWN else []) + [1, 2]

            def s0(i):
                t, par = tiles[i], i % 2
                k.dma("sp", xt[par][:], x_ap[t * 128:(t + 1) * 128, :], (), [B_xt[par]])
                k.act(junk[:], xt[par][:], AF.Square, [B_xt[par]], [B_st[par]], accum=stat[par][:, 0:1])
                k.act(stat[par][:, 1:2], stat[par][:, 0:1], AF.Sqrt, [B_st[par]], [B_st[par]], scale=1.0 / 1024, bias=EPS)
                k.recip(stat[par][:, 2:3], stat[par][:, 1:2], [B_st[par]], [B_st[par]])
                k.stt("dve", a_bf[par][:], xt[par][:], stat[par][:, 2:3], gmix[:], ALU.mult, ALU.mult,
                      [B_xt[par], B_st[par], B_g], [B_a[par]])

            def s1_(i):
                par = i % 2
                for kc in range(8):
                    k.tr(pT[par][:, kc * 128:(kc + 1) * 128], a_bf[par][:, kc * 128:(kc + 1) * 128], idb[:],
                         [B_a[par], B_id], [B_pT[par]])
                k.copy("act", aT[par][:].rearrange("p a b -> p (a b)"), pT[par][:], [B_pT[par]], [B_aT[par]])

            def s2_(i):
                t, par = tiles[i], i % 2
                for cg in groups(i):
                    pp = cnt["pp"] % 3
                    cnt["pp"] += 1
                    for kc in range(8):
                        k.mm(pP[pp][:], aT[par][:, kc, :], w_sb[:, kc, cg * 512:(cg + 1) * 512], kc == 0, kc == 7,
                             [B_aT[par], B_w[cg]], [B_pP[pp]])
                    if cg == 2:
                        if da:
                            k.copy("act", Vst[:, t, :, 0:128], pP[pp][:].rearrange("p (a b) -> p a b", b=128), [B_pP[pp]], [B_V[t]])
                        else:
                            k.copy("act", Vst[:, i, :, 0:32], pP[pp][:].rearrange("p (a b) -> p a b", b=32), [B_pP[pp]], [B_V[i]])
                    else:
                        k.copy("dve", raw[par][cg][:], pP[pp][:], [B_pP[pp]], [B_raw[par][cg]])

            def s3_chain(i, cg):
                t, par = tiles[i], i % 2
                wk, bw = wks[cg], BW[cg]
                ch = head_norm(raw[par][cg], ngrp, gd, gq if cg == 0 else gk, wk, bw, B_raw[par][cg])
                if da:
                    kn3 = wk["kn"][:].rearrange("p (a b) -> p a b", b=64)
                    A3 = wk["A"][:].rearrange("p (a b) -> p a b", b=64)
                    Bt3 = wk["Bt"][:].rearrange("p (a b) -> p a b", b=64)
                    ch += [
                        lambda: k.tt("dve", A3, kn3, bc(cs_sb[:, t, :].unsqueeze(1), [128, 8, 64]), ALU.mult,
                                     [bw["kn"], B_tab], [bw["A"]]),
                        lambda: k.tt("pool", Bt3[:, :, 0:32], kn3[:, :, 32:64], bc(sn_sb[:, t, 0:32].unsqueeze(1), [128, 8, 32]),
                                     ALU.mult, [bw["kn"], B_tab], [bw["Bt"]]),
                        lambda: k.tt("pool", Bt3[:, :, 32:64], kn3[:, :, 0:32], bc(sn_sb[:, t, 32:64].unsqueeze(1), [128, 8, 32]),
                                     ALU.mult, [bw["kn"], B_tab], [bw["Bt"]]),
                        lambda: k.tt("dve", knb[par][cg][:], wk["A"][:], wk["Bt"][:], ALU.add, [bw["A"], bw["Bt"]],
                                     [B_knb[par][cg]]),
                    ]
                else:
                    ch.append(lambda: k.copy("dve", knb[par][cg][:], wk["kn"][:], [bw["kn"]], [B_knb[par][cg]]))
                return ch

            def s3_(i):
                chains = [s3_chain(i, cg) for cg in groups(i) if cg != 2]
                for j in range(max(len(c) for c in chains)):
                    for c in chains:
                        if j < len(c):
                            c[j]()

            def s4_(i):
                t, par = tiles[i], i % 2
                for cg in groups(i):
                    if cg == 2:
                        continue
                    p2 = cnt["p2"] % 2
                    cnt["p2"] += 1
                    for hh in range(4):
                        k.tr(pT2[p2][:, hh * 128:(hh + 1) * 128], knb[par][cg][:, hh * 128:(hh + 1) * 128], idb[:],
                             [B_knb[par][cg], B_id], [B_pT2[p2]])
                    src = pT2[p2][:, 0:512].rearrange("p (a b) -> p a b", b=128)
                    if cg == 0:
                        k.copy("act", QT[:, :, t * 128:(t + 1) * 128], src, [B_pT2[p2]], [B_QT[t]])
                    else:
                        kt_ = t if da else i
                        k.copy("act", KT[:, :, kt_ * 128:(kt_ + 1) * 128], src, [B_pT2[p2]], [B_KT[kt_]])

            pipeline(len(tiles), [s0, s1_, s2_, s3_, s4_], [0, 1, 2, 3, 4])

        with ExitStack() as sa:
            QaT = sb(sa, "QaT", [128, 4, 2048], BF16)
            KaT = sb(sa, "KaT", [128, 4, 4096], BF16)
            Va = sb(sa, "Va", [128, NT_ALL, 4, 129], BF16)
            B_QaT = [Buf("QaT%d" % t) for t in range(NT_OWN)]
            B_KaT = [Buf("KaT%d" % t) for t in range(NT_ALL)]
            B_Va = [Buf("Va%d" % t) for t in range(NT_ALL)]
            k.memset("pool", Va[:], 1.0, B_Va)

            for _once in ([0] if on("A1") else []):
                with ExitStack() as s1:
                    proj_phase(s1, "da", list(range(NT_ALL)), 0, QaT, KaT, Va, B_QaT, B_KaT, B_Va)
                    dump("A1", "QaT", QaT[:], [128, 4, 2048], BF16)
                    dump("A1", "KaT", KaT[:], [128, 4, 4096], BF16)
                    dump("A1", "Va", Va[:], [128, NT_ALL, 4, 129], BF16)
                    S.barrier()

            for _once in ([0] if on("B1") else []):
                with ExitStack() as s2:
                    NB = 4
                    NBP = 8
                    Pt = [sb(s2, "Pt%d" % i, [128, 512], BF16) for i in range(NBP)]
                    Osb = [sb(s2, "Osb%d" % i, [128, 132], F32) for i in range(4)]
                    t1 = sb(s2, "t1", [128, 4, 128], F32)
                    ob = sb(s2, "ob", [128, 4, 128], F32)
                    sqb = sb(s2, "sqb", [128, 128], F32)
                    sm = sb(s2, "sm", [128, 4, 8], F32)
                    ssq = sb(s2, "ssq", [128, 16], F32)
                    lamb = sb(s2, "lamb", [128, 4, 64], F32)
                    lamw = sb(s2, "lamw", [128, 2, 64], F32)
                    lams = sb(s2, "lams", [128, 8], F32)
                    gsub = sb(s2, "gsub", [128, 128], F32)
                    pS = [ps(s2, "pS%d" % i, [128, 512], F32) for i in range(NB)]
                    pO = [ps(s2, "pO%d" % i, [128, 512], F32) for i in range(4)]
                    B_Pt = [Buf() for _ in range(NBP)]
                    B_pS = [Buf() for _ in range(NB)]
                    B_pO = [Buf() for _ in range(4)]
                    B_Osb = [Buf() for _ in range(4)]
                    B_t1 = [Buf() for _ in range(4)]
                    B_ob = [Buf() for _ in range(4)]
                    B_sm = [Buf() for _ in range(4)]
                    B_lam, B_gs, B_ssq, B_sqb = Buf(), Buf(), Buf(), Buf()
                    for i, n in enumerate(("lam_q1", "lam_k1", "lam_q2", "lam_k2")):
                        k.dma("sp", lamb[:, i, :], bvec(n, 64), (), [B_lam])
                    k.dma("sp", gsub[:], bvec("g_sub", 128), (), [B_gs])
                    k.ts("dve", gsub[:], gsub[:], 0.8, ALU.mult, [B_gs], [B_gs])
                    k.tt("dve", lamw[:, 0, :], lamb[:, 0, :], lamb[:, 1, :], ALU.mult, [B_lam], [B_lam])
                    k.tt("dve", lamw[:, 1, :], lamb[:, 2, :], lamb[:, 3, :], ALU.mult, [B_lam], [B_lam])
                    k.rsum(lams[:, 0:2], lamw[:], [B_lam], [B_lam])
                    k.act(lams[:, 2:4], lams[:, 0:2], AF.Exp, [B_lam], [B_lam])
                    k.tt("dve", lams[:, 4:5], lams[:, 3:4], lams[:, 2:3], ALU.subtract, [B_lam], [B_lam])
                    k.ts("dve", lams[:, 5:6], lams[:, 4:5], -0.2, ALU.add, [B_lam], [B_lam])
                    items = [(h, qg, comp, kt) for h in range(4) for qg in range(4) for comp in range(2) for kt in range(NT_ALL)]
                    QaZ = [sb(s2, "QaZ%d" % c, [128, 4, 2048], BF16) for c in range(2)]
                    B_QaZ = [[Buf() for _ in range(4)] for _ in range(2)]
                    for c in range(2):
                        zr = slice(64, 128) if c == 0 else slice(0, 64)
                        cr = slice(0, 64) if c == 0 else slice(64, 128)
                        k.memset("pool", QaZ[c][zr], 0.0, B_QaZ[c])
                        for qg_ in range(4):
                            k.copy("dve" if c == 0 else "pool", QaZ[c][cr, :, qg_ * 512:(qg_ + 1) * 512],
                                   QaT[cr, :, qg_ * 512:(qg_ + 1) * 512], B_QaT[qg_ * 4:qg_ * 4 + 4], [B_QaZ[c][qg_]])

                    def b1_s0(i):
                        h, qg, comp, kt = items[i]
                        s = i % NB
                        k.mm(pS[s][:], KaT[:, h, kt * 128:(kt + 1) * 128], QaZ[comp][:, h, qg * 512:(qg + 1) * 512],
                             True, True, [B_KaT[kt], B_QaZ[comp][qg]], [B_pS[s]])
                        k.act(Pt[i % NBP][:], pS[s][:], AF.Exp, [B_pS[s]], [B_Pt[i % NBP]], scale=0.125)

                    def b1_s1(i):
                        h, qg, comp, kt = items[i]
                        s = i % NBP
                        for qs in range(4):
                            k.mm(pO[qs][:, 0:129], Pt[s][:, qs * 128:(qs + 1) * 128], Va[:, kt, h, :],
                                 kt == 0, kt == NT_ALL - 1, [B_Pt[s], B_Va[kt]], [B_pO[qs]])
                        if kt != NT_ALL - 1:
                            return
                        for qs in range(4):
                            k.copy("dve", Osb[qs][:, 0:129], pO[qs][:, 0:129], [B_pO[qs]], [B_Osb[qs]])
                        for qs in range(4):
                            O = Osb[qs]
                            bo = B_Osb[qs]
                            if comp == 0:
                                k.recip(sm[:, qs, 0:1], O[:, 128:129], [bo], [B_sm[qs]])
                                k.ts("dve", t1[:, qs, :], O[:, 0:128], sm[:, qs, 0:1], ALU.mult, [bo, B_sm[qs]], [B_t1[qs]])
                            else:
                                k.recip(sm[:, qs, 1:2], O[:, 128:129], [bo], [B_sm[qs]])
                                k.tt("dve", sm[:, qs, 2:3], sm[:, qs, 1:2], lams[:, 5:6], ALU.mult, [B_sm[qs], B_lam], [B_sm[qs]])
                                k.stt("dve", ob[:, qs, :], O[:, 0:128], sm[:, qs, 2:3], t1[:, qs, :], ALU.mult, ALU.add,
                                      [bo, B_sm[qs], B_t1[qs]], [B_ob[qs]])
                                k.tt("dve", sqb[:], ob[:, qs, :], ob[:, qs, :], ALU.mult, [B_ob[qs]], [B_sqb])
                                k.rsum(ssq[:, qs:qs + 1], sqb[:], [B_sqb], [B_ssq])
                        if comp == 1:
                            k.act(ssq[:, 4:8], ssq[:, 0:4], AF.Sqrt, [B_ssq], [B_ssq], scale=1.0 / 128, bias=EPS)
                            k.recip(ssq[:, 8:12], ssq[:, 4:8], [B_ssq], [B_ssq])
                            for qs in range(4):
                                tq = qg * 4 + qs
                                k.stt("dve", o_cat[:, tq, h * 128:(h + 1) * 128], ob[:, qs, :], ssq[:, 8 + qs:9 + qs], gsub[:],
                                      ALU.mult, ALU.mult, [B_ob[qs], B_ssq, B_gs], [B_ocat[tq][0]])

                    pipeline(len(items), [b1_s0, b1_s1], [0, 6])
                    dump("B1", "ocat", o_cat[:], [128, NT_OWN, 1024], BF16)
                    S.barrier()

        with ExitStack() as sa:
            QbT = sb(sa, "QbT", [128, 4, 2048], BF16)
            KbT = sb(sa, "KbT", [128, 4, 20 * 128], BF16)
            Vb = sb(sa, "Vb", [128, 20, 16, 33], BF16)
            B_QbT = [Buf() for _ in range(NT_OWN)]
            B_KbT = [Buf() for _ in range(20)]
            B_Vb = [Buf() for _ in range(20)]
            k.memset("pool", Vb[:], 1.0, B_Vb)
            for _once in ([0] if on("A2") else []):
                with ExitStack() as s1:
                    s2t = [30, 31] + list(range(16)) + [16, 17]
                    proj_phase(s1, "na", s2t, 1536, QbT, KbT, Vb, B_QbT, B_KbT, B_Vb)
                    dump("A2", "QbT", QbT[:], [128, 4, 2048], BF16)
                    dump("A2", "KbT", KbT[:], [128, 4, 2560], BF16)
                    dump("A2", "Vb", Vb[:], [128, 20, 16, 33], BF16)
                    S.barrier()

            for _once in ([0] if on("B2") else []):
                with ExitStack() as s2:
                    NB = 4
                    biasG = sb(s2, "biasG", [128, 4, 5, 128], F32)
                    biasS = [sb(s2, "biasS%d" % i, [128, 4, 6, 128], F32) for i in range(2)]
                    Sb = [sb(s2, "Sb%d" % i, [128, 768], F32) for i in range(2)]
                    Pb = [sb(s2, "Pb%d" % i, [128, 768], BF16) for i in range(NB)]
                    rl = [sb(s2, "rl%d" % i, [128, 4], F32) for i in range(2)]
                    pS = [ps(s2, "pSb%d" % i, [128, 1024], F32) for i in range(2)]
                    pO = [ps(s2, "pOb%d" % i, [128, 4, 128], F32) for i in range(2)]
                    B_bG = [Buf() for _ in range(4)]
                    B_bS = [Buf(), Buf()]
                    B_Sb = [Buf(), Buf()]
                    B_Pb = [Buf() for _ in range(NB)]
                    B_rl = [Buf(), Buf()]
                    B_pS = [Buf(), Buf()]
                    B_pO = [Buf(), Buf()]
                    scale_b = 32 ** -0.5
                    items = [(g, j, hh) for g in range(4) for j in range(NT_OWN) for hh in range(4)]
                    spec_idx = {}
                    c_ = 0
                    for g in range(4):
                        for j in range(NT_OWN):
                            if j in NA_OFFS:
                                spec_idx[(g, j)] = c_ % 2
                                c_ += 1

                    def b2_s0(i):
                        g, j, hh = items[i]
                        if j == 0 and hh == 0:
                            k.dma("sp", biasG[:].rearrange("p a b c -> p (a b c)"), dr["biasG"].ap()[g], (), [B_bG[0]])
                        if j in NA_OFFS:
                            offs = NA_OFFS[j]
                            sp_i = spec_idx[(g, j)]
                            if hh == 0:
                                k.dma("sp", biasS[sp_i][:].rearrange("p a b c -> p (a b c)"),
                                      dr["biasS"].ap()[NA_SPECIAL.index(j), g], (), [B_bS[sp_i]])
                            btile, bbuf = biasS[sp_i], B_bS[sp_i]
                        else:
                            offs = NA_GEN
                            btile, bbuf = biasG, B_bG[0]
                        nof = len(offs)
                        b2 = i % 2
                        pb = i % NB
                        rows = slice(hh * 32, (hh + 1) * 32)
                        for ci, off in enumerate(offs):
                            s_idx = j + off + 2
                            k.mm(pS[b2][:, ci * 128:(ci + 1) * 128], KbT[rows, g, s_idx * 128:(s_idx + 1) * 128],
                                 QbT[rows, g, j * 128:(j + 1) * 128], True, True,
                                 [B_KbT[s_idx], B_QbT[j]], [B_pS[b2]], tp=(hh * 32, 0))
                        k.stt("dve", Sb[b2][:, 0:nof * 128], pS[b2][:, 0:nof * 128], scale_b,
                              btile[:, hh, 0:nof, :].rearrange("p a b -> p (a b)"), ALU.mult, ALU.add,
                              [B_pS[b2], bbuf], [B_Sb[b2]])
                        k.act(Pb[pb][:, 0:nof * 128], Sb[b2][:, 0:nof * 128], AF.Exp, [B_Sb[b2]], [B_Pb[pb]])

                    def b2_s1(i):
                        g, j, hh = items[i]
                        offs = NA_OFFS.get(j, NA_GEN)
                        nof = len(offs)
                        h = g * 4 + hh
                        pb = i % NB
                        oi = (g * NT_OWN + j) % 2
                        for ci, off in enumerate(offs):
                            s_idx = j + off + 2
                            k.mm(pO[oi][:, hh, 0:33], Pb[pb][:, ci * 128:(ci + 1) * 128], Vb[:, s_idx, h, :],
                                 ci == 0, ci == nof - 1, [B_Pb[pb], B_Vb[s_idx]], [B_pO[oi]])
                        if hh == 3:
                            k.recip(rl[oi][:].unsqueeze(2), pO[oi][:, :, 32:33], [B_pO[oi]], [B_rl[oi]])
                            k.tt("dve", o_cat[:, j, 512 + g * 128:512 + (g + 1) * 128].rearrange("p (a b) -> p a b", b=32),
                                 pO[oi][:, :, 0:32], bc(rl[oi][:].unsqueeze(2), [128, 4, 32]), ALU.mult,
                                 [B_pO[oi], B_rl[oi]], [B_ocat[j][1]])

                    pipeline(len(items), [b2_s0, b2_s1], [0, 3])
                    dump("B2", "ocat", o_cat[:], [128, NT_OWN, 1024], BF16)
                    S.barrier()

        hres = sb(es, "hres", [128, NT_OWN, 1024], F32)
        B_h = [[Buf(), Buf()] for _ in range(NT_OWN)]
        for _once in ([0] if on("C") else []):
            with ExitStack() as s1:
                w_sb = sb(s1, "w_outb", [128, 8, 1024], BF16)
                xt = [sb(s1, "xtc%d" % i, [128, 1024], F32) for i in range(2)]
                oT = [sb(s1, "oT%d" % i, [128, 8, 128], BF16) for i in range(2)]
                pT = [ps(s1, "pTc%d" % i, [128, 1024], BF16) for i in range(2)]
                pP = [ps(s1, "pPc%d" % i, [128, 512], F32) for i in range(3)]
                B_w = Buf()
                B_xt = [Buf(), Buf()]
                B_oT = [Buf(), Buf()]
                B_pT = [Buf(), Buf()]
                B_pP = [Buf() for _ in range(3)]
                k.dma("pool", w_sb[:], dr["w_out"].ap().rearrange("(kc p) n -> p kc n", p=128), (), [B_w])
                cntc = {"pp": 0}

                def c_s0(t):
                    par = t % 2
                    k.dma("sp", xt[par][:], x_ap[t * 128:(t + 1) * 128, :], (), [B_xt[par]])
                    for kc in range(8):
                        k.tr(pT[par][:, kc * 128:(kc + 1) * 128], o_cat[:, t, kc * 128:(kc + 1) * 128], idb[:],
                             [B_ocat[t][0], B_ocat[t][1], B_id], [B_pT[par]])
                    k.copy("act", oT[par][:].rearrange("p a b -> p (a b)"), pT[par][:], [B_pT[par]], [B_oT[par]])

                def c_s1(t):
                    par = t % 2
                    for cg in range(2):
                        pp = cntc["pp"] % 3
                        cntc["pp"] += 1
                        for kc in range(8):
                            k.mm(pP[pp][:], oT[par][:, kc, :], w_sb[:, kc, cg * 512:(cg + 1) * 512], kc == 0, kc == 7,
                                 [B_oT[par], B_w], [B_pP[pp]])
                        k.tt("dve", hres[:, t, cg * 512:(cg + 1) * 512], pP[pp][:], xt[par][:, cg * 512:(cg + 1) * 512], ALU.add,
                             [B_pP[pp], B_xt[par]], [B_h[t][cg]])

                pipeline(NT_OWN, [c_s0, c_s1], [0, 1])
                dump("C", "hres", hres[:], [128, NT_OWN, 1024], F32)
                S.barrier()

        with ExitStack() as sd:
            xnT = sb(sd, "xnT", [128, 8, 2048], BF16)
            gates = sb(sd, "gates", [128, NT_OWN, 32], F32)
            B_xnT = [Buf() for _ in range(NT_OWN)]
            B_gates = [Buf() for _ in range(NT_OWN)]
            for _once in ([0] if on("D1") else []):
                with ExitStack() as s1:
                    gffn = sb(s1, "gffn", [128, 1024], F32)
                    w_r = sb(s1, "w_r", [128, 8, 36], F32)
                    xn = [sb(s1, "xn%d" % i, [128, 1024], F32) for i in range(2)]
                    junk = sb(s1, "junkd", [128, 1024], BF16)
                    stat = [sb(s1, "statd%d" % i, [128, 4], F32) for i in range(2)]
                    xT32 = [sb(s1, "xT32%d" % i, [128, 8, 128], F32) for i in range(2)]
                    rt = [sb(s1, "rt%d" % i, [128, 128], F32) for i in range(2)]
                    pT = [ps(s1, "pTd%d" % i, [128, 1024], F32) for i in range(2)]
                    pL = [ps(s1, "pL%d" % i, [128, 512], F32) for i in range(2)]
                    B_g, B_wr = Buf(), Buf()
                    B_xn = [Buf(), Buf()]
                    B_st = [Buf(), Buf()]
                    B_xT = [Buf(), Buf()]
                    B_rt = [Buf(), Buf()]
                    B_pT = [Buf(), Buf()]
                    B_pL = [Buf(), Buf()]
                    k.dma("sp", gffn[:], bvec("g_ffn", 1024), (), [B_g])
                    k.dma("sp", w_r[:], dr["w_r"].ap().rearrange("(kc p) n -> p kc n", p=128), (), [B_wr])

                    def d1_s0(t):
                        par = t % 2
                        hb = B_h[t]
                        k.act(junk[:], hres[:, t, :], AF.Square, hb, [B_st[par]], accum=stat[par][:, 0:1])
                        k.act(stat[par][:, 1:2], stat[par][:, 0:1], AF.Sqrt, [B_st[par]], [B_st[par]], scale=1.0 / 1024, bias=EPS)
                        k.recip(stat[par][:, 2:3], stat[par][:, 1:2], [B_st[par]], [B_st[par]])
                        k.stt("dve", xn[par][:], hres[:, t, :], stat[par][:, 2:3], gffn[:], ALU.mult, ALU.mult,
                              hb + [B_st[par], B_g], [B_xn[par]])

                    def d1_s1(t):
                        par = t % 2
                        for kc in range(8):
                            k.tr(pT[par][:, kc * 128:(kc + 1) * 128], xn[par][:, kc * 128:(kc + 1) * 128], idf[:],
                                 [B_xn[par], B_id], [B_pT[par]])
                        k.copy("act", xT32[par][:].rearrange("p a b -> p (a b)"), pT[par][:], [B_pT[par]], [B_xT[par], B_pT[par]])
                        k.copy("dve", xnT[:, :, t * 128:(t + 1) * 128], pT[par][:].rearrange("p (a b) -> p a b", b=128),
                               [B_pT[par]], [B_xnT[t]])

                    def d1_s2_chain(t):
                        par = t % 2
                        r = rt[par]
                        br = [B_rt[par]]
                        e3 = lambda ap: ap.rearrange("p (e g) -> p e g", g=4)

                        def _mm():
                            for kc in range(8):
                                k.mm(pL[par][:, 0:36], xT32[par][:, kc, :], w_r[:, kc, :], kc == 0, kc == 7,
                                     [B_xT[par], B_wr], [B_pL[par]])
                        return [
                            _mm,
                            lambda: k.copy("dve", r[:, 0:36], pL[par][:, 0:36], [B_pL[par]], br),
                            lambda: k.rmax(r[:, 40:41], r[:, 0:4], br, br),
                            lambda: k.ts("dve", r[:, 44:48], r[:, 0:4], r[:, 40:41], ALU.is_ge, br, br),
                            lambda: k.ts("dve", r[:, 48:52], r[:, 0:4], r[:, 40:41], ALU.subtract, br, br),
                            lambda: k.act(r[:, 52:56], r[:, 48:52], AF.Exp, br, br),
                            lambda: k.rsum(r[:, 41:42], r[:, 52:56], br, br),
                            lambda: k.recip(r[:, 42:43], r[:, 41:42], br, br),
                            lambda: k.tt("dve", e3(r[:, 64:96]), r[:, 4:36].rearrange("p (g e) -> p e g", e=8),
                                         bc(r[:, 44:48].unsqueeze(1), [128, 8, 4]), ALU.mult, br, br),
                            lambda: k.rsum(r[:, 56:64], e3(r[:, 64:96]), br, br),
                            lambda: k.rmax(r[:, 96:97], r[:, 56:64], br, br),
                            lambda: k.ts("dve", r[:, 100:108], r[:, 56:64], r[:, 96:97], ALU.is_ge, br, br),
                            lambda: k.stt("dve", r[:, 108:116], r[:, 100:108], -1e30, r[:, 56:64], ALU.mult, ALU.add, br, br),
                            lambda: k.rmax(r[:, 97:98], r[:, 108:116], br, br),
                            lambda: k.ts("dve", r[:, 116:124], r[:, 56:64], r[:, 97:98], ALU.is_ge, br, br),
                            lambda: k.ts("dve", r[:, 100:108], r[:, 56:64], r[:, 96:97], ALU.subtract, br, br),
                            lambda: k.act(r[:, 100:108], r[:, 100:108], AF.Exp, br, br),
                            lambda: k.tt("dve", r[:, 100:108], r[:, 100:108], r[:, 116:124], ALU.mult, br, br),
                            lambda: k.rsum(r[:, 98:99], r[:, 100:108], br, br),
                            lambda: k.recip(r[:, 99:100], r[:, 98:99], br, br),
                            lambda: k.tt("dve", r[:, 99:100], r[:, 99:100], r[:, 42:43], ALU.mult, br, br),
                            lambda: k.ts("dve", r[:, 100:108], r[:, 100:108], r[:, 99:100], ALU.mult, br, br),
                            lambda: k.tt("dve", gates[:, t, :].rearrange("p (g e) -> p g e", e=8),
                                         bc(r[:, 44:48].unsqueeze(2), [128, 4, 8]), bc(r[:, 100:108].unsqueeze(1), [128, 4, 8]),
                                         ALU.mult, br, [B_gates[t]]),
                        ]

                    for T in range(NT_OWN + 3):
                        ch = []
                        if T >= 3 and (T - 3) % 2 == 0:
                            ch = [d1_s2_chain(T - 3), d1_s2_chain(T - 2)]
                        for j in range(max([len(c) for c in ch] + [0])):
                            for c in ch:
                                if j < len(c):
                                    c[j]()
                        if 0 <= T - 1 < NT_OWN:
                            d1_s1(T - 1)
                        if 0 <= T < NT_OWN:
                            d1_s0(T)
                    dump("D1", "gates", gates[:], [128, NT_OWN, 32], F32)
                    dump("D1", "xnT", xnT[:], [128, 8, 2048], BF16)
                    S.barrier()

            for _once in ([0] if on("D2") else []):
                with ExitStack() as s2:
                    w1s = [sb(s2, "w1s%d" % i, [128, 8, 512], BF16) for i in range(2)]
                    w3s = [sb(s2, "w3s%d" % i, [128, 8, 512], BF16) for i in range(2)]
                    w2s = [sb(s2, "w2s%d" % i, [128, 4, 1024], BF16) for i in range(2)]
                    hdn = [sb(s2, "hdn%d" % i, [128, 4, 512], BF16) for i in range(2)]
                    sil = [sb(s2, "sil%d" % i, [128, 512], F32) for i in range(2)]
                    p1 = [ps(s2, "p1_%d" % i, [128, 512], F32) for i in range(2)]
                    p3 = [ps(s2, "p3_%d" % i, [128, 512], F32) for i in range(2)]
                    pY = [ps(s2, "pY%d" % i, [128, 512], F32) for i in range(3)]
                    B_w1 = [Buf(), Buf()]
                    B_w3 = [Buf(), Buf()]
                    B_w2 = [Buf(), Buf()]
                    B_hdn = [[Buf() for _ in range(4)] for _ in range(2)]
                    B_sil = [Buf(), Buf()]
                    B_p1 = [Buf(), Buf()]
                    B_p3 = [Buf(), Buf()]
                    B_pY = [Buf() for _ in range(3)]
                    w1v = dr["w1"].ap()
                    w3v = dr["w3"].ap()
                    w2v = dr["w2"].ap()
                    cntd = {"p": 0, "y": 0}

                    def load_w(e):
                        wb = e % 2
                        k.dma("pool", w1s[wb][:], w1v[e].rearrange("(kc p) n -> p kc n", p=128), (), [B_w1[wb]])
                        k.dma("pool", w3s[wb][:], w3v[e].rearrange("(kc p) n -> p kc n", p=128), (), [B_w3[wb]])
                        k.dma("pool", w2s[wb][:], w2v[e].rearrange("(kc p) n -> p kc n", p=128), (), [B_w2[wb]])

                    load_w(0)

                    def d2_s0(i):
                        e, tg = i // 4, i % 4
                        wb = e % 2
                        hb = i % 2
                        if tg == 0 and e + 1 < 32:
                            load_w(e + 1)
                        for hc in range(4):
                            pb = cntd["p"] % 2
                            cntd["p"] += 1
                            for kc in range(8):
                                k.mm(p1[pb][:], w1s[wb][:, kc, hc * 128:(hc + 1) * 128], xnT[:, kc, tg * 512:(tg + 1) * 512],
                                     kc == 0, kc == 7, [B_w1[wb]] + B_xnT[tg * 4:tg * 4 + 4], [B_p1[pb]])
                            for kc in range(8):
                                k.mm(p3[pb][:], w3s[wb][:, kc, hc * 128:(hc + 1) * 128], xnT[:, kc, tg * 512:(tg + 1) * 512],
                                     kc == 0, kc == 7, [B_w3[wb]] + B_xnT[tg * 4:tg * 4 + 4], [B_p3[pb]])
                            k.act(sil[pb][:], p1[pb][:], AF.Silu, [B_p1[pb]], [B_sil[pb]])
                            k.tt("dve", hdn[hb][:, hc, :], sil[pb][:], p3[pb][:], ALU.mult, [B_sil[pb], B_p3[pb]], [B_hdn[hb][hc]])

                    def d2_s1(i):
                        e, tg = i // 4, i % 4
                        wb = e % 2
                        hb = i % 2
                        for tt_ in range(4):
                            t = tg * 4 + tt_
                            for cg in range(2):
                                yb = cntd["y"] % 3
                                cntd["y"] += 1
                                for hc in range(4):
                                    k.mm(pY[yb][:], hdn[hb][:, hc, tt_ * 128:(tt_ + 1) * 128], w2s[wb][:, hc, cg * 512:(cg + 1) * 512],
                                         hc == 0, hc == 3, [B_hdn[hb][hc], B_w2[wb]], [B_pY[yb]])
                                k.stt("dve", hres[:, t, cg * 512:(cg + 1) * 512], pY[yb][:], gates[:, t, e:e + 1],
                                      hres[:, t, cg * 512:(cg + 1) * 512], ALU.mult, ALU.add,
                                      [B_pY[yb], B_gates[t], B_h[t][cg]], [B_h[t][cg]])

                    pipeline(128, [d2_s0, d2_s1], [0, 1])
                    dump("D2", "hres", hres[:], [128, NT_OWN, 1024], F32)
                    S.barrier()

        out_toks = []
        for _once in ([0] if on("E") else []):
            with ExitStack() as s1:
                wple = sb(s1, "wple", [128, 2, 1024], BF16)
                wplg = sb(s1, "wplg", [128, 8, 1024], BF16)
                gplg = sb(s1, "gplg", [128, 1024], F32)
                gple = sb(s1, "gple", [128, 1024], F32)
                pt = [sb(s1, "pt%d" % i, [128, 256], F32) for i in range(2)]
                ptb = [sb(s1, "ptb%d" % i, [128, 256], BF16) for i in range(2)]
                pTs = [sb(s1, "pTs%d" % i, [128, 2, 128], BF16) for i in range(2)]
                hn = [sb(s1, "hn%d" % i, [128, 1024], BF16) for i in range(2)]
                hT = [sb(s1, "hT%d" % i, [128, 8, 128], BF16) for i in range(2)]
                junk = sb(s1, "junke", [128, 1024], BF16)
                statA = [sb(s1, "stateA%d" % i, [128, 4], F32) for i in range(2)]
                statB = [sb(s1, "stateB%d" % i, [128, 4], F32) for i in range(2)]
                pe_s = [sb(s1, "pe_s%d" % i, [128, 1024], F32) for i in range(2)]
                sg = [sb(s1, "sg%d" % i, [128, 1024], F32) for i in range(2)]
                yo = [sb(s1, "yo%d" % i, [128, 1024], F32) for i in range(2)]
                pTp = ps(s1, "pTp", [128, 1024], BF16)
                pTh = ps(s1, "pTh", [128, 1024], BF16)
                pE = ps(s1, "pE", [128, 1024], F32)
                pG = ps(s1, "pG", [128, 1024], F32)
                B = {n: [Buf(), Buf()] for n in ("pt", "ptb", "pTs", "hn", "hT", "stA", "stB", "pe_s", "sg", "yo")}
                B_pTp, B_pTh, B_pE, B_pG = Buf(), Buf(), [Buf(), Buf()], [Buf(), Buf()]
                B_wple, B_wplg, B_g1, B_g2 = Buf(), Buf(), Buf(), Buf()
                k.dma("pool", wple[:], dr["w_ple"].ap().rearrange("(kc p) n -> p kc n", p=128), (), [B_wple])
                k.dma("pool", wplg[:], dr["w_plg"].ap().rearrange("(kc p) n -> p kc n", p=128), (), [B_wplg])
                k.dma("sp", gplg[:], bvec("g_plg", 1024), (), [B_g1])
                k.dma("sp", gple[:], bvec("g_ple", 1024), (), [B_g2])

                def e_s0(t):
                    par = t % 2
                    hb = B_h[t]
                    st_ = statA[par]
                    bs = [B["stA"][par]]
                    k.dma("sp", pt[par][:], p_ap[t * 128:(t + 1) * 128, :], (), [B["pt"][par]])
                    k.copy("dve", ptb[par][:], pt[par][:], [B["pt"][par]], [B["ptb"][par]])
                    k.act(junk[:], hres[:, t, :], AF.Square, hb, bs, accum=st_[:, 0:1])
                    k.act(st_[:, 1:2], st_[:, 0:1], AF.Sqrt, bs, bs, scale=1.0 / 1024, bias=EPS)
                    k.recip(st_[:, 2:3], st_[:, 1:2], bs, bs)
                    k.stt("dve", hn[par][:], hres[:, t, :], st_[:, 2:3], gplg[:], ALU.mult, ALU.mult, hb + bs + [B_g1], [B["hn"][par]])

                def e_s1(t):
                    par = t % 2
                    for kc in range(2):
                        k.tr(pTp[:, kc * 128:(kc + 1) * 128], ptb[par][:, kc * 128:(kc + 1) * 128], idb[:], [B["ptb"][par], B_id], [B_pTp])
                    k.copy("act", pTs[par][:].rearrange("p a b -> p (a b)"), pTp[:, 0:256], [B_pTp], [B["pTs"][par]])
                    for kc in range(8):
                        k.tr(pTh[:, kc * 128:(kc + 1) * 128], hn[par][:, kc * 128:(kc + 1) * 128], idb[:], [B["hn"][par], B_id], [B_pTh])
                    k.copy("act", hT[par][:].rearrange("p a b -> p (a b)"), pTh[:], [B_pTh], [B["hT"][par]])

                def e_s2(t):
                    par = t % 2
                    for cg in range(2):
                        for kc in range(2):
                            k.mm(pE[:, cg * 512:(cg + 1) * 512], pTs[par][:, kc, :], wple[:, kc, cg * 512:(cg + 1) * 512], kc == 0, kc == 1,
                                 [B["pTs"][par], B_wple], [B_pE[cg]])
                    k.copy("act", pe_s[par][:], pE[:], B_pE, [B["pe_s"][par]])
                    for cg in range(2):
                        for kc in range(8):
                            k.mm(pG[:, cg * 512:(cg + 1) * 512], hT[par][:, kc, :], wplg[:, kc, cg * 512:(cg + 1) * 512], kc == 0, kc == 7,
                                 [B["hT"][par], B_wplg], [B_pG[cg]])
                    k.act(sg[par][:], pG[:], AF.Sigmoid, B_pG, [B["sg"][par]])

                def e_s3(t):
                    par = t % 2
                    hb = B_h[t]
                    st_ = statB[par]
                    bs = [B["stB"][par]]
                    k.act(junk[:], pe_s[par][:], AF.Square, [B["pe_s"][par]], bs, accum=st_[:, 0:1])
                    k.act(st_[:, 1:2], st_[:, 0:1], AF.Sqrt, bs, bs, scale=1.0 / 1024, bias=EPS)
                    k.recip(st_[:, 2:3], st_[:, 1:2], bs, bs)
                    k.stt("dve", pe_s[par][:], pe_s[par][:], st_[:, 2:3], gple[:], ALU.mult, ALU.mult,
                          [B["pe_s"][par], B_g2] + bs, [B["pe_s"][par]])
                    k.tt("dve", sg[par][:], sg[par][:], pe_s[par][:], ALU.mult, [B["sg"][par], B["pe_s"][par]], [B["sg"][par]])
                    k.tt("dve", yo[par][:], sg[par][:], hres[:, t, :], ALU.add, [B["sg"][par]] + hb, [B["yo"][par]])
                    out_toks.append(k.dma("sp", y_ap[t * 128:(t + 1) * 128, :], yo[par][:], [B["yo"][par]], ()))

                pipeline(NT_OWN, [e_s0, e_s1, e_s2, e_s3], [0, 1, 2, 3])
                S.barrier()
        S.ops["sp"].append(Op(None, list(out_toks) + dbg_toks, None))
        S.emit()
    return nc


def _rope_tables():
    inv = (np.float32(10000.0) ** (-np.arange(0, 64, 2, dtype=np.float32) / np.float32(64))).astype(np.float32)
    ang = np.arange(4096, dtype=np.float32)[:, None] * inv[None, :]
    return np.cos(ang).astype(np.float32), np.sin(ang).astype(np.float32)


def _bias_tables(rpb, hf):
    kp = np.arange(128)
    kpar, kcol = kp // 64, kp % 64
    qpar, qcol = kp // 64, kp % 64
    cs = np.clip(qcol - 8, 0, 48)
    col_valid = (kcol[:, None] >= cs[None, :]) & (kcol[:, None] < cs[None, :] + 16)
    col_off = np.clip(kcol[:, None] - qcol[None, :] + 15, 0, 30)

    def tile(j, off, hf_):
        r = hf_ * 32 + 2 * j + qpar
        kr = hf_ * 32 + 2 * (j + off) + kpar
        rs = np.clip(r - 4, 0, 56)
        row_valid = (kr[:, None] >= rs[None, :]) & (kr[:, None] <= rs[None, :] + 7) & (kr[:, None] >= 0) & (kr[:, None] <= 63)
        row_off = np.clip(kr[:, None] - r[None, :] + 7, 0, 14)
        valid = row_valid & col_valid
        vals = rpb[:, row_off, col_off]
        return np.where(valid[None], vals, np.float32(NEG)).astype(np.float32)

    G = np.zeros((4, 128, 4, 5, 128), np.float32)
    for oi, off in enumerate(NA_GEN):
        tl = tile(8, off, 0)
        for g in range(4):
            G[g, :, :, oi, :] = tl[g * 4:(g + 1) * 4].transpose(1, 0, 2)
    Sp = np.full((4, 4, 128, 4, 6, 128), np.float32(NEG), np.float32)
    for ji, j in enumerate(NA_SPECIAL):
        for oi, off in enumerate(NA_OFFS[j]):
            tl = tile(j, off, hf)
            for g in range(4):
                Sp[ji, g, :, :, oi, :] = tl[g * 4:(g + 1) * 4].transpose(1, 0, 2)
    return G.reshape(4, 128, 4 * 5 * 128), Sp.reshape(4, 4, 128, 4 * 6 * 128)


_NC_CACHE = {}


def prep_inputs(x, p, g_mix, w_in, g_qa, g_ka, lam_q1, lam_k1, lam_q2, lam_k2, g_sub, g_qb, g_kb,
           rpb, w_out, g_ffn, w_rg, w_re, w1, w3, w2, g_plg, w_plg, w_ple, g_ple):
    f = lambda a: np.ascontiguousarray(np.asarray(a, dtype=np.float32))
    x = f(x); p = f(p)
    cos, sin = _rope_tables()
    common = {
        "w_in": f(w_in)[0], "w_out": f(w_out)[0], "w_plg": f(w_plg)[0], "w_ple": f(w_ple)[0],
        "w_r": np.ascontiguousarray(np.concatenate([f(w_rg)[0], f(w_re)[0]], axis=1)),
        "w1": f(w1)[0], "w3": f(w3)[0], "w2": f(w2)[0],
        "g_mix": f(g_mix), "g_ffn": f(g_ffn), "g_plg": f(g_plg), "g_ple": f(g_ple),
        "g_qa": f(g_qa), "g_ka": f(g_ka), "lam_q1": f(lam_q1), "lam_k1": f(lam_k1),
        "lam_q2": f(lam_q2), "lam_k2": f(lam_k2), "g_sub": f(g_sub), "g_qb": f(g_qb), "g_kb": f(g_kb),
        "ident": np.eye(128, dtype=np.float32),
    }
    rpb0 = f(rpb)[0]
    in_maps = []
    for c in range(8):
        b, hf = c // 2, c % 2
        own = slice(hf * 2048, (hf + 1) * 2048)
        oth = slice((1 - hf) * 2048, (2 - hf) * 2048)
        xc = np.ascontiguousarray(np.concatenate([x[b, own], x[b, oth]], axis=0))
        pos = np.concatenate([np.arange(4096)[own], np.arange(4096)[oth]])
        cc = np.concatenate([cos[pos], cos[pos]], axis=1)
        ss = np.concatenate([-sin[pos], sin[pos]], axis=1)
        cs_t = np.ascontiguousarray(cc.reshape(32, 128, 64).transpose(1, 0, 2).reshape(128, 32 * 64))
        sn_t = np.ascontiguousarray(ss.reshape(32, 128, 64).transpose(1, 0, 2).reshape(128, 32 * 64))
        G, Sp = _bias_tables(rpb0, hf)
        m = dict(common)
        m.update({"x": xc, "p": np.ascontiguousarray(p[0, b, own]), "cs": cs_t, "sn": sn_t, "biasG": G, "biasS": Sp})
        in_maps.append(m)
    return in_maps


def kernel(**inputs):
    in_maps = prep_inputs(**inputs)
    if "nc" not in _NC_CACHE:
        _NC_CACHE["nc"] = build_program()
    nc = _NC_CACHE["nc"]
    res = run_bass_kernel_spmd(nc, in_maps, core_ids=list(range(8)))
    out = np.zeros((4, 4096, 1024), np.float32)
    for c in range(8):
        b, hf = c // 2, c % 2
        out[b, hf * 2048:(hf + 1) * 2048] = res.results[c]["y"]
    return out
```

```python
import math
import numpy as np
import concourse.bass as bass
import concourse.mybir as mybir
from contextlib import ExitStack
from concourse.bass_utils import run_bass_kernel_spmd

F32 = mybir.dt.float32
BF16 = mybir.dt.bfloat16
AF = mybir.ActivationFunctionType
ALU = mybir.AluOpType
AX = mybir.AxisListType

COMPUTE = ("pe", "act", "dve", "pool")
ALL_ENG = ("pe", "act", "dve", "pool", "sp")
N_DMA_SEMS = 24
EPS = 1e-6
NEG = -1e30
NT_OWN = 16
NT_ALL = 32


class Tok:
    __slots__ = ("eng", "idx", "needed", "sem", "val")

    def __init__(self, eng, idx):
        self.eng = eng
        self.idx = idx
        self.needed = False
        self.sem = None
        self.val = None


class Buf:
    __slots__ = ("name", "w", "r")

    def __init__(self, name=""):
        self.name = name
        self.w = None
        self.r = {}


class Op:
    __slots__ = ("fn", "waits", "tok")

    def __init__(self, fn, waits, tok):
        self.fn = fn
        self.waits = waits
        self.tok = tok


class Sched:
    def __init__(self, nc, es):
        self.nc = nc
        self.ops = {e: [] for e in ALL_ENG}
        self.waited = {e: {} for e in ALL_ENG}
        self.eng_sem = {e: es.enter_context(nc.semaphore("s_" + e)) for e in COMPUTE}
        self.dma_sems = [es.enter_context(nc.semaphore("s_dma%d" % i)) for i in range(N_DMA_SEMS)]
        self.dma_cnt = [0] * N_DMA_SEMS
        self.dma_last = [None] * N_DMA_SEMS
        self.dma_rr = 0
        self.dma_rr_sw = 0
        self.dma_rr_sw = 0
        self.n_dma = 0
        self.last_tok = {e: None for e in COMPUTE}

    def _need(self, eng, t, out):
        wd = self.waited[eng]
        if t.eng in COMPUTE:
            key = t.eng
            if wd.get(key, -1) >= t.idx:
                return
            wd[key] = t.idx
        else:
            key = t.sem
            if wd.get(key, -1) >= t.val:
                return
            wd[key] = t.val
        t.needed = True
        out.append(t)

    def _collect(self, eng, reads, writes, is_dma):
        out = []
        for b in reads:
            t = b.w
            if t is not None:
                if t.eng == eng and not is_dma and eng == "pe":
                    continue
                self._need(eng, t, out)
        for b in writes:
            t = b.w
            if t is not None:
                if not (t.eng == eng and not is_dma and eng == "pe"):
                    self._need(eng, t, out)
            for t in b.r.values():
                if t.eng == eng and not is_dma and eng == "pe":
                    continue
                self._need(eng, t, out)
        return out

    def op(self, eng, fn, reads=(), writes=()):
        waits = self._collect(eng, reads, writes, False)
        tok = Tok(eng, len(self.ops[eng]))
        self.ops[eng].append(Op(fn, waits, tok))
        self.last_tok[eng] = tok
        for b in reads:
            b.r[eng] = tok
        for b in writes:
            b.w = tok
            b.r = {}
        return tok

    def dma(self, queue, fn, reads=(), writes=()):
        waits = self._collect(queue, reads, writes, True)
        if queue == "pool":
            i = 16 + self.dma_rr_sw
            self.dma_rr_sw = (self.dma_rr_sw + 1) % (N_DMA_SEMS - 16)
        else:
            i = self.dma_rr
            self.dma_rr = (self.dma_rr + 1) % 16
        prev = self.dma_last[i]
        if prev is not None:
            self._need(queue, prev, waits)
        self.dma_cnt[i] += 1
        tok = Tok("dma", self.n_dma)
        self.n_dma += 1
        tok.sem = self.dma_sems[i]
        tok.val = 16 * self.dma_cnt[i]
        tok.needed = True
        self.dma_last[i] = tok
        self.ops[queue].append(Op(fn, waits, tok))
        for b in reads:
            b.r[("dma", tok.idx)] = tok
        for b in writes:
            b.w = tok
            b.r = {}
        return tok

    def barrier(self):
        toks = [self.last_tok[e] for e in COMPUTE if self.last_tok[e] is not None]
        toks += [t for t in self.dma_last if t is not None]
        for e in ALL_ENG:
            waits = []
            for t in toks:
                if t.eng == e:
                    continue
                self._need(e, t, waits)
            if waits:
                self.ops[e].append(Op(None, waits, None))

    def emit(self):
        nc = self.nc
        for e in COMPUTE:
            c = 0
            for o in self.ops[e]:
                t = o.tok
                if t is not None and t.eng == e and t.needed:
                    c += 1
                    t.sem = self.eng_sem[e]
                    t.val = c

        def run(e, eng):
            for o in self.ops[e]:
                for t in o.waits:
                    eng.wait_ge(t.sem, t.val)
                if o.fn is None:
                    continue
                ins = o.fn(eng)
                t = o.tok
                if t.eng == "dma":
                    ins.then_inc(t.sem, 16)
                elif t.needed:
                    ins.then_inc(t.sem, 1)

        with nc.Block() as block:
            @block.tensor
            def _(eng):
                run("pe", eng)

            @block.scalar
            def _(eng):
                run("act", eng)

            @block.vector
            def _(eng):
                run("dve", eng)

            @block.gpsimd
            def _(eng):
                run("pool", eng)

            @block.sync
            def _(eng):
                run("sp", eng)


class K:
    def __init__(self, S):
        self.S = S

    def mm(self, out, lhsT, rhs, start, stop, R, W, tp=None):
        if tp is None:
            f = lambda e: e.matmul(out, lhsT=lhsT, rhs=rhs, start=start, stop=stop)
        else:
            f = lambda e: e.matmul(out, lhsT=lhsT, rhs=rhs, start=start, stop=stop, tile_position=tp)
        return self.S.op("pe", f, R, W)

    def tr(self, out, in_, ident, R, W):
        return self.S.op("pe", lambda e: e.transpose(out=out, in_=in_, identity=ident), R, W)

    def act(self, out, in_, func, R, W, scale=1.0, bias=0.0, accum=None):
        if accum is None:
            f = lambda e: e.activation(out=out, in_=in_, func=func, bias=bias, scale=scale)
        else:
            f = lambda e: e.activation(out=out, in_=in_, func=func, bias=bias, scale=scale, accum_out=accum)
        return self.S.op("act", f, R, W)

    def tt(self, eng, out, in0, in1, op, R, W):
        return self.S.op(eng, lambda e: e.tensor_tensor(out=out, in0=in0, in1=in1, op=op), R, W)

    def ts(self, eng, out, in0, s1, op0, R, W, s2=None, op1=None):
        if op1 is None:
            f = lambda e: e.tensor_scalar(out=out, in0=in0, scalar1=s1, scalar2=None, op0=op0)
        else:
            f = lambda e: e.tensor_scalar(out=out, in0=in0, scalar1=s1, scalar2=s2, op0=op0, op1=op1)
        return self.S.op(eng, f, R, W)

    def stt(self, eng, out, in0, scalar, in1, op0, op1, R, W):
        return self.S.op(eng, lambda e: e.scalar_tensor_tensor(out=out, in0=in0, scalar=scalar, in1=in1, op0=op0, op1=op1), R, W)

    def rsum(self, out, in_, R, W):
        return self.S.op("dve", lambda e: e.reduce_sum(out=out, in_=in_, axis=AX.X), R, W)

    def rmax(self, out, in_, R, W):
        return self.S.op("dve", lambda e: e.reduce_max(out=out, in_=in_, axis=AX.X), R, W)

    def recip(self, out, in_, R, W):
        return self.S.op("dve", lambda e: e.reciprocal(out=out, in_=in_), R, W)

    def copy(self, eng, out, in_, R, W):
        if eng == "act":
            return self.S.op("act", lambda e: e.activation(out=out, in_=in_, func=AF.Copy), R, W)
        return self.S.op(eng, lambda e: e.tensor_copy(out=out, in_=in_), R, W)

    def memset(self, eng, out, val, W):
        return self.S.op(eng, lambda e: e.memset(out, val), (), W)

    def dma(self, queue, out, in_, R, W):
        return self.S.dma(queue, lambda e: e.dma_start(out=out, in_=in_), R, W)


def bc(ap, shape):
    return ap.broadcast_to(shape)


NA_OFFS = {0: [-2, -1, 0, 1, 2, 3], 1: [-2, -1, 0, 1, 2], 14: [-2, -1, 0, 1, 2], 15: [-3, -2, -1, 0, 1, 2]}
NA_GEN = [-2, -1, 0, 1, 2]
NA_SPECIAL = [0, 1, 14, 15]


PHASES = ["A1", "B1", "A2", "B2", "C", "D1", "D2", "E"]


def build_program(stop=None):
    nc = bass.Bass("TRN2", target_bir_lowering=False)
    dr = {}
    last = len(PHASES) - 1 if stop is None else PHASES.index(stop)

    def on(ph):
        return PHASES.index(ph) <= last

    def din(name, shape):
        dr[name] = nc.dram_tensor(name, list(shape), F32, kind="ExternalInput")
        return dr[name]

    din("x", [4096, 1024])
    din("p", [2048, 256])
    din("cs", [128, 32 * 64])
    din("sn", [128, 32 * 64])
    din("w_in", [1024, 3072])
    din("w_out", [1024, 1024])
    din("w_plg", [1024, 1024])
    din("w_ple", [256, 1024])
    din("w_r", [1024, 36])
    din("w1", [32, 1024, 512])
    din("w3", [32, 1024, 512])
    din("w2", [32, 512, 1024])
    for n in ("g_mix", "g_ffn", "g_plg", "g_ple"):
        din(n, [1, 1024])
    for n in ("g_qa", "g_ka", "lam_q1", "lam_k1", "lam_q2", "lam_k2"):
        din(n, [1, 64])
    din("g_sub", [1, 128])
    din("g_qb", [1, 32])
    din("g_kb", [1, 32])
    din("ident", [128, 128])
    din("biasG", [4, 128, 4 * 5 * 128])
    din("biasS", [4, 4, 128, 4 * 6 * 128])
    y = nc.dram_tensor("y", [2048, 1024], F32, kind="ExternalOutput")

    x_ap = dr["x"].ap()
    p_ap = dr["p"].ap()
    y_ap = y.ap()

    def bvec(name, n):
        return bass.AP(dr[name], 0, [[0, 128], [1, n]])

    with ExitStack() as es:
        S = Sched(nc, es)
        k = K(S)

        uid = [0]

        def sb(stack, name, shape, dt):
            uid[0] += 1
            return stack.enter_context(nc.sbuf_tensor("%s_%d" % (name, uid[0]), shape, dt))

        def ps(stack, name, shape, dt):
            uid[0] += 1
            return stack.enter_context(nc.psum_tensor("%s_%d" % (name, uid[0]), shape, dt))

        dbg_toks = []

        def dump(ph, name, tens, shape, dt):
            if stop != ph:
                return
            S.barrier()
            d = nc.dram_tensor("dbg_" + name, list(shape), dt, kind="ExternalOutput")
            dbg_toks.append(k.dma("sp", d.ap(), tens, [], ()))

        idf = sb(es, "idf", [128, 128], F32)
        idb = sb(es, "idb", [128, 128], BF16)
        o_cat = sb(es, "o_cat", [128, NT_OWN, 1024], BF16)
        B_id = Buf("id")
        B_ocat = [[Buf("ocatA%d" % t), Buf("ocatB%d" % t)] for t in range(NT_OWN)]
        k.dma("sp", idf[:], dr["ident"].ap(), (), [B_id])
        k.copy("dve", idb[:], idf[:], [B_id], [B_id])

        def pipeline(n, stages, offs):
            for T in range(n + max(offs)):
                for s_ in reversed(range(len(stages))):
                    i = T - offs[s_]
                    if 0 <= i < n:
                        stages[s_](i)

        def head_norm(raw, ngrp, gd, gtile, wk, BBw, B_raw, eng2="pool"):
            n = ngrp * gd
            v3 = lambda ap: ap[:, 0:n].rearrange("p (a b) -> p a b", b=gd)
            return [
                lambda: k.tt(eng2, wk["sq"][:, 0:n], raw[:, 0:n], raw[:, 0:n], ALU.mult, [B_raw], [BBw["sq"]]),
                lambda: k.rsum(wk["ss"][:, 0:ngrp], v3(wk["sq"]), [BBw["sq"]], [BBw["ss"]]),
                lambda: k.act(wk["ss"][:, 16:16 + ngrp], wk["ss"][:, 0:ngrp], AF.Sqrt, [BBw["ss"]], [BBw["ss"]],
                              scale=1.0 / gd, bias=EPS),
                lambda: k.recip(wk["ss"][:, 32:32 + ngrp], wk["ss"][:, 16:16 + ngrp], [BBw["ss"]], [BBw["ss"]]),
                lambda: k.tt("dve", v3(wk["kn"]), v3(raw), bc(wk["ss"][:, 32:32 + ngrp].unsqueeze(2), [128, ngrp, gd]),
                             ALU.mult, [B_raw, BBw["ss"]], [BBw["kn"]]),
                lambda: k.tt(eng2, v3(wk["kn"]), v3(wk["kn"]), bc(gtile[:].unsqueeze(1), [128, ngrp, gd]), ALU.mult,
                             [BBw["kn"], BBw["gv"]], [BBw["kn"]]),
            ]

        def proj_phase(s1, kind, tiles, col0, QT, KT, Vst, B_QT, B_KT, B_V):
            da = kind == "da"
            ngrp, gd = (8, 64) if da else (16, 32)
            w_sb = sb(s1, "w_in_" + kind, [128, 8, 1536], BF16)
            xt = [sb(s1, "xt%d" % i, [128, 1024], F32) for i in range(2)]
            a_bf = [sb(s1, "abf%d" % i, [128, 1024], BF16) for i in range(2)]
            junk = sb(s1, "junk", [128, 1024], BF16)
            stat = [sb(s1, "stat%d" % i, [128, 4], F32) for i in range(2)]
            aT = [sb(s1, "aT%d" % i, [128, 8, 128], BF16) for i in range(2)]
            gmix = sb(s1, "gmix", [128, 1024], F32)
            gq = sb(s1, "gq", [128, gd], F32)
            gk = sb(s1, "gk", [128, gd], F32)
            raw = [[sb(s1, "raw%d_%d" % (i, c), [128, 512], F32) for c in range(2)] for i in range(2)]
            knb = [[sb(s1, "knb%d_%d" % (i, c), [128, 512], BF16) for c in range(2)] for i in range(2)]
            names = ("sq", "kn", "A", "Bt") if da else ("sq", "kn")
            wks = []
            for c in range(2):
                wks.append({n: sb(s1, "wk_%s%d" % (n, c), [128, 512], F32) for n in names})
                wks[c]["ss"] = sb(s1, "wk_ss%d" % c, [128, 48], F32)
            if da:
                cs_sb = sb(s1, "cs_sb", [128, 32, 64], F32)
                sn_sb = sb(s1, "sn_sb", [128, 32, 64], F32)
            pT = [ps(s1, "pT%d" % i, [128, 1024], BF16) for i in range(2)]
            pP = [ps(s1, "pP%d" % i, [128, 512], F32) for i in range(3)]
            pT2 = [ps(s1, "pT2%d" % i, [128, 1024], BF16) for i in range(2)]
            B_xt, B_st, B_a, B_pT, B_aT = ([Buf(), Buf()] for _ in range(5))
            B_g, B_tab, B_gq, B_gk = Buf(), Buf(), Buf(), Buf()
            B_w = [Buf() for _ in range(3)]
            B_pP = [Buf() for _ in range(3)]
            B_pT2 = [Buf(), Buf()]
            B_raw = [[Buf(), Buf()], [Buf(), Buf()]]
            B_knb = [[Buf(), Buf()], [Buf(), Buf()]]
            BW = [{n: Buf() for n in ("sq", "ss", "kn", "A", "Bt")} for _ in range(2)]
            BW[0]["gv"], BW[1]["gv"] = B_gq, B_gk
            w_in_v = dr["w_in"].ap().rearrange("(kc p) n -> p kc n", p=128)
            order = [1, 2, 0]
            for cg in order:
                k.dma("pool", w_sb[:, :, cg * 512:(cg + 1) * 512], w_in_v[:, :, col0 + cg * 512:col0 + (cg + 1) * 512], (), [B_w[cg]])
            k.dma("sp", gmix[:], bvec("g_mix", 1024), (), [B_g])
            k.dma("sp", gq[:], bvec("g_qa" if da else "g_qb", gd), (), [B_gq])
            k.dma("sp", gk[:], bvec("g_ka" if da else "g_kb", gd), (), [B_gk])
            if da:
                k.dma("sp", cs_sb[:].rearrange("p a b -> p (a b)"), dr["cs"].ap(), (), [B_tab])
                k.dma("sp", sn_sb[:].rearrange("p a b -> p (a b)"), dr["sn"].ap(), (), [B_tab])
            cnt = {"pp": 0, "p2": 0}

            def groups(i):
                return ([0] if tiles[i] < NT_OWN else []) + [1, 2]

            def s0(i):
                t, par = tiles[i], i % 2
                k.dma("sp", xt[par][:], x_ap[t * 128:(t + 1) * 128, :], (), [B_xt[par]])
                k.act(junk[:], xt[par][:], AF.Square, [B_xt[par]], [B_st[par]], accum=stat[par][:, 0:1])
                k.act(stat[par][:, 1:2], stat[par][:, 0:1], AF.Sqrt, [B_st[par]], [B_st[par]], scale=1.0 / 1024, bias=EPS)
                k.recip(stat[par][:, 2:3], stat[par][:, 1:2], [B_st[par]], [B_st[par]])
                k.stt("dve", a_bf[par][:], xt[par][:], stat[par][:, 2:3], gmix[:], ALU.mult, ALU.mult,
                      [B_xt[par], B_st[par], B_g], [B_a[par]])

            def s1_(i):
                par = i % 2
                for kc in range(8):
                    k.tr(pT[par][:, kc * 128:(kc + 1) * 128], a_bf[par][:, kc * 128:(kc + 1) * 128], idb[:],
                         [B_a[par], B_id], [B_pT[par]])
                k.copy("act", aT[par][:].rearrange("p a b -> p (a b)"), pT[par][:], [B_pT[par]], [B_aT[par]])

            def s2_(i):
                t, par = tiles[i], i % 2
                for cg in groups(i):
                    pp = cnt["pp"] % 3
                    cnt["pp"] += 1
                    for kc in range(8):
                        k.mm(pP[pp][:], aT[par][:, kc, :], w_sb[:, kc, cg * 512:(cg + 1) * 512], kc == 0, kc == 7,
                             [B_aT[par], B_w[cg]], [B_pP[pp]])
                    if cg == 2:
                        if da:
                            k.copy("act", Vst[:, t, :, 0:128], pP[pp][:].rearrange("p (a b) -> p a b", b=128), [B_pP[pp]], [B_V[t]])
                        else:
                            k.copy("act", Vst[:, i, :, 0:32], pP[pp][:].rearrange("p (a b) -> p a b", b=32), [B_pP[pp]], [B_V[i]])
                    else:
                        k.copy("dve", raw[par][cg][:], pP[pp][:], [B_pP[pp]], [B_raw[par][cg]])

            def s3_chain(i, cg):
                t, par = tiles[i], i % 2
                wk, bw = wks[cg], BW[cg]
                ch = head_norm(raw[par][cg], ngrp, gd, gq if cg == 0 else gk, wk, bw, B_raw[par][cg])
                if da:
                    kn3 = wk["kn"][:].rearrange("p (a b) -> p a b", b=64)
                    A3 = wk["A"][:].rearrange("p (a b) -> p a b", b=64)
                    Bt3 = wk["Bt"][:].rearrange("p (a b) -> p a b", b=64)
                    ch += [
                        lambda: k.tt("dve", A3, kn3, bc(cs_sb[:, t, :].unsqueeze(1), [128, 8, 64]), ALU.mult,
                                     [bw["kn"], B_tab], [bw["A"]]),
                        lambda: k.tt("pool", Bt3[:, :, 0:32], kn3[:, :, 32:64], bc(sn_sb[:, t, 0:32].unsqueeze(1), [128, 8, 32]),
                                     ALU.mult, [bw["kn"], B_tab], [bw["Bt"]]),
                        lambda: k.tt("pool", Bt3[:, :, 32:64], kn3[:, :, 0:32], bc(sn_sb[:, t, 32:64].unsqueeze(1), [128, 8, 32]),
                                     ALU.mult, [bw["kn"], B_tab], [bw["Bt"]]),
                        lambda: k.tt("dve", knb[par][cg][:], wk["A"][:], wk["Bt"][:], ALU.add, [bw["A"], bw["Bt"]],
                                     [B_knb[par][cg]]),
                    ]
                else:
                    ch.append(lambda: k.copy("dve", knb[par][cg][:], wk["kn"][:], [bw["kn"]], [B_knb[par][cg]]))
                return ch

            def s3_(i):
                chains = [s3_chain(i, cg) for cg in groups(i) if cg != 2]
                for j in range(max(len(c) for c in chains)):
                    for c in chains:
                        if j < len(c):
                            c[j]()

            def s4_(i):
                t, par = tiles[i], i % 2
                for cg in groups(i):
                    if cg == 2:
                        continue
                    p2 = cnt["p2"] % 2
                    cnt["p2"] += 1
                    for hh in range(4):
                        k.tr(pT2[p2][:, hh * 128:(hh + 1) * 128], knb[par][cg][:, hh * 128:(hh + 1) * 128], idb[:],
                             [B_knb[par][cg], B_id], [B_pT2[p2]])
                    src = pT2[p2][:, 0:512].rearrange("p (a b) -> p a b", b=128)
                    if cg == 0:
                        k.copy("act", QT[:, :, t * 128:(t + 1) * 128], src, [B_pT2[p2]], [B_QT[t]])
                    else:
                        kt_ = t if da else i
                        k.copy("act", KT[:, :, kt_ * 128:(kt_ + 1) * 128], src, [B_pT2[p2]], [B_KT[kt_]])

            pipeline(len(tiles), [s0, s1_, s2_, s3_, s4_], [0, 1, 2, 3, 4])

        with ExitStack() as sa:
            QaT = sb(sa, "QaT", [128, 4, 2048], BF16)
            KaT = sb(sa, "KaT", [128, 4, 4096], BF16)
            Va = sb(sa, "Va", [128, NT_ALL, 4, 129], BF16)
            B_QaT = [Buf("QaT%d" % t) for t in range(NT_OWN)]
            B_KaT = [Buf("KaT%d" % t) for t in range(NT_ALL)]
            B_Va = [Buf("Va%d" % t) for t in range(NT_ALL)]
            k.memset("pool", Va[:], 1.0, B_Va)

            for _once in ([0] if on("A1") else []):
                with ExitStack() as s1:
                    proj_phase(s1, "da", list(range(NT_ALL)), 0, QaT, KaT, Va, B_QaT, B_KaT, B_Va)
                    dump("A1", "QaT", QaT[:], [128, 4, 2048], BF16)
                    dump("A1", "KaT", KaT[:], [128, 4, 4096], BF16)
                    dump("A1", "Va", Va[:], [128, NT_ALL, 4, 129], BF16)
                    S.barrier()

            for _once in ([0] if on("B1") else []):
                with ExitStack() as s2:
                    NB = 4
                    NBP = 8
                    Pt = [sb(s2, "Pt%d" % i, [128, 512], BF16) for i in range(NBP)]
                    Osb = [sb(s2, "Osb%d" % i, [128, 132], F32) for i in range(4)]
                    t1 = sb(s2, "t1", [128, 4, 128], F32)
                    ob = sb(s2, "ob", [128, 4, 128], F32)
                    sqb = sb(s2, "sqb", [128, 128], F32)
                    sm = sb(s2, "sm", [128, 4, 8], F32)
                    ssq = sb(s2, "ssq", [128, 16], F32)
                    lamb = sb(s2, "lamb", [128, 4, 64], F32)
                    lamw = sb(s2, "lamw", [128, 2, 64], F32)
                    lams = sb(s2, "lams", [128, 8], F32)
                    gsub = sb(s2, "gsub", [128, 128], F32)
                    pS = [ps(s2, "pS%d" % i, [128, 512], F32) for i in range(NB)]
                    pO = [ps(s2, "pO%d" % i, [128, 512], F32) for i in range(4)]
                    B_Pt = [Buf() for _ in range(NBP)]
                    B_pS = [Buf() for _ in range(NB)]
                    B_pO = [Buf() for _ in range(4)]
                    B_Osb = [Buf() for _ in range(4)]
                    B_t1 = [Buf() for _ in range(4)]
                    B_ob = [Buf() for _ in range(4)]
                    B_sm = [Buf() for _ in range(4)]
                    B_lam, B_gs, B_ssq, B_sqb = Buf(), Buf(), Buf(), Buf()
                    for i, n in enumerate(("lam_q1", "lam_k1", "lam_q2", "lam_k2")):
                        k.dma("sp", lamb[:, i, :], bvec(n, 64), (), [B_lam])
                    k.dma("sp", gsub[:], bvec("g_sub", 128), (), [B_gs])
                    k.ts("dve", gsub[:], gsub[:], 0.8, ALU.mult, [B_gs], [B_gs])
                    k.tt("dve", lamw[:, 0, :], lamb[:, 0, :], lamb[:, 1, :], ALU.mult, [B_lam], [B_lam])
                    k.tt("dve", lamw[:, 1, :], lamb[:, 2, :], lamb[:, 3, :], ALU.mult, [B_lam], [B_lam])
                    k.rsum(lams[:, 0:2], lamw[:], [B_lam], [B_lam])
                    k.act(lams[:, 2:4], lams[:, 0:2], AF.Exp, [B_lam], [B_lam])
                    k.tt("dve", lams[:, 4:5], lams[:, 3:4], lams[:, 2:3], ALU.subtract, [B_lam], [B_lam])
                    k.ts("dve", lams[:, 5:6], lams[:, 4:5], -0.2, ALU.add, [B_lam], [B_lam])
                    items = [(h, qg, comp, kt) for h in range(4) for qg in range(4) for comp in range(2) for kt in range(NT_ALL)]
                    QaZ = [sb(s2, "QaZ%d" % c, [128, 4, 2048], BF16) for c in range(2)]
                    B_QaZ = [[Buf() for _ in range(4)] for _ in range(2)]
                    for c in range(2):
                        zr = slice(64, 128) if c == 0 else slice(0, 64)
                        cr = slice(0, 64) if c == 0 else slice(64, 128)
                        k.memset("pool", QaZ[c][zr], 0.0, B_QaZ[c])
                        for qg_ in range(4):
                            k.copy("dve" if c == 0 else "pool", QaZ[c][cr, :, qg_ * 512:(qg_ + 1) * 512],
                                   QaT[cr, :, qg_ * 512:(qg_ + 1) * 512], B_QaT[qg_ * 4:qg_ * 4 + 4], [B_QaZ[c][qg_]])

                    def b1_s0(i):
                        h, qg, comp, kt = items[i]
                        s = i % NB
                        k.mm(pS[s][:], KaT[:, h, kt * 128:(kt + 1) * 128], QaZ[comp][:, h, qg * 512:(qg + 1) * 512],
                             True, True, [B_KaT[kt], B_QaZ[comp][qg]], [B_pS[s]])
                        k.act(Pt[i % NBP][:], pS[s][:], AF.Exp, [B_pS[s]], [B_Pt[i % NBP]], scale=0.125)

                    def b1_s1(i):
                        h, qg, comp, kt = items[i]
                        s = i % NBP
                        for qs in range(4):
                            k.mm(pO[qs][:, 0:129], Pt[s][:, qs * 128:(qs + 1) * 128], Va[:, kt, h, :],
                                 kt == 0, kt == NT_ALL - 1, [B_Pt[s], B_Va[kt]], [B_pO[qs]])
                        if kt != NT_ALL - 1:
                            return
                        for qs in range(4):
                            k.copy("dve", Osb[qs][:, 0:129], pO[qs][:, 0:129], [B_pO[qs]], [B_Osb[qs]])
                        for qs in range(4):
                            O = Osb[qs]
                            bo = B_Osb[qs]
                            if comp == 0:
                                k.recip(sm[:, qs, 0:1], O[:, 128:129], [bo], [B_sm[qs]])
                                k.ts("dve", t1[:, qs, :], O[:, 0:128], sm[:, qs, 0:1], ALU.mult, [bo, B_sm[qs]], [B_t1[qs]])
                            else:
                                k.recip(sm[:, qs, 1:2], O[:, 128:129], [bo], [B_sm[qs]])
                                k.tt("dve", sm[:, qs, 2:3], sm[:, qs, 1:2], lams[:, 5:6], ALU.mult, [B_sm[qs], B_lam], [B_sm[qs]])
                                k.stt("dve", ob[:, qs, :], O[:, 0:128], sm[:, qs, 2:3], t1[:, qs, :], ALU.mult, ALU.add,
                                      [bo, B_sm[qs], B_t1[qs]], [B_ob[qs]])
                                k.tt("dve", sqb[:], ob[:, qs, :], ob[:, qs, :], ALU.mult, [B_ob[qs]], [B_sqb])
                                k.rsum(ssq[:, qs:qs + 1], sqb[:], [B_sqb], [B_ssq])
                        if comp == 1:
                            k.act(ssq[:, 4:8], ssq[:, 0:4], AF.Sqrt, [B_ssq], [B_ssq], scale=1.0 / 128, bias=EPS)
                            k.recip(ssq[:, 8:12], ssq[:, 4:8], [B_ssq], [B_ssq])
                            for qs in range(4):
                                tq = qg * 4 + qs
                                k.stt("dve", o_cat[:, tq, h * 128:(h + 1) * 128], ob[:, qs, :], ssq[:, 8 + qs:9 + qs], gsub[:],
                                      ALU.mult, ALU.mult, [B_ob[qs], B_ssq, B_gs], [B_ocat[tq][0]])

                    pipeline(len(items), [b1_s0, b1_s1], [0, 6])
                    dump("B1", "ocat", o_cat[:], [128, NT_OWN, 1024], BF16)
                    S.barrier()

        with ExitStack() as sa:
            QbT = sb(sa, "QbT", [128, 4, 2048], BF16)
            KbT = sb(sa, "KbT", [128, 4, 20 * 128], BF16)
            Vb = sb(sa, "Vb", [128, 20, 16, 33], BF16)
            B_QbT = [Buf() for _ in range(NT_OWN)]
            B_KbT = [Buf() for _ in range(20)]
            B_Vb = [Buf() for _ in range(20)]
            k.memset("pool", Vb[:], 1.0, B_Vb)
            for _once in ([0] if on("A2") else []):
                with ExitStack() as s1:
                    s2t = [30, 31] + list(range(16)) + [16, 17]
                    proj_phase(s1, "na", s2t, 1536, QbT, KbT, Vb, B_QbT, B_KbT, B_Vb)
                    dump("A2", "QbT", QbT[:], [128, 4, 2048], BF16)
                    dump("A2", "KbT", KbT[:], [128, 4, 2560], BF16)
                    dump("A2", "Vb", Vb[:], [128, 20, 16, 33], BF16)
                    S.barrier()

            for _once in ([0] if on("B2") else []):
                with ExitStack() as s2:
                    NB = 4
                    biasG = sb(s2, "biasG", [128, 4, 5, 128], F32)
                    biasS = [sb(s2, "biasS%d" % i, [128, 4, 6, 128], F32) for i in range(2)]
                    Sb = [sb(s2, "Sb%d" % i, [128, 768], F32) for i in range(2)]
                    Pb = [sb(s2, "Pb%d" % i, [128, 768], BF16) for i in range(NB)]
                    rl = [sb(s2, "rl%d" % i, [128, 4], F32) for i in range(2)]
                    pS = [ps(s2, "pSb%d" % i, [128, 1024], F32) for i in range(2)]
                    pO = [ps(s2, "pOb%d" % i, [128, 4, 128], F32) for i in range(2)]
                    B_bG = [Buf() for _ in range(4)]
                    B_bS = [Buf(), Buf()]
                    B_Sb = [Buf(), Buf()]
                    B_Pb = [Buf() for _ in range(NB)]
                    B_rl = [Buf(), Buf()]
                    B_pS = [Buf(), Buf()]
                    B_pO = [Buf(), Buf()]
                    scale_b = 32 ** -0.5
                    items = [(g, j, hh) for g in range(4) for j in range(NT_OWN) for hh in range(4)]
                    spec_idx = {}
                    c_ = 0
                    for g in range(4):
                        for j in range(NT_OWN):
                            if j in NA_OFFS:
                                spec_idx[(g, j)] = c_ % 2
                                c_ += 1

                    def b2_s0(i):
                        g, j, hh = items[i]
                        if j == 0 and hh == 0:
                            k.dma("sp", biasG[:].rearrange("p a b c -> p (a b c)"), dr["biasG"].ap()[g], (), [B_bG[0]])
                        if j in NA_OFFS:
                            offs = NA_OFFS[j]
                            sp_i = spec_idx[(g, j)]
                            if hh == 0:
                                k.dma("sp", biasS[sp_i][:].rearrange("p a b c -> p (a b c)"),
                                      dr["biasS"].ap()[NA_SPECIAL.index(j), g], (), [B_bS[sp_i]])
                            btile, bbuf = biasS[sp_i], B_bS[sp_i]
                        else:
                            offs = NA_GEN
                            btile, bbuf = biasG, B_bG[0]
                        nof = len(offs)
                        b2 = i % 2
                        pb = i % NB
                        rows = slice(hh * 32, (hh + 1) * 32)
                        for ci, off in enumerate(offs):
                            s_idx = j + off + 2
                            k.mm(pS[b2][:, ci * 128:(ci + 1) * 128], KbT[rows, g, s_idx * 128:(s_idx + 1) * 128],
                                 QbT[rows, g, j * 128:(j + 1) * 128], True, True,
                                 [B_KbT[s_idx], B_QbT[j]], [B_pS[b2]], tp=(hh * 32, 0))
                        k.stt("dve", Sb[b2][:, 0:nof * 128], pS[b2][:, 0:nof * 128], scale_b,
                              btile[:, hh, 0:nof, :].rearrange("p a b -> p (a b)"), ALU.mult, ALU.add,
                              [B_pS[b2], bbuf], [B_Sb[b2]])
                        k.act(Pb[pb][:, 0:nof * 128], Sb[b2][:, 0:nof * 128], AF.Exp, [B_Sb[b2]], [B_Pb[pb]])

                    def b2_s1(i):
                        g, j, hh = items[i]
                        offs = NA_OFFS.get(j, NA_GEN)
                        nof = len(offs)
                        h = g * 4 + hh
                        pb = i % NB
                        oi = (g * NT_OWN + j) % 2
                        for ci, off in enumerate(offs):
                            s_idx = j + off + 2
                            k.mm(pO[oi][:, hh, 0:33], Pb[pb][:, ci * 128:(ci + 1) * 128], Vb[:, s_idx, h, :],
                                 ci == 0, ci == nof - 1, [B_Pb[pb], B_Vb[s_idx]], [B_pO[oi]])
                        if hh == 3:
                            k.recip(rl[oi][:].unsqueeze(2), pO[oi][:, :, 32:33], [B_pO[oi]], [B_rl[oi]])
                            k.tt("dve", o_cat[:, j, 512 + g * 128:512 + (g + 1) * 128].rearrange("p (a b) -> p a b", b=32),
                                 pO[oi][:, :, 0:32], bc(rl[oi][:].unsqueeze(2), [128, 4, 32]), ALU.mult,
                                 [B_pO[oi], B_rl[oi]], [B_ocat[j][1]])

                    pipeline(len(items), [b2_s0, b2_s1], [0, 3])
                    dump("B2", "ocat", o_cat[:], [128, NT_OWN, 1024], BF16)
                    S.barrier()

        hres = sb(es, "hres", [128, NT_OWN, 1024], F32)
        B_h = [[Buf(), Buf()] for _ in range(NT_OWN)]
        for _once in ([0] if on("C") else []):
            with ExitStack() as s1:
                w_sb = sb(s1, "w_outb", [128, 8, 1024], BF16)
                xt = [sb(s1, "xtc%d" % i, [128, 1024], F32) for i in range(2)]
                oT = [sb(s1, "oT%d" % i, [128, 8, 128], BF16) for i in range(2)]
                pT = [ps(s1, "pTc%d" % i, [128, 1024], BF16) for i in range(2)]
                pP = [ps(s1, "pPc%d" % i, [128, 512], F32) for i in range(3)]
                B_w = Buf()
                B_xt = [Buf(), Buf()]
                B_oT = [Buf(), Buf()]
                B_pT = [Buf(), Buf()]
                B_pP = [Buf() for _ in range(3)]
                k.dma("pool", w_sb[:], dr["w_out"].ap().rearrange("(kc p) n -> p kc n", p=128), (), [B_w])
                cntc = {"pp": 0}

                def c_s0(t):
                    par = t % 2
                    k.dma("sp", xt[par][:], x_ap[t * 128:(t + 1) * 128, :], (), [B_xt[par]])
                    for kc in range(8):
                        k.tr(pT[par][:, kc * 128:(kc + 1) * 128], o_cat[:, t, kc * 128:(kc + 1) * 128], idb[:],
                             [B_ocat[t][0], B_ocat[t][1], B_id], [B_pT[par]])
                    k.copy("act", oT[par][:].rearrange("p a b -> p (a b)"), pT[par][:], [B_pT[par]], [B_oT[par]])

                def c_s1(t):
                    par = t % 2
                    for cg in range(2):
                        pp = cntc["pp"] % 3
                        cntc["pp"] += 1
                        for kc in range(8):
                            k.mm(pP[pp][:], oT[par][:, kc, :], w_sb[:, kc, cg * 512:(cg + 1) * 512], kc == 0, kc == 7,
                                 [B_oT[par], B_w], [B_pP[pp]])
                        k.tt("dve", hres[:, t, cg * 512:(cg + 1) * 512], pP[pp][:], xt[par][:, cg * 512:(cg + 1) * 512], ALU.add,
                             [B_pP[pp], B_xt[par]], [B_h[t][cg]])

                pipeline(NT_OWN, [c_s0, c_s1], [0, 1])
                dump("C", "hres", hres[:], [128, NT_OWN, 1024], F32)
                S.barrier()

        with ExitStack() as sd:
            xnT = sb(sd, "xnT", [128, 8, 2048], BF16)
            gates = sb(sd, "gates", [128, NT_OWN, 32], F32)
            B_xnT = [Buf() for _ in range(NT_OWN)]
            B_gates = [Buf() for _ in range(NT_OWN)]
            w1s = [sb(sd, "w1s%d" % i, [128, 8, 512], BF16) for i in range(2)]
            w3s = [sb(sd, "w3s%d" % i, [128, 8, 512], BF16) for i in range(2)]
            w2s = [sb(sd, "w2s%d" % i, [128, 4, 1024], BF16) for i in range(2)]
            B_w1 = [Buf(), Buf()]
            B_w3 = [Buf(), Buf()]
            B_w2 = [Buf(), Buf()]
            w1v = dr["w1"].ap()
            w3v = dr["w3"].ap()
            w2v = dr["w2"].ap()

            def load_w(e):
                wb = e % 2
                k.dma("pool", w1s[wb][:], w1v[e].rearrange("(kc p) n -> p kc n", p=128), (), [B_w1[wb]])
                k.dma("pool", w3s[wb][:], w3v[e].rearrange("(kc p) n -> p kc n", p=128), (), [B_w3[wb]])
                k.dma("pool", w2s[wb][:], w2v[e].rearrange("(kc p) n -> p kc n", p=128), (), [B_w2[wb]])

            if on("D2"):
                load_w(0)
            for _once in ([0] if on("D1") else []):
                with ExitStack() as s1:
                    gffn = sb(s1, "gffn", [128, 1024], F32)
                    w_r = sb(s1, "w_r", [128, 8, 36], F32)
                    xn = [sb(s1, "xn%d" % i, [128, 1024], F32) for i in range(2)]
                    junk = sb(s1, "junkd", [128, 1024], BF16)
                    stat = [sb(s1, "statd%d" % i, [128, 4], F32) for i in range(2)]
                    xT32 = [sb(s1, "xT32%d" % i, [128, 8, 128], F32) for i in range(2)]
                    rt = [sb(s1, "rt%d" % i, [128, 128], F32) for i in range(2)]
                    pT = [ps(s1, "pTd%d" % i, [128, 1024], F32) for i in range(2)]
                    pL = [ps(s1, "pL%d" % i, [128, 512], F32) for i in range(2)]
                    B_g, B_wr = Buf(), Buf()
                    B_xn = [Buf(), Buf()]
                    B_st = [Buf(), Buf()]
                    B_xT = [Buf(), Buf()]
                    B_rt = [Buf(), Buf()]
                    B_pT = [Buf(), Buf()]
                    B_pL = [Buf(), Buf()]
                    k.dma("sp", gffn[:], bvec("g_ffn", 1024), (), [B_g])
                    k.dma("sp", w_r[:], dr["w_r"].ap().rearrange("(kc p) n -> p kc n", p=128), (), [B_wr])

                    def d1_s0(t):
                        par = t % 2
                        hb = B_h[t]
                        k.act(junk[:], hres[:, t, :], AF.Square, hb, [B_st[par]], accum=stat[par][:, 0:1])
                        k.act(stat[par][:, 1:2], stat[par][:, 0:1], AF.Sqrt, [B_st[par]], [B_st[par]], scale=1.0 / 1024, bias=EPS)
                        k.recip(stat[par][:, 2:3], stat[par][:, 1:2], [B_st[par]], [B_st[par]])
                        k.stt("dve", xn[par][:], hres[:, t, :], stat[par][:, 2:3], gffn[:], ALU.mult, ALU.mult,
                              hb + [B_st[par], B_g], [B_xn[par]])

                    def d1_s1(t):
                        par = t % 2
                        for kc in range(8):
                            k.tr(pT[par][:, kc * 128:(kc + 1) * 128], xn[par][:, kc * 128:(kc + 1) * 128], idf[:],
                                 [B_xn[par], B_id], [B_pT[par]])
                        k.copy("act", xT32[par][:].rearrange("p a b -> p (a b)"), pT[par][:], [B_pT[par]], [B_xT[par], B_pT[par]])
                        k.copy("dve", xnT[:, :, t * 128:(t + 1) * 128], pT[par][:].rearrange("p (a b) -> p a b", b=128),
                               [B_pT[par]], [B_xnT[t]])

                    def d1_s2_chain(t):
                        par = t % 2
                        r = rt[par]
                        br = [B_rt[par]]
                        e3 = lambda ap: ap.rearrange("p (e g) -> p e g", g=4)

                        def _mm():
                            for kc in range(8):
                                k.mm(pL[par][:, 0:36], xT32[par][:, kc, :], w_r[:, kc, :], kc == 0, kc == 7,
                                     [B_xT[par], B_wr], [B_pL[par]])
                        return [
                            _mm,
                            lambda: k.copy("dve", r[:, 0:36], pL[par][:, 0:36], [B_pL[par]], br),
                            lambda: k.rmax(r[:, 40:41], r[:, 0:4], br, br),
                            lambda: k.ts("dve", r[:, 44:48], r[:, 0:4], r[:, 40:41], ALU.is_ge, br, br),
                            lambda: k.ts("dve", r[:, 48:52], r[:, 0:4], r[:, 40:41], ALU.subtract, br, br),
                            lambda: k.act(r[:, 52:56], r[:, 48:52], AF.Exp, br, br),
                            lambda: k.rsum(r[:, 41:42], r[:, 52:56], br, br),
                            lambda: k.recip(r[:, 42:43], r[:, 41:42], br, br),
                            lambda: k.tt("dve", e3(r[:, 64:96]), r[:, 4:36].rearrange("p (g e) -> p e g", e=8),
                                         bc(r[:, 44:48].unsqueeze(1), [128, 8, 4]), ALU.mult, br, br),
                            lambda: k.rsum(r[:, 56:64], e3(r[:, 64:96]), br, br),
                            lambda: k.rmax(r[:, 96:97], r[:, 56:64], br, br),
                            lambda: k.ts("dve", r[:, 100:108], r[:, 56:64], r[:, 96:97], ALU.is_ge, br, br),
                            lambda: k.stt("dve", r[:, 108:116], r[:, 100:108], -1e30, r[:, 56:64], ALU.mult, ALU.add, br, br),
                            lambda: k.rmax(r[:, 97:98], r[:, 108:116], br, br),
                            lambda: k.ts("dve", r[:, 116:124], r[:, 56:64], r[:, 97:98], ALU.is_ge, br, br),
                            lambda: k.ts("dve", r[:, 100:108], r[:, 56:64], r[:, 96:97], ALU.subtract, br, br),
                            lambda: k.act(r[:, 100:108], r[:, 100:108], AF.Exp, br, br),
                            lambda: k.tt("dve", r[:, 100:108], r[:, 100:108], r[:, 116:124], ALU.mult, br, br),
                            lambda: k.rsum(r[:, 98:99], r[:, 100:108], br, br),
                            lambda: k.recip(r[:, 99:100], r[:, 98:99], br, br),
                            lambda: k.tt("dve", r[:, 99:100], r[:, 99:100], r[:, 42:43], ALU.mult, br, br),
                            lambda: k.ts("dve", r[:, 100:108], r[:, 100:108], r[:, 99:100], ALU.mult, br, br),
                            lambda: k.tt("dve", gates[:, t, :].rearrange("p (g e) -> p g e", e=8),
                                         bc(r[:, 44:48].unsqueeze(2), [128, 4, 8]), bc(r[:, 100:108].unsqueeze(1), [128, 4, 8]),
                                         ALU.mult, br, [B_gates[t]]),
                        ]

                    for T in range(NT_OWN + 3):
                        ch = []
                        if T >= 3 and (T - 3) % 2 == 0:
                            ch = [d1_s2_chain(T - 3), d1_s2_chain(T - 2)]
                        for j in range(max([len(c) for c in ch] + [0])):
                            for c in ch:
                                if j < len(c):
                                    c[j]()
                        if 0 <= T - 1 < NT_OWN:
                            d1_s1(T - 1)
                        if 0 <= T < NT_OWN:
                            d1_s0(T)
                    dump("D1", "gates", gates[:], [128, NT_OWN, 32], F32)
                    dump("D1", "xnT", xnT[:], [128, 8, 2048], BF16)
                    S.barrier()

            for _once in ([0] if on("D2") else []):
                with ExitStack() as s2:
                    hdn = [sb(s2, "hdn%d" % i, [128, 4, 512], BF16) for i in range(2)]
                    sil = [sb(s2, "sil%d" % i, [128, 512], F32) for i in range(2)]
                    p1 = [ps(s2, "p1_%d" % i, [128, 512], F32) for i in range(2)]
                    p3 = [ps(s2, "p3_%d" % i, [128, 512], F32) for i in range(2)]
                    pY = [ps(s2, "pY%d" % i, [128, 512], F32) for i in range(3)]
                    B_hdn = [[Buf() for _ in range(4)] for _ in range(2)]
                    B_sil = [Buf(), Buf()]
                    B_p1 = [Buf(), Buf()]
                    B_p3 = [Buf(), Buf()]
                    B_pY = [Buf() for _ in range(3)]
                    cntd = {"p": 0, "y": 0}

                    def d2_s0(i):
                        e, tg = i // 4, i % 4
                        wb = e % 2
                        hb = i % 2
                        if tg == 0 and e + 1 < 32:
                            load_w(e + 1)
                        for hc in range(4):
                            pb = cntd["p"] % 2
                            cntd["p"] += 1
                            for kc in range(8):
                                k.mm(p1[pb][:], w1s[wb][:, kc, hc * 128:(hc + 1) * 128], xnT[:, kc, tg * 512:(tg + 1) * 512],
                                     kc == 0, kc == 7, [B_w1[wb]] + B_xnT[tg * 4:tg * 4 + 4], [B_p1[pb]])
                            for kc in range(8):
                                k.mm(p3[pb][:], w3s[wb][:, kc, hc * 128:(hc + 1) * 128], xnT[:, kc, tg * 512:(tg + 1) * 512],
                                     kc == 0, kc == 7, [B_w3[wb]] + B_xnT[tg * 4:tg * 4 + 4], [B_p3[pb]])
                            k.act(sil[pb][:], p1[pb][:], AF.Silu, [B_p1[pb]], [B_sil[pb]])
                            k.tt("dve", hdn[hb][:, hc, :], sil[pb][:], p3[pb][:], ALU.mult, [B_sil[pb], B_p3[pb]], [B_hdn[hb][hc]])

                    def d2_s1(i):
                        e, tg = i // 4, i % 4
                        wb = e % 2
                        hb = i % 2
                        for tt_ in range(4):
                            t = tg * 4 + tt_
                            for cg in range(2):
                                yb = cntd["y"] % 3
                                cntd["y"] += 1
                                for hc in range(4):
                                    k.mm(pY[yb][:], hdn[hb][:, hc, tt_ * 128:(tt_ + 1) * 128], w2s[wb][:, hc, cg * 512:(cg + 1) * 512],
                                         hc == 0, hc == 3, [B_hdn[hb][hc], B_w2[wb]], [B_pY[yb]])
                                k.stt("dve", hres[:, t, cg * 512:(cg + 1) * 512], pY[yb][:], gates[:, t, e:e + 1],
                                      hres[:, t, cg * 512:(cg + 1) * 512], ALU.mult, ALU.add,
                                      [B_pY[yb], B_gates[t], B_h[t][cg]], [B_h[t][cg]])

                    pipeline(128, [d2_s0, d2_s1], [0, 1])
                    dump("D2", "hres", hres[:], [128, NT_OWN, 1024], F32)
                    S.barrier()

        out_toks = []
        for _once in ([0] if on("E") else []):
            with ExitStack() as s1:
                wple = sb(s1, "wple", [128, 2, 1024], BF16)
                wplg = sb(s1, "wplg", [128, 8, 1024], BF16)
                gplg = sb(s1, "gplg", [128, 1024], F32)
                gple = sb(s1, "gple", [128, 1024], F32)
                pt = [sb(s1, "pt%d" % i, [128, 256], F32) for i in range(2)]
                ptb = [sb(s1, "ptb%d" % i, [128, 256], BF16) for i in range(2)]
                pTs = [sb(s1, "pTs%d" % i, [128, 2, 128], BF16) for i in range(2)]
                hn = [sb(s1, "hn%d" % i, [128, 1024], BF16) for i in range(2)]
                hT = [sb(s1, "hT%d" % i, [128, 8, 128], BF16) for i in range(2)]
                junk = sb(s1, "junke", [128, 1024], BF16)
                statA = [sb(s1, "stateA%d" % i, [128, 4], F32) for i in range(2)]
                statB = [sb(s1, "stateB%d" % i, [128, 4], F32) for i in range(2)]
                pe_s = [sb(s1, "pe_s%d" % i, [128, 1024], F32) for i in range(2)]
                sg = [sb(s1, "sg%d" % i, [128, 1024], F32) for i in range(2)]
                yo = [sb(s1, "yo%d" % i, [128, 1024], F32) for i in range(2)]
                pTp = ps(s1, "pTp", [128, 1024], BF16)
                pTh = ps(s1, "pTh", [128, 1024], BF16)
                pE = ps(s1, "pE", [128, 1024], F32)
                pG = ps(s1, "pG", [128, 1024], F32)
                B = {n: [Buf(), Buf()] for n in ("pt", "ptb", "pTs", "hn", "hT", "stA", "stB", "pe_s", "sg", "yo")}
                B_pTp, B_pTh, B_pE, B_pG = Buf(), Buf(), [Buf(), Buf()], [Buf(), Buf()]
                B_wple, B_wplg, B_g1, B_g2 = Buf(), Buf(), Buf(), Buf()
                k.dma("pool", wple[:], dr["w_ple"].ap().rearrange("(kc p) n -> p kc n", p=128), (), [B_wple])
                k.dma("pool", wplg[:], dr["w_plg"].ap().rearrange("(kc p) n -> p kc n", p=128), (), [B_wplg])
                k.dma("sp", gplg[:], bvec("g_plg", 1024), (), [B_g1])
                k.dma("sp", gple[:], bvec("g_ple", 1024), (), [B_g2])

                def e_s0(t):
                    par = t % 2
                    hb = B_h[t]
                    st_ = statA[par]
                    bs = [B["stA"][par]]
                    k.dma("sp", pt[par][:], p_ap[t * 128:(t + 1) * 128, :], (), [B["pt"][par]])
                    k.copy("dve", ptb[par][:], pt[par][:], [B["pt"][par]], [B["ptb"][par]])
                    k.act(junk[:], hres[:, t, :], AF.Square, hb, bs, accum=st_[:, 0:1])
                    k.act(st_[:, 1:2], st_[:, 0:1], AF.Sqrt, bs, bs, scale=1.0 / 1024, bias=EPS)
                    k.recip(st_[:, 2:3], st_[:, 1:2], bs, bs)
                    k.stt("dve", hn[par][:], hres[:, t, :], st_[:, 2:3], gplg[:], ALU.mult, ALU.mult, hb + bs + [B_g1], [B["hn"][par]])

                def e_s1(t):
                    par = t % 2
                    for kc in range(2):
                        k.tr(pTp[:, kc * 128:(kc + 1) * 128], ptb[par][:, kc * 128:(kc + 1) * 128], idb[:], [B["ptb"][par], B_id], [B_pTp])
                    k.copy("act", pTs[par][:].rearrange("p a b -> p (a b)"), pTp[:, 0:256], [B_pTp], [B["pTs"][par]])
                    for kc in range(8):
                        k.tr(pTh[:, kc * 128:(kc + 1) * 128], hn[par][:, kc * 128:(kc + 1) * 128], idb[:], [B["hn"][par], B_id], [B_pTh])
                    k.copy("act", hT[par][:].rearrange("p a b -> p (a b)"), pTh[:], [B_pTh], [B["hT"][par]])

                def e_s2(t):
                    par = t % 2
                    for cg in range(2):
                        for kc in range(2):
                            k.mm(pE[:, cg * 512:(cg + 1) * 512], pTs[par][:, kc, :], wple[:, kc, cg * 512:(cg + 1) * 512], kc == 0, kc == 1,
                                 [B["pTs"][par], B_wple], [B_pE[cg]])
                    k.copy("act", pe_s[par][:], pE[:], B_pE, [B["pe_s"][par]])
                    for cg in range(2):
                        for kc in range(8):
                            k.mm(pG[:, cg * 512:(cg + 1) * 512], hT[par][:, kc, :], wplg[:, kc, cg * 512:(cg + 1) * 512], kc == 0, kc == 7,
                                 [B["hT"][par], B_wplg], [B_pG[cg]])
                    k.act(sg[par][:], pG[:], AF.Sigmoid, B_pG, [B["sg"][par]])

                def e_s3(t):
                    par = t % 2
                    hb = B_h[t]
                    st_ = statB[par]
                    bs = [B["stB"][par]]
                    k.act(junk[:], pe_s[par][:], AF.Square, [B["pe_s"][par]], bs, accum=st_[:, 0:1])
                    k.act(st_[:, 1:2], st_[:, 0:1], AF.Sqrt, bs, bs, scale=1.0 / 1024, bias=EPS)
                    k.recip(st_[:, 2:3], st_[:, 1:2], bs, bs)
                    k.stt("dve", pe_s[par][:], pe_s[par][:], st_[:, 2:3], gple[:], ALU.mult, ALU.mult,
                          [B["pe_s"][par], B_g2] + bs, [B["pe_s"][par]])
                    k.tt("dve", sg[par][:], sg[par][:], pe_s[par][:], ALU.mult, [B["sg"][par], B["pe_s"][par]], [B["sg"][par]])
                    k.tt("dve", yo[par][:], sg[par][:], hres[:, t, :], ALU.add, [B["sg"][par]] + hb, [B["yo"][par]])
                    out_toks.append(k.dma("sp", y_ap[t * 128:(t + 1) * 128, :], yo[par][:], [B["yo"][par]], ()))

                pipeline(NT_OWN, [e_s0, e_s1, e_s2, e_s3], [0, 1, 2, 3])
                S.barrier()
        S.ops["sp"].append(Op(None, list(out_toks) + dbg_toks, None))
        S.emit()
    return nc


def _rope_tables():
    inv = (np.float32(10000.0) ** (-np.arange(0, 64, 2, dtype=np.float32) / np.float32(64))).astype(np.float32)
    ang = np.arange(4096, dtype=np.float32)[:, None] * inv[None, :]
    return np.cos(ang).astype(np.float32), np.sin(ang).astype(np.float32)


def _bias_tables(rpb, hf):
    kp = np.arange(128)
    kpar, kcol = kp // 64, kp % 64
    qpar, qcol = kp // 64, kp % 64
    cs = np.clip(qcol - 8, 0, 48)
    col_valid = (kcol[:, None] >= cs[None, :]) & (kcol[:, None] < cs[None, :] + 16)
    col_off = np.clip(kcol[:, None] - qcol[None, :] + 15, 0, 30)

    def tile(j, off, hf_):
        r = hf_ * 32 + 2 * j + qpar
        kr = hf_ * 32 + 2 * (j + off) + kpar
        rs = np.clip(r - 4, 0, 56)
        row_valid = (kr[:, None] >= rs[None, :]) & (kr[:, None] <= rs[None, :] + 7) & (kr[:, None] >= 0) & (kr[:, None] <= 63)
        row_off = np.clip(kr[:, None] - r[None, :] + 7, 0, 14)
        valid = row_valid & col_valid
        vals = rpb[:, row_off, col_off]
        return np.where(valid[None], vals, np.float32(NEG)).astype(np.float32)

    G = np.zeros((4, 128, 4, 5, 128), np.float32)
    for oi, off in enumerate(NA_GEN):
        tl = tile(8, off, 0)
        for g in range(4):
            G[g, :, :, oi, :] = tl[g * 4:(g + 1) * 4].transpose(1, 0, 2)
    Sp = np.full((4, 4, 128, 4, 6, 128), np.float32(NEG), np.float32)
    for ji, j in enumerate(NA_SPECIAL):
        for oi, off in enumerate(NA_OFFS[j]):
            tl = tile(j, off, hf)
            for g in range(4):
                Sp[ji, g, :, :, oi, :] = tl[g * 4:(g + 1) * 4].transpose(1, 0, 2)
    return G.reshape(4, 128, 4 * 5 * 128), Sp.reshape(4, 4, 128, 4 * 6 * 128)


_NC_CACHE = {}


def prep_inputs(x, p, g_mix, w_in, g_qa, g_ka, lam_q1, lam_k1, lam_q2, lam_k2, g_sub, g_qb, g_kb,
           rpb, w_out, g_ffn, w_rg, w_re, w1, w3, w2, g_plg, w_plg, w_ple, g_ple):
    f = lambda a: np.ascontiguousarray(np.asarray(a, dtype=np.float32))
    x = f(x); p = f(p)
    cos, sin = _rope_tables()
    common = {
        "w_in": f(w_in)[0], "w_out": f(w_out)[0], "w_plg": f(w_plg)[0], "w_ple": f(w_ple)[0],
        "w_r": np.ascontiguousarray(np.concatenate([f(w_rg)[0], f(w_re)[0]], axis=1)),
        "w1": f(w1)[0], "w3": f(w3)[0], "w2": f(w2)[0],
        "g_mix": f(g_mix), "g_ffn": f(g_ffn), "g_plg": f(g_plg), "g_ple": f(g_ple),
        "g_qa": f(g_qa), "g_ka": f(g_ka), "lam_q1": f(lam_q1), "lam_k1": f(lam_k1),
        "lam_q2": f(lam_q2), "lam_k2": f(lam_k2), "g_sub": f(g_sub), "g_qb": f(g_qb), "g_kb": f(g_kb),
        "ident": np.eye(128, dtype=np.float32),
    }
    rpb0 = f(rpb)[0]
    in_maps = []
    for c in range(8):
        b, hf = c // 2, c % 2
        own = slice(hf * 2048, (hf + 1) * 2048)
        oth = slice((1 - hf) * 2048, (2 - hf) * 2048)
        xc = np.ascontiguousarray(np.concatenate([x[b, own], x[b, oth]], axis=0))
        pos = np.concatenate([np.arange(4096)[own], np.arange(4096)[oth]])
        cc = np.concatenate([cos[pos], cos[pos]], axis=1)
        ss = np.concatenate([-sin[pos], sin[pos]], axis=1)
        cs_t = np.ascontiguousarray(cc.reshape(32, 128, 64).transpose(1, 0, 2).reshape(128, 32 * 64))
        sn_t = np.ascontiguousarray(ss.reshape(32, 128, 64).transpose(1, 0, 2).reshape(128, 32 * 64))
        G, Sp = _bias_tables(rpb0, hf)
        m = dict(common)
        m.update({"x": xc, "p": np.ascontiguousarray(p[0, b, own]), "cs": cs_t, "sn": sn_t, "biasG": G, "biasS": Sp})
        in_maps.append(m)
    return in_maps


def kernel(**inputs):
    in_maps = prep_inputs(**inputs)
    if "nc" not in _NC_CACHE:
        _NC_CACHE["nc"] = build_program()
    nc = _NC_CACHE["nc"]
    res = run_bass_kernel_spmd(nc, in_maps, core_ids=list(range(8)))
    out = np.zeros((4, 4096, 1024), np.float32)
    for c in range(8):
        b, hf = c // 2, c % 2
        out[b, hf * 2048:(hf + 1) * 2048] = res.results[c]["y"]
    return out
```

```python
import math
import numpy as np
import concourse.bass as bass
import concourse.mybir as mybir
from contextlib import ExitStack
from concourse.bass_utils import run_bass_kernel_spmd

F32 = mybir.dt.float32
BF16 = mybir.dt.bfloat16
AF = mybir.ActivationFunctionType
ALU = mybir.AluOpType
AX = mybir.AxisListType

COMPUTE = ("pe", "act", "dve", "pool")
ALL_ENG = ("pe", "act", "dve", "pool", "sp")
N_DMA_SEMS = 24
EPS = 1e-6
NEG = -1e30
NT_OWN = 16
NT_ALL = 32


class Tok:
    __slots__ = ("eng", "idx", "needed", "sem", "val")

    def __init__(self, eng, idx):
        self.eng = eng
        self.idx = idx
        self.needed = False
        self.sem = None
        self.val = None


class Buf:
    __slots__ = ("name", "w", "r")

    def __init__(self, name=""):
        self.name = name
        self.w = None
        self.r = {}


class Op:
    __slots__ = ("fn", "waits", "tok")

    def __init__(self, fn, waits, tok):
        self.fn = fn
        self.waits = waits
        self.tok = tok


class Sched:
    def __init__(self, nc, es):
        self.nc = nc
        self.ops = {e: [] for e in ALL_ENG}
        self.waited = {e: {} for e in ALL_ENG}
        self.eng_sem = {e: es.enter_context(nc.semaphore("s_" + e)) for e in COMPUTE}
        self.dma_sems = [es.enter_context(nc.semaphore("s_dma%d" % i)) for i in range(N_DMA_SEMS)]
        self.dma_cnt = [0] * N_DMA_SEMS
        self.dma_last = [None] * N_DMA_SEMS
        self.dma_rr = 0
        self.dma_rr_sw = 0
        self.dma_rr_sw = 0
        self.n_dma = 0
        self.last_tok = {e: None for e in COMPUTE}

    def _need(self, eng, t, out):
        wd = self.waited[eng]
        if t.eng in COMPUTE:
            key = t.eng
            if wd.get(key, -1) >= t.idx:
                return
            wd[key] = t.idx
        else:
            key = t.sem
            if wd.get(key, -1) >= t.val:
                return
            wd[key] = t.val
        t.needed = True
        out.append(t)

    def _collect(self, eng, reads, writes, is_dma):
        out = []
        for b in reads:
            t = b.w
            if t is not None:
                if t.eng == eng and not is_dma and eng == "pe":
                    continue
                self._need(eng, t, out)
        for b in writes:
            t = b.w
            if t is not None:
                if not (t.eng == eng and not is_dma and eng == "pe"):
                    self._need(eng, t, out)
            for t in b.r.values():
                if t.eng == eng and not is_dma and eng == "pe":
                    continue
                self._need(eng, t, out)
        return out

    def op(self, eng, fn, reads=(), writes=()):
        waits = self._collect(eng, reads, writes, False)
        tok = Tok(eng, len(self.ops[eng]))
        self.ops[eng].append(Op(fn, waits, tok))
        self.last_tok[eng] = tok
        for b in reads:
            b.r[eng] = tok
        for b in writes:
            b.w = tok
            b.r = {}
        return tok

    def dma(self, queue, fn, reads=(), writes=()):
        waits = self._collect(queue, reads, writes, True)
        if queue == "pool":
            i = 16 + self.dma_rr_sw
            self.dma_rr_sw = (self.dma_rr_sw + 1) % (N_DMA_SEMS - 16)
        else:
            i = self.dma_rr
            self.dma_rr = (self.dma_rr + 1) % 16
        prev = self.dma_last[i]
        if prev is not None:
            self._need(queue, prev, waits)
        self.dma_cnt[i] += 1
        tok = Tok("dma", self.n_dma)
        self.n_dma += 1
        tok.sem = self.dma_sems[i]
        tok.val = 16 * self.dma_cnt[i]
        tok.needed = True
        self.dma_last[i] = tok
        self.ops[queue].append(Op(fn, waits, tok))
        for b in reads:
            b.r[("dma", tok.idx)] = tok
        for b in writes:
            b.w = tok
            b.r = {}
        return tok

    def barrier(self):
        toks = [self.last_tok[e] for e in COMPUTE if self.last_tok[e] is not None]
        toks += [t for t in self.dma_last if t is not None]
        for e in ALL_ENG:
            waits = []
            for t in toks:
                if t.eng == e:
                    continue
                self._need(e, t, waits)
            if waits:
                self.ops[e].append(Op(None, waits, None))

    def emit(self):
        nc = self.nc
        for e in COMPUTE:
            c = 0
            for o in self.ops[e]:
                t = o.tok
                if t is not None and t.eng == e and t.needed:
                    c += 1
                    t.sem = self.eng_sem[e]
                    t.val = c

        def run(e, eng):
            for o in self.ops[e]:
                for t in o.waits:
                    eng.wait_ge(t.sem, t.val)
                if o.fn is None:
                    continue
                ins = o.fn(eng)
                t = o.tok
                if t.eng == "dma":
                    ins.then_inc(t.sem, 16)
                elif t.needed:
                    ins.then_inc(t.sem, 1)

        with nc.Block() as block:
            @block.tensor
            def _(eng):
                run("pe", eng)

            @block.scalar
            def _(eng):
                run("act", eng)

            @block.vector
            def _(eng):
                run("dve", eng)

            @block.gpsimd
            def _(eng):
                run("pool", eng)

            @block.sync
            def _(eng):
                run("sp", eng)


class K:
    def __init__(self, S):
        self.S = S

    def mm(self, out, lhsT, rhs, start, stop, R, W, tp=None):
        if tp is None:
            f = lambda e: e.matmul(out, lhsT=lhsT, rhs=rhs, start=start, stop=stop)
        else:
            f = lambda e: e.matmul(out, lhsT=lhsT, rhs=rhs, start=start, stop=stop, tile_position=tp)
        return self.S.op("pe", f, R, W)

    def tr(self, out, in_, ident, R, W):
        return self.S.op("pe", lambda e: e.transpose(out=out, in_=in_, identity=ident), R, W)

    def act(self, out, in_, func, R, W, scale=1.0, bias=0.0, accum=None):
        if accum is None:
            f = lambda e: e.activation(out=out, in_=in_, func=func, bias=bias, scale=scale)
        else:
            f = lambda e: e.activation(out=out, in_=in_, func=func, bias=bias, scale=scale, accum_out=accum)
        return self.S.op("act", f, R, W)

    def tt(self, eng, out, in0, in1, op, R, W):
        return self.S.op(eng, lambda e: e.tensor_tensor(out=out, in0=in0, in1=in1, op=op), R, W)

    def ts(self, eng, out, in0, s1, op0, R, W, s2=None, op1=None):
        if op1 is None:
            f = lambda e: e.tensor_scalar(out=out, in0=in0, scalar1=s1, scalar2=None, op0=op0)
        else:
            f = lambda e: e.tensor_scalar(out=out, in0=in0, scalar1=s1, scalar2=s2, op0=op0, op1=op1)
        return self.S.op(eng, f, R, W)

    def stt(self, eng, out, in0, scalar, in1, op0, op1, R, W):
        return self.S.op(eng, lambda e: e.scalar_tensor_tensor(out=out, in0=in0, scalar=scalar, in1=in1, op0=op0, op1=op1), R, W)

    def rsum(self, out, in_, R, W):
        return self.S.op("dve", lambda e: e.reduce_sum(out=out, in_=in_, axis=AX.X), R, W)

    def rmax(self, out, in_, R, W):
        return self.S.op("dve", lambda e: e.reduce_max(out=out, in_=in_, axis=AX.X), R, W)

    def recip(self, out, in_, R, W):
        return self.S.op("dve", lambda e: e.reciprocal(out=out, in_=in_), R, W)

    def copy(self, eng, out, in_, R, W):
        if eng == "act":
            return self.S.op("act", lambda e: e.activation(out=out, in_=in_, func=AF.Copy), R, W)
        return self.S.op(eng, lambda e: e.tensor_copy(out=out, in_=in_), R, W)

    def memset(self, eng, out, val, W):
        return self.S.op(eng, lambda e: e.memset(out, val), (), W)

    def dma(self, queue, out, in_, R, W):
        return self.S.dma(queue, lambda e: e.dma_start(out=out, in_=in_), R, W)


def bc(ap, shape):
    return ap.broadcast_to(shape)


NA_OFFS = {0: [-2, -1, 0, 1, 2, 3], 1: [-2, -1, 0, 1, 2], 14: [-2, -1, 0, 1, 2], 15: [-3, -2, -1, 0, 1, 2]}
NA_GEN = [-2, -1, 0, 1, 2]
NA_SPECIAL = [0, 1, 14, 15]


PHASES = ["A1", "B1", "A2", "B2", "C", "D1", "D2", "E"]


def build_program(stop=None):
    nc = bass.Bass("TRN2", target_bir_lowering=False)
    dr = {}
    last = len(PHASES) - 1 if stop is None else PHASES.index(stop)

    def on(ph):
        return PHASES.index(ph) <= last

    def din(name, shape):
        dr[name] = nc.dram_tensor(name, list(shape), F32, kind="ExternalInput")
        return dr[name]

    din("x", [4096, 1024])
    din("p", [2048, 256])
    din("cs", [128, 32 * 64])
    din("sn", [128, 32 * 64])
    din("w_in", [1024, 3072])
    din("w_out", [1024, 1024])
    din("w_plg", [1024, 1024])
    din("w_ple", [256, 1024])
    din("w_r", [1024, 36])
    din("w1", [32, 1024, 512])
    din("w3", [32, 1024, 512])
    din("w2", [32, 512, 1024])
    for n in ("g_mix", "g_ffn", "g_plg", "g_ple"):
        din(n, [1, 1024])
    for n in ("g_qa", "g_ka", "lam_q1", "lam_k1", "lam_q2", "lam_k2"):
        din(n, [1, 64])
    din("g_sub", [1, 128])
    din("g_qb", [1, 32])
    din("g_kb", [1, 32])
    din("ident", [128, 128])
    din("biasG", [4, 128, 4 * 5 * 128])
    din("biasS", [4, 4, 128, 4 * 6 * 128])
    y = nc.dram_tensor("y", [2048, 1024], F32, kind="ExternalOutput")

    x_ap = dr["x"].ap()
    p_ap = dr["p"].ap()
    y_ap = y.ap()

    def bvec(name, n):
        return bass.AP(dr[name], 0, [[0, 128], [1, n]])

    with ExitStack() as es:
        S = Sched(nc, es)
        k = K(S)

        uid = [0]

        def sb(stack, name, shape, dt):
            uid[0] += 1
            return stack.enter_context(nc.sbuf_tensor("%s_%d" % (name, uid[0]), shape, dt))

        def ps(stack, name, shape, dt):
            uid[0] += 1
            return stack.enter_context(nc.psum_tensor("%s_%d" % (name, uid[0]), shape, dt))

        dbg_toks = []

        def dump(ph, name, tens, shape, dt):
            if stop != ph:
                return
            S.barrier()
            d = nc.dram_tensor("dbg_" + name, list(shape), dt, kind="ExternalOutput")
            dbg_toks.append(k.dma("sp", d.ap(), tens, [], ()))

        idf = sb(es, "idf", [128, 128], F32)
        idb = sb(es, "idb", [128, 128], BF16)
        o_cat = sb(es, "o_cat", [128, NT_OWN, 1024], BF16)
        B_id = Buf("id")
        B_ocat = [[Buf("ocatA%d" % t), Buf("ocatB%d" % t)] for t in range(NT_OWN)]
        k.dma("sp", idf[:], dr["ident"].ap(), (), [B_id])
        k.copy("dve", idb[:], idf[:], [B_id], [B_id])

        def pipeline(n, stages, offs):
            for T in range(n + max(offs)):
                for s_ in reversed(range(len(stages))):
                    i = T - offs[s_]
                    if 0 <= i < n:
                        stages[s_](i)

        def head_norm(raw, ngrp, gd, gtile, wk, BBw, B_raw, eng2="pool"):
            n = ngrp * gd
            v3 = lambda ap: ap[:, 0:n].rearrange("p (a b) -> p a b", b=gd)
            return [
                lambda: k.tt(eng2, wk["sq"][:, 0:n], raw[:, 0:n], raw[:, 0:n], ALU.mult, [B_raw], [BBw["sq"]]),
                lambda: k.rsum(wk["ss"][:, 0:ngrp], v3(wk["sq"]), [BBw["sq"]], [BBw["ss"]]),
                lambda: k.act(wk["ss"][:, 16:16 + ngrp], wk["ss"][:, 0:ngrp], AF.Sqrt, [BBw["ss"]], [BBw["ss"]],
                              scale=1.0 / gd, bias=EPS),
                lambda: k.recip(wk["ss"][:, 32:32 + ngrp], wk["ss"][:, 16:16 + ngrp], [BBw["ss"]], [BBw["ss"]]),
                lambda: k.tt("dve", v3(wk["kn"]), v3(raw), bc(wk["ss"][:, 32:32 + ngrp].unsqueeze(2), [128, ngrp, gd]),
                             ALU.mult, [B_raw, BBw["ss"]], [BBw["kn"]]),
                lambda: k.tt(eng2, v3(wk["kn"]), v3(wk["kn"]), bc(gtile[:].unsqueeze(1), [128, ngrp, gd]), ALU.mult,
                             [BBw["kn"], BBw["gv"]], [BBw["kn"]]),
            ]

        def proj_phase(s1, kind, tiles, col0, QT, KT, Vst, B_QT, B_KT, B_V):
            da = kind == "da"
            ngrp, gd = (8, 64) if da else (16, 32)
            w_sb = sb(s1, "w_in_" + kind, [128, 8, 1536], BF16)
            xt = [sb(s1, "xt%d" % i, [128, 1024], F32) for i in range(2)]
            a_bf = [sb(s1, "abf%d" % i, [128, 1024], BF16) for i in range(2)]
            junk = sb(s1, "junk", [128, 1024], BF16)
            stat = [sb(s1, "stat%d" % i, [128, 4], F32) for i in range(2)]
            aT = [sb(s1, "aT%d" % i, [128, 8, 128], BF16) for i in range(2)]
            gmix = sb(s1, "gmix", [128, 1024], F32)
            gq = sb(s1, "gq", [128, gd], F32)
            gk = sb(s1, "gk", [128, gd], F32)
            raw = [[sb(s1, "raw%d_%d" % (i, c), [128, 512], F32) for c in range(2)] for i in range(2)]
            knb = [[sb(s1, "knb%d_%d" % (i, c), [128, 512], BF16) for c in range(2)] for i in range(2)]
            names = ("sq", "kn", "A", "Bt") if da else ("sq", "kn")
            wks = []
            for c in range(2):
                wks.append({n: sb(s1, "wk_%s%d" % (n, c), [128, 512], F32) for n in names})
                wks[c]["ss"] = sb(s1, "wk_ss%d" % c, [128, 48], F32)
            if da:
                cs_sb = sb(s1, "cs_sb", [128, 32, 64], F32)
                sn_sb = sb(s1, "sn_sb", [128, 32, 64], F32)
            pT = [ps(s1, "pT%d" % i, [128, 1024], BF16) for i in range(2)]
            pP = [ps(s1, "pP%d" % i, [128, 512], F32) for i in range(3)]
            pT2 = [ps(s1, "pT2%d" % i, [128, 1024], BF16) for i in range(2)]
            B_xt, B_st, B_a, B_pT, B_aT = ([Buf(), Buf()] for _ in range(5))
            B_g, B_tab, B_gq, B_gk = Buf(), Buf(), Buf(), Buf()
            B_w = [Buf() for _ in range(3)]
            B_pP = [Buf() for _ in range(3)]
            B_pT2 = [Buf(), Buf()]
            B_raw = [[Buf(), Buf()], [Buf(), Buf()]]
            B_knb = [[Buf(), Buf()], [Buf(), Buf()]]
            BW = [{n: Buf() for n in ("sq", "ss", "kn", "A", "Bt")} for _ in range(2)]
            BW[0]["gv"], BW[1]["gv"] = B_gq, B_gk
            w_in_v = dr["w_in"].ap().rearrange("(kc p) n -> p kc n", p=128)
            order = [1, 2, 0]
            for cg in order:
                k.dma("pool", w_sb[:, :, cg * 512:(cg + 1) * 512], w_in_v[:, :, col0 + cg * 512:col0 + (cg + 1) * 512], (), [B_w[cg]])
            k.dma("sp", gmix[:], bvec("g_mix", 1024), (), [B_g])
            k.dma("sp", gq[:], bvec("g_qa" if da else "g_qb", gd), (), [B_gq])
            k.dma("sp", gk[:], bvec("g_ka" if da else "g_kb", gd), (), [B_gk])
            if da:
                k.dma("sp", cs_sb[:].rearrange("p a b -> p (a b)"), dr["cs"].ap(), (), [B_tab])
                k.dma("sp", sn_sb[:].rearrange("p a b -> p (a b)"), dr["sn"].ap(), (), [B_tab])
            cnt = {"pp": 0, "p2": 0}

            def groups(i):
                return ([0] if tiles[i] < NT_OWN else []) + [1, 2]

            def s0(i):
                t, par = tiles[i], i % 2
                k.dma("sp", xt[par][:], x_ap[t * 128:(t + 1) * 128, :], (), [B_xt[par]])
                k.act(junk[:], xt[par][:], AF.Square, [B_xt[par]], [B_st[par]], accum=stat[par][:, 0:1])
                k.act(stat[par][:, 1:2], stat[par][:, 0:1], AF.Sqrt, [B_st[par]], [B_st[par]], scale=1.0 / 1024, bias=EPS)
                k.recip(stat[par][:, 2:3], stat[par][:, 1:2], [B_st[par]], [B_st[par]])
                k.stt("dve", a_bf[par][:], xt[par][:], stat[par][:, 2:3], gmix[:], ALU.mult, ALU.mult,
                      [B_xt[par], B_st[par], B_g], [B_a[par]])

            def s1_(i):
                par = i % 2
                for kc in range(8):
                    k.tr(pT[par][:, kc * 128:(kc + 1) * 128], a_bf[par][:, kc * 128:(kc + 1) * 128], idb[:],
                         [B_a[par], B_id], [B_pT[par]])
                k.copy("act", aT[par][:].rearrange("p a b -> p (a b)"), pT[par][:], [B_pT[par]], [B_aT[par]])

            def s2_(i):
                t, par = tiles[i], i % 2
                for cg in groups(i):
                    pp = cnt["pp"] % 3
                    cnt["pp"] += 1
                    for kc in range(8):
                        k.mm(pP[pp][:], aT[par][:, kc, :], w_sb[:, kc, cg * 512:(cg + 1) * 512], kc == 0, kc == 7,
                             [B_aT[par], B_w[cg]], [B_pP[pp]])
                    if cg == 2:
                        if da:
                            k.copy("act", Vst[:, t, :, 0:128], pP[pp][:].rearrange("p (a b) -> p a b", b=128), [B_pP[pp]], [B_V[t]])
                        else:
                            k.copy("act", Vst[:, i, :, 0:32], pP[pp][:].rearrange("p (a b) -> p a b", b=32), [B_pP[pp]], [B_V[i]])
                    else:
                        k.copy("dve", raw[par][cg][:], pP[pp][:], [B_pP[pp]], [B_raw[par][cg]])

            def s3_chain(i, cg):
                t, par = tiles[i], i % 2
                wk, bw = wks[cg], BW[cg]
                ch = head_norm(raw[par][cg], ngrp, gd, gq if cg == 0 else gk, wk, bw, B_raw[par][cg])
                if da:
                    kn3 = wk["kn"][:].rearrange("p (a b) -> p a b", b=64)
                    A3 = wk["A"][:].rearrange("p (a b) -> p a b", b=64)
                    Bt3 = wk["Bt"][:].rearrange("p (a b) -> p a b", b=64)
                    ch += [
                        lambda: k.tt("dve", A3, kn3, bc(cs_sb[:, t, :].unsqueeze(1), [128, 8, 64]), ALU.mult,
                                     [bw["kn"], B_tab], [bw["A"]]),
                        lambda: k.tt("pool", Bt3[:, :, 0:32], kn3[:, :, 32:64], bc(sn_sb[:, t, 0:32].unsqueeze(1), [128, 8, 32]),
                                     ALU.mult, [bw["kn"], B_tab], [bw["Bt"]]),
                        lambda: k.tt("pool", Bt3[:, :, 32:64], kn3[:, :, 0:32], bc(sn_sb[:, t, 32:64].unsqueeze(1), [128, 8, 32]),
                                     ALU.mult, [bw["kn"], B_tab], [bw["Bt"]]),
                        lambda: k.tt("dve", knb[par][cg][:], wk["A"][:], wk["Bt"][:], ALU.add, [bw["A"], bw["Bt"]],
                                     [B_knb[par][cg]]),
                    ]
                else:
                    ch.append(lambda: k.copy("dve", knb[par][cg][:], wk["kn"][:], [bw["kn"]], [B_knb[par][cg]]))
                return ch

            def s3_(i):
                chains = [s3_chain(i, cg) for cg in groups(i) if cg != 2]
                for j in range(max(len(c) for c in chains)):
                    for c in chains:
                        if j < len(c):
                            c[j]()

            def s4_(i):
                t, par = tiles[i], i % 2
                for cg in groups(i):
                    if cg == 2:
                        continue
                    p2 = cnt["p2"] % 2
                    cnt["p2"] += 1
                    for hh in range(4):
                        k.tr(pT2[p2][:, hh * 128:(hh + 1) * 128], knb[par][cg][:, hh * 128:(hh + 1) * 128], idb[:],
                             [B_knb[par][cg], B_id], [B_pT2[p2]])
                    src = pT2[p2][:, 0:512].rearrange("p (a b) -> p a b", b=128)
                    if cg == 0:
                        k.copy("act", QT[:, :, t * 128:(t + 1) * 128], src, [B_pT2[p2]], [B_QT[t]])
                    else:
                        kt_ = t if da else i
                        k.copy("act", KT[:, :, kt_ * 128:(kt_ + 1) * 128], src, [B_pT2[p2]], [B_KT[kt_]])

            pipeline(len(tiles), [s0, s1_, s2_, s3_, s4_], [0, 1, 2, 3, 4])

        with ExitStack() as sa:
            QaT = sb(sa, "QaT", [128, 4, 2048], BF16)
            KaT = sb(sa, "KaT", [128, 4, 4096], BF16)
            Va = sb(sa, "Va", [128, NT_ALL, 4, 129], BF16)
            B_QaT = [Buf("QaT%d" % t) for t in range(NT_OWN)]
            B_KaT = [Buf("KaT%d" % t) for t in range(NT_ALL)]
            B_Va = [Buf("Va%d" % t) for t in range(NT_ALL)]
            k.memset("pool", Va[:], 1.0, B_Va)

            for _once in ([0] if on("A1") else []):
                with ExitStack() as s1:
                    proj_phase(s1, "da", list(range(NT_ALL)), 0, QaT, KaT, Va, B_QaT, B_KaT, B_Va)
                    dump("A1", "QaT", QaT[:], [128, 4, 2048], BF16)
                    dump("A1", "KaT", KaT[:], [128, 4, 4096], BF16)
                    dump("A1", "Va", Va[:], [128, NT_ALL, 4, 129], BF16)
                    S.barrier()

            for _once in ([0] if on("B1") else []):
                with ExitStack() as s2:
                    NB = 4
                    NBP = 8
                    Pt = [sb(s2, "Pt%d" % i, [128, 512], BF16) for i in range(NBP)]
                    Osb = [sb(s2, "Osb%d" % i, [128, 132], F32) for i in range(4)]
                    t1 = sb(s2, "t1", [128, 4, 128], F32)
                    ob = sb(s2, "ob", [128, 4, 128], F32)
                    sqb = sb(s2, "sqb", [128, 128], F32)
                    sm = sb(s2, "sm", [128, 4, 8], F32)
                    ssq = sb(s2, "ssq", [128, 16], F32)
                    lamb = sb(s2, "lamb", [128, 4, 64], F32)
                    lamw = sb(s2, "lamw", [128, 2, 64], F32)
                    lams = sb(s2, "lams", [128, 8], F32)
                    gsub = sb(s2, "gsub", [128, 128], F32)
                    pS = [ps(s2, "pS%d" % i, [128, 512], F32) for i in range(NB)]
                    pO = [ps(s2, "pO%d" % i, [128, 512], F32) for i in range(4)]
                    B_Pt = [Buf() for _ in range(NBP)]
                    B_pS = [Buf() for _ in range(NB)]
                    B_pO = [Buf() for _ in range(4)]
                    B_Osb = [Buf() for _ in range(4)]
                    B_t1 = [Buf() for _ in range(4)]
                    B_ob = [Buf() for _ in range(4)]
                    B_sm = [Buf() for _ in range(4)]
                    B_lam, B_gs, B_ssq, B_sqb = Buf(), Buf(), Buf(), Buf()
                    for i, n in enumerate(("lam_q1", "lam_k1", "lam_q2", "lam_k2")):
                        k.dma("sp", lamb[:, i, :], bvec(n, 64), (), [B_lam])
                    k.dma("sp", gsub[:], bvec("g_sub", 128), (), [B_gs])
                    k.ts("dve", gsub[:], gsub[:], 0.8, ALU.mult, [B_gs], [B_gs])
                    k.tt("dve", lamw[:, 0, :], lamb[:, 0, :], lamb[:, 1, :], ALU.mult, [B_lam], [B_lam])
                    k.tt("dve", lamw[:, 1, :], lamb[:, 2, :], lamb[:, 3, :], ALU.mult, [B_lam], [B_lam])
                    k.rsum(lams[:, 0:2], lamw[:], [B_lam], [B_lam])
                    k.act(lams[:, 2:4], lams[:, 0:2], AF.Exp, [B_lam], [B_lam])
                    k.tt("dve", lams[:, 4:5], lams[:, 3:4], lams[:, 2:3], ALU.subtract, [B_lam], [B_lam])
                    k.ts("dve", lams[:, 5:6], lams[:, 4:5], -0.2, ALU.add, [B_lam], [B_lam])
                    items = [(h, qg, comp, kt) for h in range(4) for qg in range(4) for comp in range(2) for kt in range(NT_ALL)]
                    QaZ = [sb(s2, "QaZ%d" % c, [128, 4, 2048], BF16) for c in range(2)]
                    B_QaZ = [[Buf() for _ in range(4)] for _ in range(2)]
                    for c in range(2):
                        zr = slice(64, 128) if c == 0 else slice(0, 64)
                        cr = slice(0, 64) if c == 0 else slice(64, 128)
                        k.memset("pool", QaZ[c][zr], 0.0, B_QaZ[c])
                        for qg_ in range(4):
                            k.copy("dve" if c == 0 else "pool", QaZ[c][cr, :, qg_ * 512:(qg_ + 1) * 512],
                                   QaT[cr, :, qg_ * 512:(qg_ + 1) * 512], B_QaT[qg_ * 4:qg_ * 4 + 4], [B_QaZ[c][qg_]])

                    def b1_s0(i):
                        h, qg, comp, kt = items[i]
                        s = i % NB
                        k.mm(pS[s][:], KaT[:, h, kt * 128:(kt + 1) * 128], QaZ[comp][:, h, qg * 512:(qg + 1) * 512],
                             True, True, [B_KaT[kt], B_QaZ[comp][qg]], [B_pS[s]])
                        k.act(Pt[i % NBP][:], pS[s][:], AF.Exp, [B_pS[s]], [B_Pt[i % NBP]], scale=0.125)

                    def b1_s1(i):
                        h, qg, comp, kt = items[i]
                        s = i % NBP
                        for qs in range(4):
                            k.mm(pO[qs][:, 0:129], Pt[s][:, qs * 128:(qs + 1) * 128], Va[:, kt, h, :],
                                 kt == 0, kt == NT_ALL - 1, [B_Pt[s], B_Va[kt]], [B_pO[qs]])
                        if kt != NT_ALL - 1:
                            return
                        for qs in range(4):
                            k.copy("dve", Osb[qs][:, 0:129], pO[qs][:, 0:129], [B_pO[qs]], [B_Osb[qs]])
                        for qs in range(4):
                            O = Osb[qs]
                            bo = B_Osb[qs]
                            if comp == 0:
                                k.recip(sm[:, qs, 0:1], O[:, 128:129], [bo], [B_sm[qs]])
                                k.ts("dve", t1[:, qs, :], O[:, 0:128], sm[:, qs, 0:1], ALU.mult, [bo, B_sm[qs]], [B_t1[qs]])
                            else:
                                k.recip(sm[:, qs, 1:2], O[:, 128:129], [bo], [B_sm[qs]])
                                k.tt("dve", sm[:, qs, 2:3], sm[:, qs, 1:2], lams[:, 5:6], ALU.mult, [B_sm[qs], B_lam], [B_sm[qs]])
                                k.stt("dve", ob[:, qs, :], O[:, 0:128], sm[:, qs, 2:3], t1[:, qs, :], ALU.mult, ALU.add,
                                      [bo, B_sm[qs], B_t1[qs]], [B_ob[qs]])
                                k.tt("dve", sqb[:], ob[:, qs, :], ob[:, qs, :], ALU.mult, [B_ob[qs]], [B_sqb])
                                k.rsum(ssq[:, qs:qs + 1], sqb[:], [B_sqb], [B_ssq])
                        if comp == 1:
                            k.act(ssq[:, 4:8], ssq[:, 0:4], AF.Sqrt, [B_ssq], [B_ssq], scale=1.0 / 128, bias=EPS)
                            k.recip(ssq[:, 8:12], ssq[:, 4:8], [B_ssq], [B_ssq])
                            for qs in range(4):
                                tq = qg * 4 + qs
                                k.stt("dve", o_cat[:, tq, h * 128:(h + 1) * 128], ob[:, qs, :], ssq[:, 8 + qs:9 + qs], gsub[:],
                                      ALU.mult, ALU.mult, [B_ob[qs], B_ssq, B_gs], [B_ocat[tq][0]])

                    pipeline(len(items), [b1_s0, b1_s1], [0, 6])
                    dump("B1", "ocat", o_cat[:], [128, NT_OWN, 1024], BF16)
                    S.barrier()

        with ExitStack() as sa:
            QbT = sb(sa, "QbT", [128, 4, 2048], BF16)
            KbT = sb(sa, "KbT", [128, 4, 20 * 128], BF16)
            Vb = sb(sa, "Vb", [128, 20, 16, 33], BF16)
            B_QbT = [Buf() for _ in range(NT_OWN)]
            B_KbT = [Buf() for _ in range(20)]
            B_Vb = [Buf() for _ in range(20)]
            k.memset("pool", Vb[:], 1.0, B_Vb)
            for _once in ([0] if on("A2") else []):
                with ExitStack() as s1:
                    s2t = [30, 31] + list(range(16)) + [16, 17]
                    proj_phase(s1, "na", s2t, 1536, QbT, KbT, Vb, B_QbT, B_KbT, B_Vb)
                    dump("A2", "QbT", QbT[:], [128, 4, 2048], BF16)
                    dump("A2", "KbT", KbT[:], [128, 4, 2560], BF16)
                    dump("A2", "Vb", Vb[:], [128, 20, 16, 33], BF16)
                    S.barrier()

            for _once in ([0] if on("B2") else []):
                with ExitStack() as s2:
                    NB = 4
                    biasG = sb(s2, "biasG", [128, 4, 5, 128], F32)
                    biasS = [sb(s2, "biasS%d" % i, [128, 4, 6, 128], F32) for i in range(2)]
                    Sb = [sb(s2, "Sb%d" % i, [128, 768], F32) for i in range(2)]
                    Pb = [sb(s2, "Pb%d" % i, [128, 768], BF16) for i in range(NB)]
                    rl = [sb(s2, "rl%d" % i, [128, 4], F32) for i in range(2)]
                    pS = [ps(s2, "pSb%d" % i, [128, 1024], F32) for i in range(2)]
                    pO = [ps(s2, "pOb%d" % i, [128, 4, 128], F32) for i in range(2)]
                    B_bG = [Buf() for _ in range(4)]
                    B_bS = [Buf(), Buf()]
                    B_Sb = [Buf(), Buf()]
                    B_Pb = [Buf() for _ in range(NB)]
                    B_rl = [Buf(), Buf()]
                    B_pS = [Buf(), Buf()]
                    B_pO = [Buf(), Buf()]
                    scale_b = 32 ** -0.5
                    items = [(g, j, hh) for g in range(4) for j in range(NT_OWN) for hh in range(4)]
                    spec_idx = {}
                    c_ = 0
                    for g in range(4):
                        for j in range(NT_OWN):
                            if j in NA_OFFS:
                                spec_idx[(g, j)] = c_ % 2
                                c_ += 1

                    def b2_s0(i):
                        g, j, hh = items[i]
                        if j == 0 and hh == 0:
                            k.dma("sp", biasG[:].rearrange("p a b c -> p (a b c)"), dr["biasG"].ap()[g], (), [B_bG[0]])
                        if j in NA_OFFS:
                            offs = NA_OFFS[j]
                            sp_i = spec_idx[(g, j)]
                            if hh == 0:
                                k.dma("sp", biasS[sp_i][:].rearrange("p a b c -> p (a b c)"),
                                      dr["biasS"].ap()[NA_SPECIAL.index(j), g], (), [B_bS[sp_i]])
                            btile, bbuf = biasS[sp_i], B_bS[sp_i]
                        else:
                            offs = NA_GEN
                            btile, bbuf = biasG, B_bG[0]
                        nof = len(offs)
                        b2 = i % 2
                        pb = i % NB
                        rows = slice(hh * 32, (hh + 1) * 32)
                        for ci, off in enumerate(offs):
                            s_idx = j + off + 2
                            k.mm(pS[b2][:, ci * 128:(ci + 1) * 128], KbT[rows, g, s_idx * 128:(s_idx + 1) * 128],
                                 QbT[rows, g, j * 128:(j + 1) * 128], True, True,
                                 [B_KbT[s_idx], B_QbT[j]], [B_pS[b2]], tp=(hh * 32, 0))
                        k.stt("dve", Sb[b2][:, 0:nof * 128], pS[b2][:, 0:nof * 128], scale_b,
                              btile[:, hh, 0:nof, :].rearrange("p a b -> p (a b)"), ALU.mult, ALU.add,
                              [B_pS[b2], bbuf], [B_Sb[b2]])
                        k.act(Pb[pb][:, 0:nof * 128], Sb[b2][:, 0:nof * 128], AF.Exp, [B_Sb[b2]], [B_Pb[pb]])

                    def b2_s1(i):
                        g, j, hh = items[i]
                        offs = NA_OFFS.get(j, NA_GEN)
                        nof = len(offs)
                        h = g * 4 + hh
                        pb = i % NB
                        oi = (g * NT_OWN + j) % 2
                        for ci, off in enumerate(offs):
                            s_idx = j + off + 2
                            k.mm(pO[oi][:, hh, 0:33], Pb[pb][:, ci * 128:(ci + 1) * 128], Vb[:, s_idx, h, :],
                                 ci == 0, ci == nof - 1, [B_Pb[pb], B_Vb[s_idx]], [B_pO[oi]])
                        if hh == 3:
                            k.recip(rl[oi][:].unsqueeze(2), pO[oi][:, :, 32:33], [B_pO[oi]], [B_rl[oi]])
                            k.tt("dve", o_cat[:, j, 512 + g * 128:512 + (g + 1) * 128].rearrange("p (a b) -> p a b", b=32),
                                 pO[oi][:, :, 0:32], bc(rl[oi][:].unsqueeze(2), [128, 4, 32]), ALU.mult,
                                 [B_pO[oi], B_rl[oi]], [B_ocat[j][1]])

                    pipeline(len(items), [b2_s0, b2_s1], [0, 3])
                    dump("B2", "ocat", o_cat[:], [128, NT_OWN, 1024], BF16)
                    S.barrier()

        hres = sb(es, "hres", [128, NT_OWN, 1024], F32)
        B_h = [[Buf(), Buf()] for _ in range(NT_OWN)]
        for _once in ([0] if on("C") else []):
            with ExitStack() as s1:
                w_sb = sb(s1, "w_outb", [128, 8, 1024], BF16)
                xt = [sb(s1, "xtc%d" % i, [128, 1024], F32) for i in range(2)]
                oT = [sb(s1, "oT%d" % i, [128, 8, 128], BF16) for i in range(2)]
                pT = [ps(s1, "pTc%d" % i, [128, 1024], BF16) for i in range(2)]
                pP = [ps(s1, "pPc%d" % i, [128, 512], F32) for i in range(3)]
                B_w = Buf()
                B_xt = [Buf(), Buf()]
                B_oT = [Buf(), Buf()]
                B_pT = [Buf(), Buf()]
                B_pP = [Buf() for _ in range(3)]
                k.dma("pool", w_sb[:], dr["w_out"].ap().rearrange("(kc p) n -> p kc n", p=128), (), [B_w])
                cntc = {"pp": 0}

                def c_s0(t):
                    par = t % 2
                    k.dma("sp", xt[par][:], x_ap[t * 128:(t + 1) * 128, :], (), [B_xt[par]])
                    for kc in range(8):
                        k.tr(pT[par][:, kc * 128:(kc + 1) * 128], o_cat[:, t, kc * 128:(kc + 1) * 128], idb[:],
                             [B_ocat[t][0], B_ocat[t][1], B_id], [B_pT[par]])
                    k.copy("act", oT[par][:].rearrange("p a b -> p (a b)"), pT[par][:], [B_pT[par]], [B_oT[par]])

                def c_s1(t):
                    par = t % 2
                    for cg in range(2):
                        pp = cntc["pp"] % 3
                        cntc["pp"] += 1
                        for kc in range(8):
                            k.mm(pP[pp][:], oT[par][:, kc, :], w_sb[:, kc, cg * 512:(cg + 1) * 512], kc == 0, kc == 7,
                                 [B_oT[par], B_w], [B_pP[pp]])
                        k.tt("dve", hres[:, t, cg * 512:(cg + 1) * 512], pP[pp][:], xt[par][:, cg * 512:(cg + 1) * 512], ALU.add,
                             [B_pP[pp], B_xt[par]], [B_h[t][cg]])

                pipeline(NT_OWN, [c_s0, c_s1], [0, 1])
                dump("C", "hres", hres[:], [128, NT_OWN, 1024], F32)
                S.barrier()

        with ExitStack() as sd:
            xnT = sb(sd, "xnT", [128, 8, 2048], BF16)
            gates = sb(sd, "gates", [128, NT_OWN, 32], F32)
            B_xnT = [Buf() for _ in range(NT_OWN)]
            B_gates = [Buf() for _ in range(NT_OWN)]
            w1s = [sb(sd, "w1s%d" % i, [128, 8, 512], BF16) for i in range(2)]
            w3s = [sb(sd, "w3s%d" % i, [128, 8, 512], BF16) for i in range(2)]
            w2s = [sb(sd, "w2s%d" % i, [128, 4, 1024], BF16) for i in range(2)]
            B_w1 = [Buf(), Buf()]
            B_w3 = [Buf(), Buf()]
            B_w2 = [Buf(), Buf()]
            w1v = dr["w1"].ap()
            w3v = dr["w3"].ap()
            w2v = dr["w2"].ap()

            def load_w(e):
                wb = e % 2
                k.dma("pool", w1s[wb][:], w1v[e].rearrange("(kc p) n -> p kc n", p=128), (), [B_w1[wb]])
                k.dma("pool", w3s[wb][:], w3v[e].rearrange("(kc p) n -> p kc n", p=128), (), [B_w3[wb]])
                k.dma("pool", w2s[wb][:], w2v[e].rearrange("(kc p) n -> p kc n", p=128), (), [B_w2[wb]])

            if on("D2"):
                load_w(0)
            for _once in ([0] if on("D1") else []):
                with ExitStack() as s1:
                    gffn = sb(s1, "gffn", [128, 1024], F32)
                    w_r = sb(s1, "w_r", [128, 8, 36], F32)
                    xn = [sb(s1, "xn%d" % i, [128, 1024], F32) for i in range(2)]
                    junk = sb(s1, "junkd", [128, 1024], BF16)
                    stat = [sb(s1, "statd%d" % i, [128, 4], F32) for i in range(2)]
                    xT32 = [sb(s1, "xT32%d" % i, [128, 8, 128], F32) for i in range(2)]
                    rt = [sb(s1, "rt%d" % i, [128, 128], F32) for i in range(2)]
                    pT = [ps(s1, "pTd%d" % i, [128, 1024], F32) for i in range(2)]
                    pL = [ps(s1, "pL%d" % i, [128, 512], F32) for i in range(2)]
                    B_g, B_wr = Buf(), Buf()
                    B_xn = [Buf(), Buf()]
                    B_st = [Buf(), Buf()]
                    B_xT = [Buf(), Buf()]
                    B_rt = [Buf(), Buf()]
                    B_pT = [Buf(), Buf()]
                    B_pL = [Buf(), Buf()]
                    k.dma("sp", gffn[:], bvec("g_ffn", 1024), (), [B_g])
                    k.dma("sp", w_r[:], dr["w_r"].ap().rearrange("(kc p) n -> p kc n", p=128), (), [B_wr])

                    def d1_s0(t):
                        par = t % 2
                        hb = B_h[t]
                        k.act(junk[:], hres[:, t, :], AF.Square, hb, [B_st[par]], accum=stat[par][:, 0:1])
                        k.act(stat[par][:, 1:2], stat[par][:, 0:1], AF.Sqrt, [B_st[par]], [B_st[par]], scale=1.0 / 1024, bias=EPS)
                        k.recip(stat[par][:, 2:3], stat[par][:, 1:2], [B_st[par]], [B_st[par]])
                        k.stt("dve", xn[par][:], hres[:, t, :], stat[par][:, 2:3], gffn[:], ALU.mult, ALU.mult,
                              hb + [B_st[par], B_g], [B_xn[par]])

                    def d1_s1(t):
                        par = t % 2
                        for kc in range(8):
                            k.tr(pT[par][:, kc * 128:(kc + 1) * 128], xn[par][:, kc * 128:(kc + 1) * 128], idf[:],
                                 [B_xn[par], B_id], [B_pT[par]])
                        k.copy("act", xT32[par][:].rearrange("p a b -> p (a b)"), pT[par][:], [B_pT[par]], [B_xT[par], B_pT[par]])
                        k.copy("dve", xnT[:, :, t * 128:(t + 1) * 128], pT[par][:].rearrange("p (a b) -> p a b", b=128),
                               [B_pT[par]], [B_xnT[t]])

                    def d1_s2_chain(t):
                        par = t % 2
                        r = rt[par]
                        br = [B_rt[par]]
                        e3 = lambda ap: ap.rearrange("p (e g) -> p e g", g=4)

                        def _mm():
                            for kc in range(8):
                                k.mm(pL[par][:, 0:36], xT32[par][:, kc, :], w_r[:, kc, :], kc == 0, kc == 7,
                                     [B_xT[par], B_wr], [B_pL[par]])
                        return [
                            _mm,
                            lambda: k.copy("dve", r[:, 0:36], pL[par][:, 0:36], [B_pL[par]], br),
                            lambda: k.rmax(r[:, 40:41], r[:, 0:4], br, br),
                            lambda: k.ts("dve", r[:, 44:48], r[:, 0:4], r[:, 40:41], ALU.is_ge, br, br),
                            lambda: k.ts("dve", r[:, 48:52], r[:, 0:4], r[:, 40:41], ALU.subtract, br, br),
                            lambda: k.act(r[:, 52:56], r[:, 48:52], AF.Exp, br, br),
                            lambda: k.rsum(r[:, 41:42], r[:, 52:56], br, br),
                            lambda: k.recip(r[:, 42:43], r[:, 41:42], br, br),
                            lambda: k.tt("dve", e3(r[:, 64:96]), r[:, 4:36].rearrange("p (g e) -> p e g", e=8),
                                         bc(r[:, 44:48].unsqueeze(1), [128, 8, 4]), ALU.mult, br, br),
                            lambda: k.rsum(r[:, 56:64], e3(r[:, 64:96]), br, br),
                            lambda: k.rmax(r[:, 96:97], r[:, 56:64], br, br),
                            lambda: k.ts("dve", r[:, 100:108], r[:, 56:64], r[:, 96:97], ALU.is_ge, br, br),
                            lambda: k.stt("dve", r[:, 108:116], r[:, 100:108], -1e30, r[:, 56:64], ALU.mult, ALU.add, br, br),
                            lambda: k.rmax(r[:, 97:98], r[:, 108:116], br, br),
                            lambda: k.ts("dve", r[:, 116:124], r[:, 56:64], r[:, 97:98], ALU.is_ge, br, br),
                            lambda: k.ts("dve", r[:, 100:108], r[:, 56:64], r[:, 96:97], ALU.subtract, br, br),
                            lambda: k.act(r[:, 100:108], r[:, 100:108], AF.Exp, br, br),
                            lambda: k.tt("dve", r[:, 100:108], r[:, 100:108], r[:, 116:124], ALU.mult, br, br),
                            lambda: k.rsum(r[:, 98:99], r[:, 100:108], br, br),
                            lambda: k.recip(r[:, 99:100], r[:, 98:99], br, br),
                            lambda: k.tt("dve", r[:, 99:100], r[:, 99:100], r[:, 42:43], ALU.mult, br, br),
                            lambda: k.ts("dve", r[:, 100:108], r[:, 100:108], r[:, 99:100], ALU.mult, br, br),
                            lambda: k.tt("dve", gates[:, t, :].rearrange("p (g e) -> p g e", e=8),
                                         bc(r[:, 44:48].unsqueeze(2), [128, 4, 8]), bc(r[:, 100:108].unsqueeze(1), [128, 4, 8]),
                                         ALU.mult, br, [B_gates[t]]),
                        ]

                    for T in range(NT_OWN + 3):
                        ch = []
                        if T >= 3 and (T - 3) % 2 == 0:
                            ch = [d1_s2_chain(T - 3), d1_s2_chain(T - 2)]
                        for j in range(max([len(c) for c in ch] + [0])):
                            for c in ch:
                                if j < len(c):
                                    c[j]()
                        if 0 <= T - 1 < NT_OWN:
                            d1_s1(T - 1)
                        if 0 <= T < NT_OWN:
                            d1_s0(T)
                    dump("D1", "gates", gates[:], [128, NT_OWN, 32], F32)
                    dump("D1", "xnT", xnT[:], [128, 8, 2048], BF16)
                    S.barrier()

            for _once in ([0] if on("D2") else []):
                with ExitStack() as s2:
                    hdn = [sb(s2, "hdn%d" % i, [128, 4, 512], BF16) for i in range(3)]
                    sil = [sb(s2, "sil%d" % i, [128, 512], F32) for i in range(2)]
                    p1 = [ps(s2, "p1_%d" % i, [128, 512], F32) for i in range(2)]
                    p3 = [ps(s2, "p3_%d" % i, [128, 512], F32) for i in range(2)]
                    pY = [ps(s2, "pY%d" % i, [128, 512], F32) for i in range(3)]
                    B_hdn = [[Buf() for _ in range(4)] for _ in range(3)]
                    B_sil = [Buf(), Buf()]
                    B_p1 = [Buf(), Buf()]
                    B_p3 = [Buf(), Buf()]
                    B_pY = [Buf() for _ in range(3)]
                    cntd = {"p": 0, "y": 0}

                    def d2_s0(i):
                        e, tg = i // 4, i % 4
                        wb = e % 2
                        hb = i % 3
                        if tg == 1 and e + 1 < 32:
                            load_w(e + 1)
                        for hc in range(4):
                            pb = cntd["p"] % 2
                            cntd["p"] += 1
                            for kc in range(8):
                                k.mm(p1[pb][:], w1s[wb][:, kc, hc * 128:(hc + 1) * 128], xnT[:, kc, tg * 512:(tg + 1) * 512],
                                     kc == 0, kc == 7, [B_w1[wb]] + B_xnT[tg * 4:tg * 4 + 4], [B_p1[pb]])
                            for kc in range(8):
                                k.mm(p3[pb][:], w3s[wb][:, kc, hc * 128:(hc + 1) * 128], xnT[:, kc, tg * 512:(tg + 1) * 512],
                                     kc == 0, kc == 7, [B_w3[wb]] + B_xnT[tg * 4:tg * 4 + 4], [B_p3[pb]])
                            k.act(sil[pb][:], p1[pb][:], AF.Silu, [B_p1[pb]], [B_sil[pb]])
                            k.tt("dve", hdn[hb][:, hc, :], sil[pb][:], p3[pb][:], ALU.mult, [B_sil[pb], B_p3[pb]], [B_hdn[hb][hc]])

                    def d2_s1(i):
                        e, tg = i // 4, i % 4
                        wb = e % 2
                        hb = i % 3
                        for tt_ in range(4):
                            t = tg * 4 + tt_
                            for cg in range(2):
                                yb = cntd["y"] % 3
                                cntd["y"] += 1
                                for hc in range(4):
                                    k.mm(pY[yb][:], hdn[hb][:, hc, tt_ * 128:(tt_ + 1) * 128], w2s[wb][:, hc, cg * 512:(cg + 1) * 512],
                                         hc == 0, hc == 3, [B_hdn[hb][hc], B_w2[wb]], [B_pY[yb]])
                                k.stt("dve", hres[:, t, cg * 512:(cg + 1) * 512], pY[yb][:], gates[:, t, e:e + 1],
                                      hres[:, t, cg * 512:(cg + 1) * 512], ALU.mult, ALU.add,
                                      [B_pY[yb], B_gates[t], B_h[t][cg]], [B_h[t][cg]])

                    pipeline(128, [d2_s0, d2_s1], [0, 2])
                    dump("D2", "hres", hres[:], [128, NT_OWN, 1024], F32)
                    S.barrier()

        out_toks = []
        for _once in ([0] if on("E") else []):
            with ExitStack() as s1:
                wple = sb(s1, "wple", [128, 2, 1024], BF16)
                wplg = sb(s1, "wplg", [128, 8, 1024], BF16)
                gplg = sb(s1, "gplg", [128, 1024], F32)
                gple = sb(s1, "gple", [128, 1024], F32)
                pt = [sb(s1, "pt%d" % i, [128, 256], F32) for i in range(2)]
                ptb = [sb(s1, "ptb%d" % i, [128, 256], BF16) for i in range(2)]
                pTs = [sb(s1, "pTs%d" % i, [128, 2, 128], BF16) for i in range(2)]
                hn = [sb(s1, "hn%d" % i, [128, 1024], BF16) for i in range(2)]
                hT = [sb(s1, "hT%d" % i, [128, 8, 128], BF16) for i in range(2)]
                junk = sb(s1, "junke", [128, 1024], BF16)
                statA = [sb(s1, "stateA%d" % i, [128, 4], F32) for i in range(2)]
                statB = [sb(s1, "stateB%d" % i, [128, 4], F32) for i in range(2)]
                pe_s = [sb(s1, "pe_s%d" % i, [128, 1024], F32) for i in range(2)]
                sg = [sb(s1, "sg%d" % i, [128, 1024], F32) for i in range(2)]
                yo = [sb(s1, "yo%d" % i, [128, 1024], F32) for i in range(2)]
                pTp = ps(s1, "pTp", [128, 1024], BF16)
                pTh = ps(s1, "pTh", [128, 1024], BF16)
                pE = ps(s1, "pE", [128, 1024], F32)
                pG = ps(s1, "pG", [128, 1024], F32)
                B = {n: [Buf(), Buf()] for n in ("pt", "ptb", "pTs", "hn", "hT", "stA", "stB", "pe_s", "sg", "yo")}
                B_pTp, B_pTh, B_pE, B_pG = Buf(), Buf(), [Buf(), Buf()], [Buf(), Buf()]
                B_wple, B_wplg, B_g1, B_g2 = Buf(), Buf(), Buf(), Buf()
                k.dma("pool", wple[:], dr["w_ple"].ap().rearrange("(kc p) n -> p kc n", p=128), (), [B_wple])
                k.dma("pool", wplg[:], dr["w_plg"].ap().rearrange("(kc p) n -> p kc n", p=128), (), [B_wplg])
                k.dma("sp", gplg[:], bvec("g_plg", 1024), (), [B_g1])
                k.dma("sp", gple[:], bvec("g_ple", 1024), (), [B_g2])

                def e_s0(t):
                    par = t % 2
                    hb = B_h[t]
                    st_ = statA[par]
                    bs = [B["stA"][par]]
                    k.dma("sp", pt[par][:], p_ap[t * 128:(t + 1) * 128, :], (), [B["pt"][par]])
                    k.copy("dve", ptb[par][:], pt[par][:], [B["pt"][par]], [B["ptb"][par]])
                    k.act(junk[:], hres[:, t, :], AF.Square, hb, bs, accum=st_[:, 0:1])
                    k.act(st_[:, 1:2], st_[:, 0:1], AF.Sqrt, bs, bs, scale=1.0 / 1024, bias=EPS)
                    k.recip(st_[:, 2:3], st_[:, 1:2], bs, bs)
                    k.stt("dve", hn[par][:], hres[:, t, :], st_[:, 2:3], gplg[:], ALU.mult, ALU.mult, hb + bs + [B_g1], [B["hn"][par]])

                def e_s1(t):
                    par = t % 2
                    for kc in range(2):
                        k.tr(pTp[:, kc * 128:(kc + 1) * 128], ptb[par][:, kc * 128:(kc + 1) * 128], idb[:], [B["ptb"][par], B_id], [B_pTp])
                    k.copy("act", pTs[par][:].rearrange("p a b -> p (a b)"), pTp[:, 0:256], [B_pTp], [B["pTs"][par]])
                    for kc in range(8):
                        k.tr(pTh[:, kc * 128:(kc + 1) * 128], hn[par][:, kc * 128:(kc + 1) * 128], idb[:], [B["hn"][par], B_id], [B_pTh])
                    k.copy("act", hT[par][:].rearrange("p a b -> p (a b)"), pTh[:], [B_pTh], [B["hT"][par]])

                def e_s2(t):
                    par = t % 2
                    for cg in range(2):
                        for kc in range(2):
                            k.mm(pE[:, cg * 512:(cg + 1) * 512], pTs[par][:, kc, :], wple[:, kc, cg * 512:(cg + 1) * 512], kc == 0, kc == 1,
                                 [B["pTs"][par], B_wple], [B_pE[cg]])
                    k.copy("act", pe_s[par][:], pE[:], B_pE, [B["pe_s"][par]])
                    for cg in range(2):
                        for kc in range(8):
                            k.mm(pG[:, cg * 512:(cg + 1) * 512], hT[par][:, kc, :], wplg[:, kc, cg * 512:(cg + 1) * 512], kc == 0, kc == 7,
                                 [B["hT"][par], B_wplg], [B_pG[cg]])
                    k.act(sg[par][:], pG[:], AF.Sigmoid, B_pG, [B["sg"][par]])

                def e_s3(t):
                    par = t % 2
                    hb = B_h[t]
                    st_ = statB[par]
                    bs = [B["stB"][par]]
                    k.act(junk[:], pe_s[par][:], AF.Square, [B["pe_s"][par]], bs, accum=st_[:, 0:1])
                    k.act(st_[:, 1:2], st_[:, 0:1], AF.Sqrt, bs, bs, scale=1.0 / 1024, bias=EPS)
                    k.recip(st_[:, 2:3], st_[:, 1:2], bs, bs)
                    k.stt("dve", pe_s[par][:], pe_s[par][:], st_[:, 2:3], gple[:], ALU.mult, ALU.mult,
                          [B["pe_s"][par], B_g2] + bs, [B["pe_s"][par]])
                    k.tt("dve", sg[par][:], sg[par][:], pe_s[par][:], ALU.mult, [B["sg"][par], B["pe_s"][par]], [B["sg"][par]])
                    k.tt("dve", yo[par][:], sg[par][:], hres[:, t, :], ALU.add, [B["sg"][par]] + hb, [B["yo"][par]])
                    out_toks.append(k.dma("sp", y_ap[t * 128:(t + 1) * 128, :], yo[par][:], [B["yo"][par]], ()))

                pipeline(NT_OWN, [e_s0, e_s1, e_s2, e_s3], [0, 1, 2, 3])
                S.barrier()
        S.ops["sp"].append(Op(None, list(out_toks) + dbg_toks, None))
        S.emit()
    return nc


def _rope_tables():
    inv = (np.float32(10000.0) ** (-np.arange(0, 64, 2, dtype=np.float32) / np.float32(64))).astype(np.float32)
    ang = np.arange(4096, dtype=np.float32)[:, None] * inv[None, :]
    return np.cos(ang).astype(np.float32), np.sin(ang).astype(np.float32)


def _bias_tables(rpb, hf):
    kp = np.arange(128)
    kpar, kcol = kp // 64, kp % 64
    qpar, qcol = kp // 64, kp % 64
    cs = np.clip(qcol - 8, 0, 48)
    col_valid = (kcol[:, None] >= cs[None, :]) & (kcol[:, None] < cs[None, :] + 16)
    col_off = np.clip(kcol[:, None] - qcol[None, :] + 15, 0, 30)

    def tile(j, off, hf_):
        r = hf_ * 32 + 2 * j + qpar
        kr = hf_ * 32 + 2 * (j + off) + kpar
        rs = np.clip(r - 4, 0, 56)
        row_valid = (kr[:, None] >= rs[None, :]) & (kr[:, None] <= rs[None, :] + 7) & (kr[:, None] >= 0) & (kr[:, None] <= 63)
        row_off = np.clip(kr[:, None] - r[None, :] + 7, 0, 14)
        valid = row_valid & col_valid
        vals = rpb[:, row_off, col_off]
        return np.where(valid[None], vals, np.float32(NEG)).astype(np.float32)

    G = np.zeros((4, 128, 4, 5, 128), np.float32)
    for oi, off in enumerate(NA_GEN):
        tl = tile(8, off, 0)
        for g in range(4):
            G[g, :, :, oi, :] = tl[g * 4:(g + 1) * 4].transpose(1, 0, 2)
    Sp = np.full((4, 4, 128, 4, 6, 128), np.float32(NEG), np.float32)
    for ji, j in enumerate(NA_SPECIAL):
        for oi, off in enumerate(NA_OFFS[j]):
            tl = tile(j, off, hf)
            for g in range(4):
                Sp[ji, g, :, :, oi, :] = tl[g * 4:(g + 1) * 4].transpose(1, 0, 2)
    return G.reshape(4, 128, 4 * 5 * 128), Sp.reshape(4, 4, 128, 4 * 6 * 128)


_NC_CACHE = {}


def prep_inputs(x, p, g_mix, w_in, g_qa, g_ka, lam_q1, lam_k1, lam_q2, lam_k2, g_sub, g_qb, g_kb,
           rpb, w_out, g_ffn, w_rg, w_re, w1, w3, w2, g_plg, w_plg, w_ple, g_ple):
    f = lambda a: np.ascontiguousarray(np.asarray(a, dtype=np.float32))
    x = f(x); p = f(p)
    cos, sin = _rope_tables()
    common = {
        "w_in": f(w_in)[0], "w_out": f(w_out)[0], "w_plg": f(w_plg)[0], "w_ple": f(w_ple)[0],
        "w_r": np.ascontiguousarray(np.concatenate([f(w_rg)[0], f(w_re)[0]], axis=1)),
        "w1": f(w1)[0], "w3": f(w3)[0], "w2": f(w2)[0],
        "g_mix": f(g_mix), "g_ffn": f(g_ffn), "g_plg": f(g_plg), "g_ple": f(g_ple),
        "g_qa": f(g_qa), "g_ka": f(g_ka), "lam_q1": f(lam_q1), "lam_k1": f(lam_k1),
        "lam_q2": f(lam_q2), "lam_k2": f(lam_k2), "g_sub": f(g_sub), "g_qb": f(g_qb), "g_kb": f(g_kb),
        "ident": np.eye(128, dtype=np.float32),
    }
    rpb0 = f(rpb)[0]
    in_maps = []
    for c in range(8):
        b, hf = c // 2, c % 2
        own = slice(hf * 2048, (hf + 1) * 2048)
        oth = slice((1 - hf) * 2048, (2 - hf) * 2048)
        xc = np.ascontiguousarray(np.concatenate([x[b, own], x[b, oth]], axis=0))
        pos = np.concatenate([np.arange(4096)[own], np.arange(4096)[oth]])
        cc = np.concatenate([cos[pos], cos[pos]], axis=1)
        ss = np.concatenate([-sin[pos], sin[pos]], axis=1)
        cs_t = np.ascontiguousarray(cc.reshape(32, 128, 64).transpose(1, 0, 2).reshape(128, 32 * 64))
        sn_t = np.ascontiguousarray(ss.reshape(32, 128, 64).transpose(1, 0, 2).reshape(128, 32 * 64))
        G, Sp = _bias_tables(rpb0, hf)
        m = dict(common)
        m.update({"x": xc, "p": np.ascontiguousarray(p[0, b, own]), "cs": cs_t, "sn": sn_t, "biasG": G, "biasS": Sp})
        in_maps.append(m)
    return in_maps


def kernel(**inputs):
    in_maps = prep_inputs(**inputs)
    if "nc" not in _NC_CACHE:
        _NC_CACHE["nc"] = build_program()
    nc = _NC_CACHE["nc"]
    res = run_bass_kernel_spmd(nc, in_maps, core_ids=list(range(8)))
    out = np.zeros((4, 4096, 1024), np.float32)
    for c in range(8):
        b, hf = c // 2, c % 2
        out[b, hf * 2048:(hf + 1) * 2048] = res.results[c]["y"]
    return out
```
